# Optimizing a Trainium2 kernel written in Bass

```python
import math
import jax, jax.numpy as jnp
from jax import lax
import numpy as np

D_MODEL = 1024
BATCH = 32
SEQ = 2048
DEPTH = 1

PLE_DIM = 256
EPS = 1e-6
NEG = -1e30

HEAD_DIM_A = 64
A_WIDTH = D_MODEL // 2
N_HEADS_A = A_WIDTH // HEAD_DIM_A
DILATED_BRANCHES = ((128, 1), (512, 4), (2048, 16))
BAND_BLOCK = 128

REL_BUCKETS = 32
REL_MAX_DISTANCE = 2048

M_WIDTH = D_MODEL - A_WIDTH
V_DIM = 64
N_HEADS_M = M_WIDTH // V_DIM
Q_LORA = 384
KV_LORA = 256
NOPE_DIM = 64
ROPE_DIM = 32
ROPE_THETA = 10000.0
QUERY_BLOCK = 128

IN_COLS = 3 * A_WIDTH + Q_LORA + KV_LORA + ROPE_DIM

N_GROUPS = 4
EXPERTS_PER_GROUP = 8
N_EXPERTS = N_GROUPS * EXPERTS_PER_GROUP
TOP_K_IN_GROUP = 2
EXPERT_FF = 512
MOE_BLOCK = 256

kernel_name = "hybrid_dilated_mla_hiermoe_layer"


def rms_norm(x, gain):
    xf = x.astype(jnp.float32)
    y = xf * lax.rsqrt(jnp.mean(xf * xf, axis=-1, keepdims=True) + EPS)
    return (y * gain.astype(jnp.float32)).astype(x.dtype)


def rope(x, pos):
    half = ROPE_DIM // 2
    inv = 1.0 / (ROPE_THETA ** (jnp.arange(half, dtype=jnp.float32) * 2.0 / ROPE_DIM))
    ang = pos.astype(jnp.float32)[:, None] * inv[None, :]
    cos = jnp.cos(ang)[:, None, :]
    sin = jnp.sin(ang)[:, None, :]
    xf = x.astype(jnp.float32)
    x1, x2 = xf[..., :half], xf[..., half:]
    return jnp.concatenate([x1 * cos - x2 * sin, x2 * cos + x1 * sin], axis=-1).astype(x.dtype)


def t5_bucket(dist):
    max_exact = REL_BUCKETS // 2
    n = jnp.maximum(dist, 0)
    nf = jnp.maximum(n, 1).astype(jnp.float32)
    large = max_exact + (jnp.log(nf / max_exact) / math.log(REL_MAX_DISTANCE / max_exact)
                         * (REL_BUCKETS - max_exact)).astype(jnp.int32)
    large = jnp.minimum(large, REL_BUCKETS - 1)
    return jnp.where(n < max_exact, n, large)


def dilated_branch(q, k, v, rel_bias, window, dilation):
    B, S, H, Dh = q.shape
    L = S // dilation
    nb = -(-L // BAND_BLOCK)
    Lp = nb * BAND_BLOCK
    band = window // dilation
    BB = BAND_BLOCK

    def to_sub(t):
        t = t.reshape(B, L, dilation, H, Dh).transpose(0, 3, 2, 1, 4)
        t = jnp.pad(t, ((0, 0), (0, 0), (0, 0), (0, Lp - L), (0, 0)))
        return t.reshape(B, H, dilation, nb, BB, Dh)

    def with_prev(t):
        prev = jnp.pad(t, ((0, 0), (0, 0), (0, 0), (1, 0), (0, 0), (0, 0)))[:, :, :, :-1]
        return jnp.concatenate([prev, t], axis=4)

    qs = to_sub(q)
    kk = with_prev(to_sub(k))
    vv = with_prev(to_sub(v)).astype(jnp.float32)

    s = jnp.einsum('bhrnid,bhrnjd->bhrnij', qs, kk).astype(jnp.float32) * (Dh ** -0.5)
    i = jnp.arange(BB)[:, None]
    j = jnp.arange(2 * BB)[None, :]
    delta = BB + i - j
    bias = rel_bias.astype(jnp.float32)[t5_bucket(delta * dilation)]
    bias = bias.transpose(2, 0, 1)[:, None, None]
    band_ok = (delta >= 0) & (delta <= band)
    key_ok = (jnp.arange(nb)[:, None, None] > 0) | (j[None] >= BB)
    mask = band_ok[None] & key_ok
    s = jnp.where(mask, s + bias, NEG)
    m = jnp.max(s, axis=-1, keepdims=True)
    e = jnp.exp(s - m)
    den = jnp.sum(e, axis=-1)
    o = jnp.einsum('bhrnij,bhrnjd->bhrnid', e, vv) / den[..., None]
    lse = m[..., 0] + jnp.log(den)

    o = o.reshape(B, H, dilation, Lp, Dh)[:, :, :, :L].transpose(0, 3, 2, 1, 4).reshape(B, S, H, Dh)
    lse = lse.reshape(B, H, dilation, Lp)[:, :, :, :L].transpose(0, 3, 2, 1).reshape(B, S, H)
    return o, lse


def dilated_attention(q, k, v, rel_bias):
    outs, lses = [], []
    for window, dilation in DILATED_BRANCHES:
        o, lse = dilated_branch(q, k, v, rel_bias, window, dilation)
        outs.append(o)
        lses.append(lse)
    w = jax.nn.softmax(jnp.stack(lses), axis=0)
    o = jnp.sum(w[..., None] * jnp.stack(outs), axis=0)
    B, S, H, Dh = o.shape
    return o.reshape(B, S, H * Dh)


def mla(cq, ckv, kr, q_a_gain, w_q_up, kv_a_gain, w_kv_up,
        qn_nope_gain, qn_rope_gain, kn_nope_gain, kn_rope_gain, pos):
    B, S, _ = cq.shape
    q = (rms_norm(cq, q_a_gain) @ w_q_up).reshape(B, S, N_HEADS_M, NOPE_DIM + ROPE_DIM)
    kv = (rms_norm(ckv, kv_a_gain) @ w_kv_up).reshape(B, S, N_HEADS_M, NOPE_DIM + V_DIM)
    k_nope, v = kv[..., :NOPE_DIM], kv[..., NOPE_DIM:]
    q_nope = rms_norm(q[..., :NOPE_DIM], qn_nope_gain)
    q_rope = rope(rms_norm(q[..., NOPE_DIM:], qn_rope_gain), pos)
    k_nope = rms_norm(k_nope, kn_nope_gain)
    k_rope = rope(rms_norm(kr, kn_rope_gain)[:, :, None, :], pos)
    qh = jnp.concatenate([q_nope, q_rope], axis=-1).transpose(0, 2, 1, 3)
    kh = jnp.concatenate([k_nope, jnp.broadcast_to(k_rope, (B, S, N_HEADS_M, ROPE_DIM))],
                         axis=-1).transpose(0, 2, 1, 3)
    vh = v.transpose(0, 2, 1, 3).astype(jnp.float32)
    scale = (NOPE_DIM + ROPE_DIM) ** -0.5
    nq = S // QUERY_BLOCK
    qb = qh.reshape(B, N_HEADS_M, nq, QUERY_BLOCK, NOPE_DIM + ROPE_DIM).transpose(2, 0, 1, 3, 4)
    kpos = jnp.arange(S)

    def block(args):
        qi, bi = args
        s = jnp.einsum('bhid,bhjd->bhij', qi, kh).astype(jnp.float32) * scale
        qpos = bi * QUERY_BLOCK + jnp.arange(QUERY_BLOCK)
        s = jnp.where(kpos[None, :] <= qpos[:, None], s, NEG)
        pr = jax.nn.softmax(s, axis=-1)
        return jnp.einsum('bhij,bhjd->bhid', pr, vh)

    o = lax.map(block, (qb, jnp.arange(nq)))
    return o.transpose(1, 0, 3, 2, 4).reshape(B, S, N_HEADS_M * V_DIM)


def hier_moe(xn, w_rg, b_rg, w_re, b_re, w_gate, w_up, w_down):
    B, S, D = xn.shape
    N = B * S
    K = TOP_K_IN_GROUP
    xt = xn.reshape(N, D)
    gprob = jax.nn.softmax((xt @ w_rg + b_rg).astype(jnp.float32), axis=-1)
    g_gate, g_idx = lax.top_k(gprob, 1)
    elog = (xt @ w_re + b_re).astype(jnp.float32).reshape(N, N_GROUPS, EXPERTS_PER_GROUP)
    elog = jnp.take_along_axis(elog, g_idx[:, :, None], axis=1)[:, 0]
    e_w, e_idx = lax.top_k(jax.nn.softmax(elog, axis=-1), K)
    e_w = e_w / jnp.sum(e_w, axis=-1, keepdims=True)
    weights = (g_gate * e_w).reshape(-1)
    flat_e = (g_idx * EXPERTS_PER_GROUP + e_idx).reshape(-1).astype(jnp.int32)
    flat_t = jnp.repeat(jnp.arange(N, dtype=jnp.int32), K)
    order = jnp.argsort(flat_e)
    sorted_e = flat_e[order]
    counts = jax.ops.segment_sum(jnp.ones_like(flat_e), flat_e, num_segments=N_EXPERTS)
    pcounts = (counts + MOE_BLOCK - 1) // MOE_BLOCK * MOE_BLOCK
    pend = jnp.cumsum(pcounts)
    pstart = pend - pcounts
    start = jnp.cumsum(counts) - counts
    dest = pstart[sorted_e] + (jnp.arange(N * K, dtype=jnp.int32) - start[sorted_e])
    R = N * K + N_EXPERTS * MOE_BLOCK
    row_tok = jnp.full((R,), N, jnp.int32).at[dest].set(flat_t[order])
    row_w = jnp.zeros((R,), jnp.float32).at[dest].set(weights[order])
    x_pad = jnp.concatenate([xt, jnp.zeros((1, D), xt.dtype)], axis=0)
    xr = x_pad[row_tok].reshape(R // MOE_BLOCK, MOE_BLOCK, D)
    blk_e = jnp.minimum(jnp.searchsorted(pend, jnp.arange(R // MOE_BLOCK) * MOE_BLOCK, side='right'),
                        N_EXPERTS - 1)

    def expert_block(args):
        xb, e = args
        hdn = jax.nn.silu(xb @ w_gate[e]) * (xb @ w_up[e])
        return hdn @ w_down[e]

    y = lax.map(expert_block, (xr, blk_e)).reshape(R, D)
    out = jax.ops.segment_sum(y.astype(jnp.float32) * row_w[:, None], row_tok, num_segments=N + 1)[:N]
    return out.reshape(B, S, D).astype(xn.dtype)


def setup_inputs(seed: int = 0) -> dict:
    key = jax.random.key(seed)
    ks = jax.random.split(key, 32)
    f32 = jnp.float32
    L = DEPTH
    D = D_MODEL

    def w(k, shape, fan_in):
        return jax.random.normal(k, shape, f32) * (fan_in ** -0.5)

    def gain(k, shape):
        return 1.0 + 0.05 * jax.random.normal(k, shape, f32)

    return {
        "x": jax.random.normal(ks[0], (BATCH, SEQ, D), f32),
        "p": jax.random.normal(ks[1], (DEPTH, BATCH, SEQ, PLE_DIM), f32),
        "rel_bias": 0.5 * jax.random.normal(ks[2], (REL_BUCKETS, N_HEADS_A), f32),
        "norm_mix_gain": gain(ks[3], (L, D)),
        "w_in": w(ks[4], (L, D, IN_COLS), D),
        "qn_a_gain": gain(ks[5], (L, HEAD_DIM_A)),
        "kn_a_gain": gain(ks[6], (L, HEAD_DIM_A)),
        "q_a_gain": gain(ks[7], (L, Q_LORA)),
        "w_q_up": w(ks[8], (L, Q_LORA, N_HEADS_M * (NOPE_DIM + ROPE_DIM)), Q_LORA),
        "kv_a_gain": gain(ks[9], (L, KV_LORA)),
        "w_kv_up": w(ks[10], (L, KV_LORA, N_HEADS_M * (NOPE_DIM + V_DIM)), KV_LORA),
        "qn_nope_gain": gain(ks[11], (L, NOPE_DIM)),
        "qn_rope_gain": gain(ks[12], (L, ROPE_DIM)),
        "kn_nope_gain": gain(ks[13], (L, NOPE_DIM)),
        "kn_rope_gain": gain(ks[14], (L, ROPE_DIM)),
        "w_out": w(ks[15], (L, A_WIDTH + M_WIDTH, D), A_WIDTH + M_WIDTH),
        "norm_ffn_gain": gain(ks[16], (L, D)),
        "w_router_group": w(ks[17], (L, D, N_GROUPS), D),
        "b_router_group": 0.01 * jax.random.normal(ks[18], (L, N_GROUPS), f32),
        "w_router_expert": w(ks[19], (L, D, N_EXPERTS), D),
        "b_router_expert": 0.01 * jax.random.normal(ks[20], (L, N_EXPERTS), f32),
        "w_exp_gate": w(ks[21], (L, N_EXPERTS, D, EXPERT_FF), D),
        "w_exp_up": w(ks[22], (L, N_EXPERTS, D, EXPERT_FF), D),
        "w_exp_down": w(ks[23], (L, N_EXPERTS, EXPERT_FF, D), EXPERT_FF),
        "w_ple_proj": w(ks[24], (L, PLE_DIM, D), PLE_DIM),
        "ple_norm_gain": gain(ks[25], (L, D)),
        "w_ple_gate": w(ks[26], (L, D, D), D),
        "b_ple_gate": 0.1 * jax.random.normal(ks[27], (L, D), f32),
    }


def reference(x, p, rel_bias, norm_mix_gain, w_in, qn_a_gain, kn_a_gain, q_a_gain, w_q_up,
              kv_a_gain, w_kv_up, qn_nope_gain, qn_rope_gain, kn_nope_gain, kn_rope_gain,
              w_out, norm_ffn_gain, w_router_group, b_router_group, w_router_expert,
              b_router_expert, w_exp_gate, w_exp_up, w_exp_down, w_ple_proj, ple_norm_gain,
              w_ple_gate, b_ple_gate):
    B, S, D = x.shape
    pos = jnp.arange(S, dtype=jnp.int32)
    h = x
    c0 = 0
    c1 = A_WIDTH
    c2 = 2 * A_WIDTH
    c3 = 3 * A_WIDTH
    c4 = c3 + Q_LORA
    c5 = c4 + KV_LORA
    for i in range(DEPTH):
        xn = rms_norm(h, norm_mix_gain[i])
        proj = xn @ w_in[i]
        qa = rms_norm(proj[..., c0:c1].reshape(B, S, N_HEADS_A, HEAD_DIM_A), qn_a_gain[i])
        ka = rms_norm(proj[..., c1:c2].reshape(B, S, N_HEADS_A, HEAD_DIM_A), kn_a_gain[i])
        va = proj[..., c2:c3].reshape(B, S, N_HEADS_A, HEAD_DIM_A)
        o_a = dilated_attention(qa, ka, va, rel_bias)
        o_m = mla(proj[..., c3:c4], proj[..., c4:c5], proj[..., c5:], q_a_gain[i], w_q_up[i],
                  kv_a_gain[i], w_kv_up[i], qn_nope_gain[i], qn_rope_gain[i],
                  kn_nope_gain[i], kn_rope_gain[i], pos)
        mix = jnp.concatenate([o_a, o_m], axis=-1).astype(h.dtype)
        h = h + mix @ w_out[i]
        h = h + hier_moe(rms_norm(h, norm_ffn_gain[i]), w_router_group[i], b_router_group[i],
                         w_router_expert[i], b_router_expert[i], w_exp_gate[i], w_exp_up[i],
                         w_exp_down[i])
        e = rms_norm(p[i] @ w_ple_proj[i], ple_norm_gain[i]).astype(jnp.float32)
        g = jax.nn.sigmoid((h @ w_ple_gate[i] + b_ple_gate[i]).astype(jnp.float32))
        h = h + (g * e).astype(h.dtype)
    return h
```

```python
import contextlib
import math
import numpy as np
import ml_dtypes
import concourse.bass as bass
import concourse.mybir as mybir
from concourse.bass_utils import run_bass_kernel_spmd

F32 = mybir.dt.float32
BF16 = mybir.dt.bfloat16
I32 = mybir.dt.int32
U8 = mybir.dt.uint8
ALU = mybir.AluOpType
AF = mybir.ActivationFunctionType
AX = mybir.AxisListType

NCORES = 8
D = 1024
S = 2048
NSEQ = 4
NTOK = NSEQ * S
NT = NTOK // 128
TPS = S // 128
INC = 2208
PLE = 256
NE = 32
FF = 512
CAP = 768
NSLOT = NE * CAP
TRASH = NSLOT
MTW = 2176
EPS = 1e-6
ENGS = ("pe", "act", "dve", "pool", "sp")


class _Op:
    __slots__ = ("eng", "fn", "deps", "is_dma", "idx", "signal", "semi", "semval", "dsem", "dval", "dprev")

    def __init__(self, eng, fn, is_dma, idx):
        self.eng = eng
        self.fn = fn
        self.deps = []
        self.is_dma = is_dma
        self.idx = idx
        self.signal = False
        self.semi = 0
        self.semval = 0
        self.dsem = None
        self.dval = 0
        self.dprev = 0


class Prog:
    EPOCH = 20000
    NDMA = 16

    def __init__(self, nc):
        self.nc = nc
        self.ops = []
        self.last_w = {}
        self.readers = {}
        self.bar_deps = None
        self.bar_seen = set()
        self.last_eng = {}
        self.dma_since = []

    def _add(self, eng, fn, reads, writes, is_dma):
        op = _Op(eng, fn, is_dma, len(self.ops))
        deps = {}
        for r in reads:
            w = self.last_w.get(r)
            if w is not None:
                deps[w.idx] = w
        for w_ in writes:
            w = self.last_w.get(w_)
            if w is not None:
                deps[w.idx] = w
            for rd in self.readers.get(w_, ()):
                deps[rd.idx] = rd
        for r in reads:
            self.readers.setdefault(r, []).append(op)
        for w_ in writes:
            self.last_w[w_] = op
            self.readers[w_] = []
        if self.bar_deps is not None and eng not in self.bar_seen:
            self.bar_seen.add(eng)
            for d in self.bar_deps:
                deps[d.idx] = d
        deps.pop(op.idx, None)
        for d in deps.values():
            if (not d.is_dma) and (not is_dma) and d.eng == "pe" and eng == "pe":
                continue
            op.deps.append(d)
        self.ops.append(op)
        if is_dma:
            self.dma_since.append(op)
        else:
            self.last_eng[eng] = op
        return op

    def op(self, eng, fn, reads=(), writes=()):
        return self._add(eng, fn, reads, writes, False)

    def dma(self, eng, fn, reads=(), writes=()):
        return self._add(eng, fn, reads, writes, True)

    def barrier(self):
        deps = list(self.last_eng.values()) + list(self.dma_since)
        self.bar_deps = deps
        self.bar_seen = set()
        self.last_w = {}
        self.readers = {}

    def emit(self, final_wait_ops=()):
        nc = self.nc
        ops = self.ops
        for o in ops:
            for d in o.deps:
                d.signal = True
        for o in final_wait_ops:
            o.signal = True
        cnt = {e: 0 for e in ENGS}
        for o in ops:
            if o.is_dma:
                continue
            if o.signal:
                c = cnt[o.eng]
                o.semi = c // self.EPOCH
                o.semval = c % self.EPOCH + 1
                cnt[o.eng] = c + 1
        nsem = {e: (cnt[e] + self.EPOCH - 1) // self.EPOCH for e in ENGS}
        dcount = 0
        dlast = [0] * self.NDMA
        for o in ops:
            if o.is_dma:
                k = dcount % self.NDMA
                dcount += 1
                o.dsem = k
                o.dprev = dlast[k]
                dlast[k] += 16
                o.dval = dlast[k]
        with contextlib.ExitStack() as st:
            csem = {e: [st.enter_context(nc.semaphore(f"c_{e}_{i}")) for i in range(nsem[e])] for e in ENGS}
            dsem = [st.enter_context(nc.semaphore(f"d_{i}")) for i in range(self.NDMA)]
            block = st.enter_context(nc.Block())
            per_eng = {e: [o for o in ops if o.eng == e] for e in ENGS}
            finals = list(final_wait_ops)

            def run(e, engobj):
                known_c = {}
                known_d = {}

                def wait_for(d):
                    if d.is_dma:
                        if known_d.get(d.dsem, 0) >= d.dval:
                            return
                        engobj.wait_ge(dsem[d.dsem], d.dval)
                        known_d[d.dsem] = d.dval
                    else:
                        key = (d.eng, d.semi)
                        if known_c.get(key, 0) >= d.semval:
                            return
                        engobj.wait_ge(csem[d.eng][d.semi], d.semval)
                        known_c[key] = d.semval

                def reduce_deps(deps):
                    best = {}
                    for d in deps:
                        k = ("d", d.dsem) if d.is_dma else ("c", d.eng, d.semi)
                        v = d.dval if d.is_dma else d.semval
                        if k not in best or v > best[k][0]:
                            best[k] = (v, d)
                    return [bd[1] for bd in best.values()]

                for o in per_eng[e]:
                    for d in sorted(reduce_deps(o.deps), key=lambda z: z.idx):
                        wait_for(d)
                    if o.is_dma:
                        if o.dprev > 0 and known_d.get(o.dsem, 0) < o.dprev:
                            engobj.wait_ge(dsem[o.dsem], o.dprev)
                            known_d[o.dsem] = o.dprev
                        ins = o.fn(engobj)
                        ins.then_inc(dsem[o.dsem], 16)
                    else:
                        ins = o.fn(engobj)
                        if o.signal:
                            ins.then_inc(csem[o.eng][o.semi], 1)
                if e == "sp":
                    for o in reduce_deps(finals):
                        wait_for(o)

            @block.tensor
            def _(eng):
                run("pe", eng)

            @block.scalar
            def _(eng):
                run("act", eng)

            @block.vector
            def _(eng):
                run("dve", eng)

            @block.gpsimd
            def _(eng):
                run("pool", eng)

            @block.sync
            def _(eng):
                run("sp", eng)


class Builder:
    def __init__(self, debug=False, stop_after=99):
        self.debug = debug
        self.stop_after = stop_after
        self.nc = bass.Bass("TRN2", target_bir_lowering=False)
        self.p = Prog(self.nc)
        self.arena_off = 0
        self.rr = 0
        self.out_ops = []

    def din(self, name, shape, dt=F32):
        return self.nc.dram_tensor(name, list(shape), dt, kind="ExternalInput").ap()

    def dscr(self, name, shape, dt, out=False):
        kind = "ExternalOutput" if (out or (self.debug and name in self.debug)) else "Internal"
        return self.nc.dram_tensor(name, list(shape), dt, kind=kind).ap()

    def sb(self, shape, dt):
        esz = {F32: 4, BF16: 2, I32: 4}[dt]
        n = 1
        for s in shape[1:]:
            n *= s
        nbytes = (n * esz + 63) // 64 * 64
        off = self.arena_off
        self.arena_off += nbytes
        assert self.arena_off <= self.arena_bytes, ("SBUF arena overflow", self.arena_off)
        v = self.arena[:, off:off + n * esz].bitcast(dt)
        if len(shape) == 3:
            v = v.rearrange("p (a b) -> p a b", a=shape[1])
        elif len(shape) == 4:
            v = v.rearrange("p (a b c) -> p a b c", a=shape[1], b=shape[2])
        return v

    def DMA(self, q, out, in_, r, w):
        o = self.p.dma(q, lambda e: e.dma_start(out=out, in_=in_), reads=r, writes=w)
        if "out" in w or (self.debug and any(k in self.debug for k in w)):
            self.out_ops.append(o)
        return o

    def MM(self, out, lhsT, rhs, start, stop, r, w):
        return self.p.op("pe", lambda e: e.matmul(out, lhsT=lhsT, rhs=rhs, start=start, stop=stop,
                                                  skip_group_check=True), reads=r, writes=w)

    def TR(self, out, in_, ident, r, w):
        return self.p.op("pe", lambda e: e.transpose(out=out, in_=in_, identity=ident), reads=r, writes=w)

    def ACT(self, out, in_, func, r, w, bias=None, scale=None, accum=None):
        kw = {}
        if bias is not None:
            kw["bias"] = bias
        if scale is not None:
            kw["scale"] = scale
        if accum is not None:
            kw["accum_out"] = accum
        return self.p.op("act", lambda e: e.activation(out=out, in_=in_, func=func, **kw), reads=r, writes=w)

    def CP(self, eng, out, in_, r, w):
        if eng == "act":
            return self.p.op("act", lambda e: e.copy(out=out, in_=in_), reads=r, writes=w)
        return self.p.op(eng, lambda e: e.tensor_copy(out=out, in_=in_), reads=r, writes=w)

    def TT(self, eng, out, in0, in1, op, r, w):
        return self.p.op(eng, lambda e: e.tensor_tensor(out=out, in0=in0, in1=in1, op=op), reads=r, writes=w)

    def TS(self, eng, out, in0, s1, s2, op0, op1, r, w):
        if op1 is None:
            return self.p.op(eng, lambda e: e.tensor_scalar(out=out, in0=in0, scalar1=s1, scalar2=None, op0=op0),
                             reads=r, writes=w)
        return self.p.op(eng, lambda e: e.tensor_scalar(out=out, in0=in0, scalar1=s1, scalar2=s2, op0=op0, op1=op1),
                         reads=r, writes=w)

    def STT(self, eng, out, in0, scalar, in1, op0, op1, r, w):
        return self.p.op(eng, lambda e: e.scalar_tensor_tensor(out=out, in0=in0, scalar=scalar, in1=in1,
                                                               op0=op0, op1=op1), reads=r, writes=w)

    def RED(self, eng, out, in_, op, r, w):
        return self.p.op(eng, lambda e: e.tensor_reduce(out=out, in_=in_, axis=AX.X, op=op), reads=r, writes=w)

    def RCP(self, out, in_, r, w):
        return self.p.op("dve", lambda e: e.reciprocal(out=out, in_=in_), reads=r, writes=w)

    def MS(self, eng, out, val, w, r=()):
        return self.p.op(eng, lambda e: e.memset(out, val), reads=r, writes=w)

    def rstd(self, ss, n, nm, r_extra=()):
        self.ACT(ss, ss, AF.Ln, [nm, "epsc"] + list(r_extra), [nm], bias=self.epsc[:, 0:1], scale=1.0 / n)
        self.ACT(ss, ss, AF.Exp, [nm], [nm], scale=-0.5)

    def build(self):
        nc = self.nc
        dbg = self.debug
        x = self.din("x", [NTOK, D])
        pin = self.din("p", [NTOK, PLE])
        w_in = self.din("w_in", [D, INC])
        w_q_up = self.din("w_q_up", [384, 768])
        w_kv_up = self.din("w_kv_up", [256, 1024])
        w_out = self.din("w_out", [D, D])
        wr = self.din("wr", [D, 36])
        br = self.din("br", [1, 36])
        self.br_d = br
        w_gate = self.din("w_exp_gate", [NE, D, FF])
        w_up = self.din("w_exp_up", [NE, D, FF])
        w_down = self.din("w_exp_down", [NE, FF, D])
        w_ple_proj = self.din("w_ple_proj", [PLE, D])
        w_ple_gate = self.din("w_ple_gate", [D, D])
        b_ple_gate = self.din("b_ple_gate", [1, D])
        gvec = self.din("gvec", [1, 4096])
        cst = self.din("cst", [128, 1024])
        bias_toep = self.din("bias_toep", [8, 128, MTW])
        mult_toep = self.din("mult_toep", [128, MTW])
        out = self.nc.dram_tensor("out", [NTOK, D], F32, kind="ExternalOutput").ap()
        qTa_d = self.dscr("qTa_d", [512, NTOK], BF16)
        kTa_d = self.dscr("kTa_d", [512, NTOK], BF16)
        va_d = self.dscr("va_d", [NTOK, 512], BF16)
        qTm_d = self.dscr("qTm_d", [768, NTOK], BF16)
        kTm_d = self.dscr("kTm_d", [768, NTOK], BF16)
        vm_d = self.dscr("vm_d", [NTOK, 512], BF16)
        mt_d = self.dscr("mt_d", [8, 128, MTW], BF16)
        h1_d = self.dscr("h1_d", [NTOK, D], F32)
        xg_d = self.dscr("xg_d", [NSLOT + 128, D], BF16)
        y_d = self.dscr("y_d", [NSLOT + 128, D], F32)
        rt_d = self.dscr("rt_d", [128, NT * 4], F32, out=False)

        with contextlib.ExitStack() as st:
            self.arena_bytes = 200 * 1024
            self.arena = st.enter_context(nc.sbuf_tensor("arena", [128, self.arena_bytes], U8))
            PS = [st.enter_context(nc.psum_tensor(f"ps{i}", [128, 512], F32)) for i in range(8)]
            PSb = [t[:, :].bitcast(BF16) for t in PS]
            PS = [t[:, :] for t in PS]
            self.PS, self.PSb = PS, PSb

            C = self.sb([128, 1024], F32)
            self.C = C
            ident_f = C[:, 0:128]
            eoff = C[:, 384:416]
            cos_t = C[:, 512:768].rearrange("p (t i) -> p t i", t=16)
            sin_t = C[:, 768:1024].rearrange("p (t i) -> p t i", t=16)
            Cb = self.sb([128, 512], BF16)
            ident_b = Cb[:, 0:128]
            causal_b = Cb[:, 128:256]
            ustrict_b = Cb[:, 256:384]
            ones_b = Cb[:, 384:512]
            onesf = self.sb([128, 128], F32)
            G = self.sb([128, 4096], F32)
            self.epsc = self.sb([128, 1], F32)
            g_mix = G[:, 0:1024]
            g_ffn = G[:, 1024:2048]
            g_ple = G[:, 2048:3072]
            g_qa = G[:, 3072:3136]
            g_ka = G[:, 3136:3200]
            g_cq = G[:, 3200:3584]
            g_ckv = G[:, 3584:3840]
            g_qn = G[:, 3840:3904]
            g_qr = G[:, 3904:3936]
            g_kn = G[:, 3936:4000]
            g_kr = G[:, 4000:4032]
            slots_all = self.sb([128, NT, 2], I32)
            wts_all = self.sb([128, NT, 2], F32)
            acc_b = self.sb([128, 32], BF16)
            wr_sb = self.sb([128, 8, 36], F32)
            br_sb = self.sb([1, 36], F32)
            persist_mark = self.arena_off

            p = self.p
            self.DMA("sp", C, cst, [], ["C"])
            self.DMA("sp", G, gvec[0:1, :].partition_broadcast(128)[:, 0, :], [], ["G"])
            self.DMA("sp", wr_sb, wr.rearrange("(c p) n -> p c n", p=128), [], ["wr"])
            self.DMA("sp", br_sb[0:1, :], br, [], ["br"])
            self.MS("pool", self.epsc, EPS, ["epsc"])
            self.MS("pool", onesf, 1.0, ["onesf"])
            self.MS("pool", acc_b, 0.0, ["acc"])
            self.MS("pool", Cb[:, 384:512], 1.0, ["Cb"])
            self.CP("dve", Cb[:, 0:384], C[:, 0:384], ["C", "Cb"], ["Cb"])
            self.MS("pool", slots_all, 0, ["slots"])
            self.MS("pool", wts_all, 0.0, ["wts"])

            w_in_b = self.sb([128, 8, INC], BF16)
            wq_b = self.sb([128, 3, 768], BF16)
            wkv_b = self.sb([128, 2, 1024], BF16)
            p1_mark = self.arena_off
            stg = [self.sb([128, INC], F32) for _ in range(2)]
            for c in range(8):
                s_ = stg[c % 2]
                self.DMA("sp", s_, w_in[c * 128:(c + 1) * 128, :], [], [f"stg{c%2}"])
                self.CP(("pool", "dve")[c % 2], w_in_b[:, c, :], s_, [f"stg{c%2}"], ["w_in_b"])
            for c in range(3):
                s_ = stg[c % 2]
                self.DMA("sp", s_[:, 0:768], w_q_up[c * 128:(c + 1) * 128, :], [], [f"stg{c%2}"])
                self.CP(("pool", "dve")[c % 2], wq_b[:, c, :], s_[:, 0:768], [f"stg{c%2}"], ["wq_b"])
            for c in range(2):
                s_ = stg[c % 2]
                self.DMA("sp", s_[:, 0:1024], w_kv_up[c * 128:(c + 1) * 128, :], [], [f"stg{c%2}"])
                self.CP(("pool", "dve")[c % 2], wkv_b[:, c, :], s_[:, 0:1024], [f"stg{c%2}"], ["wkv_b"])
            mult_sb = self.sb([128, MTW], F32)
            self.DMA("sp", mult_sb, mult_toep, [], ["mult"])
            mtb = [self.sb([128, MTW], BF16) for _ in range(2)]
            for h in range(8):
                s_ = stg[h % 2]
                self.DMA("sp", s_[:, 0:MTW], bias_toep[h], [], [f"stg{h%2}"])
                self.ACT(s_[:, 0:MTW], s_[:, 0:MTW], AF.Exp, [f"stg{h%2}"], [f"stg{h%2}"])
                self.TT("dve", mtb[h % 2], s_[:, 0:MTW], mult_sb, ALU.mult, [f"stg{h%2}", "mult"], [f"mtb{h%2}"])
                self.DMA("pool", mt_d[h], mtb[h % 2], [f"mtb{h%2}"], ["mt_d"])
            zt = self.sb([128, 7, 1024], BF16)
            ztf = self.sb([128, 1024], F32)
            self.MS("pool", zt, 0.0, ["zt"])
            self.MS("pool", ztf, 0.0, ["ztf"])
            xg_v = xg_d.rearrange("(r p) n -> p r n", p=128)
            nrt = (NSLOT + 128) // 128
            for i in range(0, nrt, 7):
                n_ = min(7, nrt - i)
                self.DMA("pool", xg_v[:, i:i + n_, :], zt[:, 0:n_, :], ["zt"], ["xg_d"])
            self.DMA("pool", y_d[NSLOT:NSLOT + 128, :], ztf, ["ztf"], ["y_d"])
            p.barrier()
            self.arena_off = p1_mark

            if self.stop_after >= 1:
                self.phase1(x, w_in_b, wq_b, wkv_b, qTa_d, kTa_d, va_d, qTm_d, kTm_d, vm_d,
                            ident_b, g_mix, g_qa, g_ka, g_cq, g_ckv, g_qn, g_qr, g_kn, g_kr, cos_t, sin_t)
            p.barrier()
            self.arena_off = persist_mark
            if self.stop_after >= 2:
                self.phase2(x, w_out, qTa_d, kTa_d, va_d, qTm_d, kTm_d, vm_d, mt_d, h1_d, xg_d, rt_d,
                            ident_f, ident_b, causal_b, ustrict_b, ones_b, onesf, eoff, g_ffn,
                            wr_sb, br_sb, slots_all, wts_all, acc_b)
            p.barrier()
            self.arena_off = persist_mark
            if self.stop_after >= 3:
                self.phase3(w_gate, w_up, w_down, xg_d, y_d, ident_b)
            p.barrier()
            self.arena_off = persist_mark
            if self.stop_after >= 4:
                self.phase4(pin, w_ple_proj, w_ple_gate, b_ple_gate, h1_d, y_d, out, ident_b, ones_b, g_ple,
                            slots_all, wts_all)
            p.emit(final_wait_ops=self.out_ops)
        return nc

    def headnorm(self, src_ps, nh, hd, stride, off, gain, dst, dst_nm, ps_nm, n_real, tag, sqt, ssum, tmpt):
        v = src_ps
        sq, sqk = sqt
        tmp, tmpk = tmpt
        self.ACT(sq[:, 0:nh * stride], v, AF.Square, [ps_nm], [sqk])
        sqv = sq[:, 0:nh * stride].rearrange("p (h d) -> p h d", h=nh)[:, :, off:off + hd]
        self.RED("dve", ssum, sqv, ALU.add, [sqk], [tag + "ss"])
        self.rstd(ssum, n_real, tag + "ss")
        vv = v.rearrange("p (h d) -> p h d", h=nh)[:, :, off:off + hd]
        self.TT("dve", tmp, vv, ssum.unsqueeze(2).to_broadcast([128, nh, hd]), ALU.mult,
                [ps_nm, tag + "ss"], [tmpk])
        self.TT("pool", dst, tmp, gain.unsqueeze(1).to_broadcast([128, nh, hd]), ALU.mult,
                [tmpk, "G"], [dst_nm])

    def phase1(self, x, w_in_b, wq_b, wkv_b, qTa_d, kTa_d, va_d, qTm_d, kTm_d, vm_d,
               ident_b, g_mix, g_qa, g_ka, g_cq, g_ckv, g_qn, g_qr, g_kn, g_kr, cos_t, sin_t):
        PS, PSb = self.PS, self.PSb
        sb = self.sb
        junk = sb([128, D], BF16)
        NS = 2
        B = []
        for si in range(NS):
            d = {}
            d["xt"] = [sb([128, D], F32) for _ in range(2)]
            d["xn"] = sb([128, D], BF16)
            d["xT"] = sb([128, 8, 128], BF16)
            d["sq"] = [sb([128, 1024], F32) for _ in range(2)]
            d["tmp"] = [sb([128, 8, 64], F32) for _ in range(2)]
            d["ss"] = [sb([128, 8], F32) for _ in range(10)]
            d["qan"] = sb([128, 8, 64], BF16)
            d["kan"] = sb([128, 8, 64], BF16)
            d["cqn"] = sb([128, 384], BF16)
            d["cqT"] = sb([128, 3, 128], BF16)
            d["ckvn"] = sb([128, 256], BF16)
            d["ckvT"] = sb([128, 2, 128], BF16)
            d["qm"] = sb([128, 8, 96], BF16)
            d["km"] = sb([128, 8, 96], BF16)
            d["rp"] = sb([128, 8, 32], F32)
            d["r1"] = sb([128, 8, 16], F32)
            d["r2"] = sb([128, 8, 16], F32)
            d["krn"] = sb([128, 32], F32)
            d["kro"] = sb([128, 32], BF16)
            d["k1"] = sb([128, 16], F32)
            d["k2"] = sb([128, 16], F32)
            d["bank"] = 0
            d["sqc"] = 0
            d["tmc"] = 0
            d["xc"] = 0
            B.append(d)
        qaT_s = [sb([128, 4, 256], BF16) for _ in range(2)]
        kaT_s = [sb([128, 4, 256], BF16) for _ in range(2)]
        qmT_s = [sb([128, 8, 256], BF16) for _ in range(2)]
        kmT_s = [sb([128, 8, 256], BF16) for _ in range(2)]
        va_s = [sb([128, 2, 512], BF16) for _ in range(2)]
        vm_s = [sb([128, 2, 8, 64], BF16) for _ in range(2)]
        chunks = [(0, 512), (512, 512), (1024, 512), (1536, 384), (1920, 288)]

        def tile_gen(t, si):
            d = B[si]
            K_ = f"s{si}"
            m, ti = divmod(t, 2)
            mi = m % 2
            pj = t % TPS

            def nb():
                b_ = 4 * si + d["bank"]
                d["bank"] = (d["bank"] + 1) % 4
                return b_

            def nsq():
                k = d["sqc"] % 2
                d["sqc"] += 1
                return d["sq"][k], f"{K_}sq{k}"

            def ntmp():
                k = d["tmc"] % 2
                d["tmc"] += 1
                return d["tmp"][k], f"{K_}tmp{k}"

            ssh = d["ss"]
            xi = d["xc"] % 2
            d["xc"] += 1
            X = d["xt"][xi]
            xk = f"{K_}xt{xi}"
            self.DMA("sp", X, x[t * 128:(t + 1) * 128, :], [], [xk])
            s1 = ssh[8][:, 0:1]
            s1k = f"{K_}ss1"
            self.MS("pool", s1, 0.0, [s1k])
            yield
            self.ACT(junk, X, AF.Square, [xk, s1k], [s1k], accum=s1)
            self.rstd(s1, D, s1k)
            yield
            XN, xnk = d["xn"], f"{K_}xn"
            self.STT("dve", XN, X, s1, g_mix, ALU.mult, ALU.mult, [xk, s1k, "G"], [xnk])
            yield
            b0 = nb()
            for c in range(8):
                self.TR(PSb[b0][:, c * 128:(c + 1) * 128], XN[:, c * 128:(c + 1) * 128], ident_b, [xnk, "Cb"], [f"ps{b0}"])
            yield
            XT, xtk = d["xT"], f"{K_}xT"
            self.CP("act", XT, PSb[b0][:, 0:1024].rearrange("p (c n) -> p c n", c=8), [f"ps{b0}"], [xtk])
            yield

            def proj(ci):
                c0, cw = chunks[ci]
                b_ = nb()
                for c in range(8):
                    self.MM(PS[b_][:, 0:cw], XT[:, c, :], w_in_b[:, c, c0:c0 + cw], c == 0, c == 7,
                            [xtk, "w_in_b"], [f"ps{b_}"])
                return b_

            def headnorm_g(src_ps, ps_nm, gain, dst, dst_nm, ssum, tag):
                sq, sqk = nsq()
                tmp, tmpk = ntmp()
                self.ACT(sq[:, 0:512], src_ps, AF.Square, [ps_nm], [sqk])
                yield
                self.RED("dve", ssum, sq[:, 0:512].rearrange("p (h d) -> p h d", h=8), ALU.add, [sqk], [tag])
                yield
                self.rstd(ssum, 64, tag)
                yield
                self.TT("dve", tmp, src_ps.rearrange("p (h d) -> p h d", h=8), ssum.unsqueeze(2).to_broadcast([128, 8, 64]),
                        ALU.mult, [ps_nm, tag], [tmpk])
                yield
                self.TT("pool", dst, tmp, gain.unsqueeze(1).to_broadcast([128, 8, 64]), ALU.mult, [tmpk, "G"], [dst_nm])
                yield

            def trans_pairs(src, src_nm, stage, snm):
                bt = nb()
                for c in range(4):
                    self.TR(PSb[bt][:, c * 128:(c + 1) * 128], src[:, 2 * c:2 * c + 2, :].rearrange("p a b -> p (a b)"),
                            ident_b, [src_nm, "Cb"], [f"ps{bt}"])
                yield
                self.CP("dve", stage[:, :, ti * 128:(ti + 1) * 128],
                        PSb[bt][:, 0:512].rearrange("p (c n) -> p c n", c=4), [f"ps{bt}"], [snm])
                yield

            bc0 = proj(0)
            yield
            bc1 = proj(1)
            yield
            bc2 = proj(2)
            yield
            self.CP("act", va_s[mi][:, ti, :], PS[bc2][:, 0:512], [f"ps{bc2}"], [f"vas{mi}"])
            yield
            bc3 = proj(3)
            yield
            yield from headnorm_g(PS[bc0][:, 0:512], f"ps{bc0}", g_qa, d["qan"], f"{K_}qan", ssh[0], f"{K_}ssqa")
            bc4 = proj(4)
            yield
            yield from headnorm_g(PS[bc1][:, 0:512], f"ps{bc1}", g_ka, d["kan"], f"{K_}kan", ssh[1], f"{K_}sska")
            yield from trans_pairs(d["qan"], f"{K_}qan", qaT_s[mi], f"qaTs{mi}")
            yield from trans_pairs(d["kan"], f"{K_}kan", kaT_s[mi], f"kaTs{mi}")
            s2 = ssh[9][:, 0:1]
            cqk = f"{K_}cqss"
            self.MS("pool", s2, 0.0, [cqk])
            yield
            self.ACT(junk[:, 0:384], PS[bc3][:, 0:384], AF.Square, [f"ps{bc3}", cqk], [cqk], accum=s2)
            self.rstd(s2, 384, cqk)
            yield
            cqn, cqnk = d["cqn"], f"{K_}cqn"
            self.STT("dve", cqn, PS[bc3][:, 0:384], s2, g_cq, ALU.mult, ALU.mult, [f"ps{bc3}", cqk, "G"], [cqnk])
            yield
            bt = nb()
            for c in range(3):
                self.TR(PSb[bt][:, c * 128:(c + 1) * 128], cqn[:, c * 128:(c + 1) * 128], ident_b, [cqnk, "Cb"], [f"ps{bt}"])
            yield
            cqT, cqTk = d["cqT"], f"{K_}cqT"
            self.CP("dve", cqT, PSb[bt][:, 0:384].rearrange("p (c n) -> p c n", c=3), [f"ps{bt}"], [cqTk])
            yield
            s5 = ssh[7]
            ckk = f"{K_}ckss"
            self.MS("pool", s5[:, 0:2], 0.0, [ckk])
            yield
            self.ACT(junk[:, 0:256], PS[bc4][:, 0:256], AF.Square, [f"ps{bc4}", ckk], [ckk], accum=s5[:, 0:1])
            self.ACT(junk[:, 256:288], PS[bc4][:, 256:288], AF.Square, [f"ps{bc4}", ckk], [ckk], accum=s5[:, 1:2])
            self.rstd(s5[:, 0:1], 256, ckk)
            self.rstd(s5[:, 1:2], 32, ckk)
            yield
            ckvn, ckvnk = d["ckvn"], f"{K_}ckvn"
            krn, kro, k1, k2 = d["krn"], d["kro"], d["k1"], d["k2"]
            self.STT("dve", ckvn, PS[bc4][:, 0:256], s5[:, 0:1], g_ckv, ALU.mult, ALU.mult, [f"ps{bc4}", ckk, "G"], [ckvnk])
            self.STT("dve", krn, PS[bc4][:, 256:288], s5[:, 1:2], g_kr, ALU.mult, ALU.mult, [f"ps{bc4}", ckk, "G"], [f"{K_}krn"])
            yield
            bq0, bq1 = nb(), nb()
            for c in range(3):
                self.MM(PS[bq0][:, 0:480], cqT[:, c, :], wq_b[:, c, 0:480], c == 0, c == 2, [cqTk, "wq_b"], [f"ps{bq0}"])
            for c in range(3):
                self.MM(PS[bq1][:, 0:288], cqT[:, c, :], wq_b[:, c, 480:768], c == 0, c == 2, [cqTk, "wq_b"], [f"ps{bq1}"])
            yield
            ck, sk = cos_t[:, pj, :], sin_t[:, pj, :]
            self.TT("dve", k1, krn[:, 0:16], ck, ALU.mult, [f"{K_}krn", "C"], [f"{K_}k1"])
            self.TT("dve", k2, krn[:, 16:32], sk, ALU.mult, [f"{K_}krn", "C"], [f"{K_}k2"])
            self.TT("dve", kro[:, 0:16], k1, k2, ALU.subtract, [f"{K_}k1", f"{K_}k2"], [f"{K_}kro"])
            self.TT("dve", k1, krn[:, 16:32], ck, ALU.mult, [f"{K_}krn", "C", f"{K_}kro"], [f"{K_}k1"])
            self.TT("dve", k2, krn[:, 0:16], sk, ALU.mult, [f"{K_}krn", "C", f"{K_}kro"], [f"{K_}k2"])
            self.TT("dve", kro[:, 16:32], k1, k2, ALU.add, [f"{K_}k1", f"{K_}k2"], [f"{K_}kro"])
            km, kmk = d["km"], f"{K_}km"
            self.CP("dve", km[:, :, 64:96], kro.unsqueeze(1).to_broadcast([128, 8, 32]), [f"{K_}kro"], [kmk])
            yield
            qm, qmk = d["qm"], f"{K_}qm"
            rp, rpk = d["rp"], f"{K_}rp"
            for (bq, h0, nh, ia, ib) in ((bq0, 0, 5, 3, 5), (bq1, 5, 3, 4, 6)):
                tag = f"{K_}qm{h0}"
                src = PS[bq][:, 0:nh * 96]
                sq, sqk = nsq()
                tmpa, tmpk = ntmp()
                self.ACT(sq[:, 0:nh * 96], src, AF.Square, [f"ps{bq}"], [sqk])
                yield
                sqv = sq[:, 0:nh * 96].rearrange("p (h d) -> p h d", h=nh)
                sn = ssh[ia][:, 0:nh]
                sr = ssh[ib][:, 0:nh]
                self.RED("dve", sn, sqv[:, :, 0:64], ALU.add, [sqk], [tag + "sn"])
                self.RED("dve", sr, sqv[:, :, 64:96], ALU.add, [sqk], [tag + "sr"])
                yield
                self.rstd(sn, 64, tag + "sn")
                self.rstd(sr, 32, tag + "sr")
                yield
                sv = src.rearrange("p (h d) -> p h d", h=nh)
                self.TT("dve", tmpa[:, 0:nh, :], sv[:, :, 0:64], sn.unsqueeze(2).to_broadcast([128, nh, 64]), ALU.mult,
                        [f"ps{bq}", tag + "sn"], [tmpk])
                self.TT("dve", rp[:, h0:h0 + nh, :], sv[:, :, 64:96], sr.unsqueeze(2).to_broadcast([128, nh, 32]), ALU.mult,
                        [f"ps{bq}", tag + "sr"], [rpk])
                yield
                self.TT("pool", qm[:, h0:h0 + nh, 0:64], tmpa[:, 0:nh, :], g_qn.unsqueeze(1).to_broadcast([128, nh, 64]),
                        ALU.mult, [tmpk, "G"], [qmk])
                yield
            r1, r2 = d["r1"], d["r2"]
            cosb = cos_t[:, pj, :].unsqueeze(1).to_broadcast([128, 8, 16])
            sinb = sin_t[:, pj, :].unsqueeze(1).to_broadcast([128, 8, 16])
            self.TT("pool", rp, rp, g_qr.unsqueeze(1).to_broadcast([128, 8, 32]), ALU.mult, [rpk, "G"], [rpk])
            self.TT("pool", r1, rp[:, :, 0:16], cosb, ALU.mult, [rpk, "C"], [f"{K_}r1"])
            self.TT("pool", r2, rp[:, :, 16:32], sinb, ALU.mult, [rpk, "C"], [f"{K_}r2"])
            self.TT("pool", qm[:, :, 64:80], r1, r2, ALU.subtract, [f"{K_}r1", f"{K_}r2"], [qmk])
            self.TT("pool", r1, rp[:, :, 16:32], cosb, ALU.mult, [rpk, "C", qmk], [f"{K_}r1"])
            self.TT("pool", r2, rp[:, :, 0:16], sinb, ALU.mult, [rpk, "C", qmk], [f"{K_}r2"])
            self.TT("pool", qm[:, :, 80:96], r1, r2, ALU.add, [f"{K_}r1", f"{K_}r2"], [qmk])
            yield
            bt = nb()
            for h in range(8):
                self.TR(PSb[bt][0:96, h * 128:(h + 1) * 128], qm[:, h, :], ident_b, [qmk, "Cb"], [f"ps{bt}"])
            yield
            self.CP("act", qmT_s[mi][0:96, :, ti * 128:(ti + 1) * 128],
                    PSb[bt][0:96, 0:1024].rearrange("p (c n) -> p c n", c=8), [f"ps{bt}"], [f"qmTs{mi}"])
            yield
            bt = nb()
            for c in range(2):
                self.TR(PSb[bt][:, c * 128:(c + 1) * 128], ckvn[:, c * 128:(c + 1) * 128], ident_b, [ckvnk, "Cb"], [f"ps{bt}"])
            yield
            ckvT, ckvTk = d["ckvT"], f"{K_}ckvT"
            self.CP("dve", ckvT, PSb[bt][:, 0:256].rearrange("p (c n) -> p c n", c=2), [f"ps{bt}"], [ckvTk])
            yield
            bk0, bk1 = nb(), nb()
            for half, bk in ((0, bk0), (1, bk1)):
                for c in range(2):
                    self.MM(PS[bk][:, 0:512], ckvT[:, c, :], wkv_b[:, c, half * 512:(half + 1) * 512], c == 0, c == 1,
                            [ckvTk, "wkv_b"], [f"ps{bk}"])
            yield
            for half, bk in ((0, bk0), (1, bk1)):
                tag = f"{K_}kv{half}"
                src = PS[bk][:, 0:512]
                h0 = half * 4
                sq, sqk = nsq()
                tmpa, tmpk = ntmp()
                self.ACT(sq[:, 0:512], src, AF.Square, [f"ps{bk}"], [sqk])
                yield
                sqv = sq[:, 0:512].rearrange("p (h d) -> p h d", h=4)
                sn = ssh[2][:, half * 4:half * 4 + 4]
                self.RED("dve", sn, sqv[:, :, 0:64], ALU.add, [sqk], [tag + "sn"])
                yield
                self.rstd(sn, 64, tag + "sn")
                sv = src.rearrange("p (h d) -> p h d", h=4)
                self.CP("act", vm_s[mi][:, ti, h0:h0 + 4, :], sv[:, :, 64:128], [f"ps{bk}", tag + "sn"], [f"vms{mi}"])
                yield
                self.TT("dve", tmpa[:, 0:4, :], sv[:, :, 0:64], sn.unsqueeze(2).to_broadcast([128, 4, 64]), ALU.mult,
                        [f"ps{bk}", tag + "sn", f"vms{mi}"], [tmpk])
                yield
                self.TT("pool", km[:, h0:h0 + 4, 0:64], tmpa[:, 0:4, :], g_kn.unsqueeze(1).to_broadcast([128, 4, 64]),
                        ALU.mult, [tmpk, "G"], [kmk])
                yield
            bt = nb()
            for h in range(8):
                self.TR(PSb[bt][0:96, h * 128:(h + 1) * 128], km[:, h, :], ident_b, [kmk, "Cb"], [f"ps{bt}"])
            yield
            self.CP("act", kmT_s[mi][0:96, :, ti * 128:(ti + 1) * 128],
                    PSb[bt][0:96, 0:1024].rearrange("p (c n) -> p c n", c=8), [f"ps{bt}"], [f"kmTs{mi}"])
            yield
            if ti == 1:
                cs = slice(m * 256, (m + 1) * 256)
                self.DMA("pool", qTa_d.rearrange("(c q) n -> q c n", q=128)[:, :, cs], qaT_s[mi], [f"qaTs{mi}"], ["qTa_d"])
                self.DMA("pool", kTa_d.rearrange("(c q) n -> q c n", q=128)[:, :, cs], kaT_s[mi], [f"kaTs{mi}"], ["kTa_d"])
                self.DMA("pool", qTm_d.rearrange("(h d) n -> d h n", d=96)[:, :, cs], qmT_s[mi][0:96], [f"qmTs{mi}"], ["qTm_d"])
                self.DMA("pool", kTm_d.rearrange("(h d) n -> d h n", d=96)[:, :, cs], kmT_s[mi][0:96], [f"kmTs{mi}"], ["kTm_d"])
                self.DMA("pool", va_d[cs, :].rearrange("(t p) n -> p t n", p=128), va_s[mi], [f"vas{mi}"], ["va_d"])
                self.DMA("pool", vm_d[cs, :].rearrange("(t p) n -> p t n", p=128),
                         vm_s[mi].rearrange("p t h d -> p t (h d)"), [f"vms{mi}"], ["vm_d"])

        for m in range(NT // 2):
            gens = [tile_gen(2 * m, 0), tile_gen(2 * m + 1, 1)]
            alive = [True, True]
            while any(alive):
                for gi_, g_ in enumerate(gens):
                    if alive[gi_]:
                        try:
                            next(g_)
                        except StopIteration:
                            alive[gi_] = False

    def phase2(self, x, w_out, qTa_d, kTa_d, va_d, qTm_d, kTm_d, vm_d, mt_d, h1_d, xg_d, rt_d,
               ident_f, ident_b, causal_b, ustrict_b, ones_b, onesf, eoff, g_ffn,
               wr_sb, br_sb, slots_all, wts_all, acc_b):
        PS, PSb = self.PS, self.PSb
        sb = self.sb
        w_out_b = sb([128, 8, D], BF16)
        br_d = self.br_d
        stg = [sb([128, D], F32) for _ in range(2)]
        for c in range(8):
            self.DMA("sp", stg[c % 2], w_out[c * 128:(c + 1) * 128, :], [], [f"wstg{c%2}"])
            self.CP(("pool", "dve")[c % 2], w_out_b[:, c, :], stg[c % 2], [f"wstg{c%2}"], ["w_out_b"])
        qT = [sb([128, S], BF16) for _ in range(2)]
        kT = [sb([128, S], BF16) for _ in range(2)]
        vaug = [sb([128, TPS, 128], BF16) for _ in range(2)]
        mt = [sb([128, MTW], BF16) for _ in range(2)]
        mixTs = [sb([128, 8, S], BF16) for _ in range(2)]
        Pt = [sb([128, 512], BF16) for _ in range(4)]
        rden = [sb([128, 512], F32) for _ in range(2)]
        xr = [sb([128, D], F32) for _ in range(2)]
        h1 = [sb([128, D], F32) for _ in range(3)]
        NXB = 7
        xn2b = [sb([128, D], BF16) for _ in range(NXB)]
        xn2T = [sb([128, 8, 128], BF16) for _ in range(2)]
        junk = sb([128, D], BF16)
        junk4 = sb([128, 4], F32)
        ss2 = [sb([128, 1], F32) for _ in range(2)]
        Ls = [sb([128, 36], F32) for _ in range(3)]
        NRT = 6
        rt = [sb([128, 8], F32) for _ in range(NRT)]
        gm = sb([128, 4], F32)
        pen = sb([128, 4], F32)
        Em = sb([128, 4, 8], F32)
        top8 = sb([128, 8], F32)
        OH = [sb([128, 2, 32], F32) for _ in range(2)]
        OHb = [sb([128, 32], BF16) for _ in range(2)]
        prod = sb([128, 2, 32], F32)
        prod2 = sb([128, 2, 32], F32)
        rk = sb([128, 2], F32)
        ek = sb([128, 2], F32)
        okk = [sb([128, 2], F32) for _ in range(2)]
        slf = sb([128, 2], F32)
        wr_b = sb([128, 8, 36], BF16)
        br_bc = sb([128, 36], F32)
        self.CP("dve", wr_b, wr_sb, ["wr"], ["wr_b"])
        self.DMA("sp", br_bc, br_d[0:1, :].partition_broadcast(128)[:, 0, :], [], ["br_bc"])
        for i in range(2):
            self.MS("pool", vaug[i], 1.0, [f"vaug{i}"])

        LA = 2
        mcount = [0]

        def head_loads(b, h, i):
            tok0 = b * S
            if h < 8:
                self.DMA("sp", qT[i][0:64, :], qTa_d[h * 64:(h + 1) * 64, tok0:tok0 + S], [], [f"qT{i}"])
                self.DMA("sp", kT[i][0:64, :], kTa_d[h * 64:(h + 1) * 64, tok0:tok0 + S], [], [f"kT{i}"])
                self.DMA("sp", vaug[i][:, :, 0:64],
                         va_d[tok0:tok0 + S, h * 64:(h + 1) * 64].rearrange("(t p) d -> p t d", p=128), [], [f"vaug{i}"])
                self.DMA("sp", mt[i], mt_d[h], [], [f"mt{i}"])
            else:
                hh = h - 8
                self.DMA("sp", qT[i][0:96, :], qTm_d[hh * 96:(hh + 1) * 96, tok0:tok0 + S], [], [f"qT{i}"])
                self.DMA("sp", kT[i][0:96, :], kTm_d[hh * 96:(hh + 1) * 96, tok0:tok0 + S], [], [f"kT{i}"])
                self.DMA("sp", vaug[i][:, :, 0:64],
                         vm_d[tok0:tok0 + S, hh * 64:(hh + 1) * 64].rearrange("(t p) d -> p t d", p=128), [], [f"vaug{i}"])

        loaded = set()

        def ensure_loaded(gidx):
            if gidx in loaded or gidx >= NSEQ * 16:
                return
            loaded.add(gidx)
            head_loads(gidx // 16, gidx % 16, gidx % 2)

        def emit_S(st):
            b, h, i, qb, kt, nk, sidx = st
            if qb == 0 and kt == 0:
                ensure_loaded(b * 16 + h)
            dq = 64 if h < 8 else 96
            q0, k0 = qb * 512, kt * 128
            qlo = max(q0, k0)
            N = q0 + 512 - qlo
            sbk = 2 + sidx % 3
            for _rep in range(2):
                self.MM(PS[sbk][:, 0:N], kT[i][0:dq, k0:k0 + 128], qT[i][0:dq, qlo:qlo + N], True, True,
                        [f"kT{i}", f"qT{i}"], [f"ps{sbk}"])

        def emit_rest(st, mixb):
            b, h, i, qb, kt, nk, sidx = st
            isA = h < 8
            scale = (64 ** -0.5) if isA else (96 ** -0.5)
            q0, k0 = qb * 512, kt * 128
            qlo = max(q0, k0)
            N = q0 + 512 - qlo
            sbk = 2 + sidx % 3
            pk = sidx % 4
            ob = qb % 2
            self.ACT(Pt[pk][:, 0:N], PS[sbk][:, 0:N], AF.Exp, [f"ps{sbk}"], [f"Pt{pk}"], scale=scale)
            if isA:
                off = qlo - k0
                eng = "dve"
                mcount[0] += 1
                self.TT(eng, Pt[pk][:, 0:N], Pt[pk][:, 0:N], mt[i][:, off:off + N], ALU.mult,
                        [f"Pt{pk}", f"mt{i}"], [f"Pt{pk}"])
            elif qlo == k0:
                self.TT("pool", Pt[pk][:, 0:128], Pt[pk][:, 0:128], causal_b, ALU.mult,
                        [f"Pt{pk}", "Cb"], [f"Pt{pk}"])
            self.MM(PS[ob][:, qlo - q0:512], vaug[i][:, kt, :], Pt[pk][:, 0:N], kt == 0, kt == nk - 1,
                    [f"vaug{i}", f"Pt{pk}"], [f"ps{ob}"])
            if kt == nk - 1:
                rd = rden[ob]
                self.ACT(rd[0:64, :], PS[ob][64:128, :], AF.Ln, [f"ps{ob}"], [f"rden{ob}"])
                self.ACT(rd[0:64, :], rd[0:64, :], AF.Exp, [f"rden{ob}"], [f"rden{ob}"], scale=-1.0)
                hp = (h % 2) * 64
                self.TT("dve", mixb[0][hp:hp + 64, h // 2, q0:q0 + 512], PS[ob][0:64, :], rd[0:64, :], ALU.mult,
                        [f"ps{ob}", f"rden{ob}"], [mixb[1]])

        def outproj_stages(b, mixb):
            mixT_, mixk = mixb

            def s0(t, j):
                i2 = t % 2
                self.DMA("sp", xr[i2], x[t * 128:(t + 1) * 128, :], [], [f"xr{i2}"])
                for n in range(2):
                    bo = 5 + n
                    for c in range(8):
                        self.MM(PS[bo][:, 0:512], mixT_[:, c, j * 128:(j + 1) * 128], w_out_b[:, c, n * 512:(n + 1) * 512],
                                c == 0, c == 7, [mixk, "w_out_b"], [f"ps{bo}"])

            def s1(t, j):
                i2, i3 = t % 2, t % 3
                for n in range(2):
                    bo = 5 + n
                    self.TT("dve", h1[i3][:, n * 512:(n + 1) * 512], PS[bo][:, 0:512], xr[i2][:, n * 512:(n + 1) * 512], ALU.add,
                            [f"ps{bo}", f"xr{i2}"], [f"h1_{i3}"])

            def s2(t, j):
                i2, i3 = t % 2, t % 3
                self.DMA("pool", h1_d[t * 128:(t + 1) * 128, :], h1[i3], [f"h1_{i3}"], ["h1_d"])
                self.MS("pool", ss2[i2], 0.0, [f"ss2_{i2}"])
                self.ACT(junk, h1[i3], AF.Square, [f"h1_{i3}", f"ss2_{i2}"], [f"ss2_{i2}"], accum=ss2[i2])
                self.ACT(ss2[i2], ss2[i2], AF.Ln, [f"ss2_{i2}", "epsc"], [f"ss2_{i2}"], bias=self.epsc[:, 0:1], scale=1.0 / D)
                self.ACT(ss2[i2], ss2[i2], AF.Exp, [f"ss2_{i2}"], [f"ss2_{i2}"], scale=-0.5)

            def s3(t, j):
                i2, i3, ix = t % 2, t % 3, t % NXB
                self.STT("dve", xn2b[ix], h1[i3], ss2[i2][:, 0:1], g_ffn, ALU.mult, ALU.mult,
                         [f"h1_{i3}", f"ss2_{i2}", "G"], [f"xn2b{ix}"])

            def s4(t, j):
                i2, ix = t % 2, t % NXB
                for c in range(8):
                    self.TR(PSb[7][:, c * 128:(c + 1) * 128], xn2b[ix][:, c * 128:(c + 1) * 128], ident_b,
                            [f"xn2b{ix}", "Cb"], ["ps7"])
                self.CP("act", xn2T[i2], PSb[7][:, 0:1024].rearrange("p (c n) -> p c n", c=8), ["ps7"], [f"xn2T{i2}"])

            def s5(t, j):
                i2, il, ir = t % 2, t % 3, t % NRT
                for c in range(8):
                    self.MM(PS[7][:, 0:36], xn2T[i2][:, c, :], wr_b[:, c, :], c == 0, c == 7, [f"xn2T{i2}", "wr_b"], ["ps7"])
                self.TT("dve", Ls[il], PS[7][:, 0:36], br_bc, ALU.add, ["ps7", "br_bc"], [f"Ls{il}"])
                self.RED("dve", rt[ir][:, 0:1], Ls[il][:, 0:4], ALU.max, [f"Ls{il}"], [f"rt{ir}"])
                self.TS("dve", rt[ir][:, 1:2], rt[ir][:, 0:1], -1.0, None, ALU.mult, None, [f"rt{ir}"], [f"rt{ir}"])
                self.MS("pool", rt[ir][:, 2:3], 0.0, [f"rt{ir}g"])

            def s6(t, j):
                il, ir = t % 3, t % NRT
                self.ACT(junk4, Ls[il][:, 0:4], AF.Exp, [f"Ls{il}", f"rt{ir}", f"rt{ir}g"], [f"rt{ir}g"],
                         bias=rt[ir][:, 1:2], scale=1.0, accum=rt[ir][:, 2:3])

            def s7(t, j):
                i2, il, ir = t % 2, t % 3, t % NRT
                R = rt[ir]
                self.RCP(R[:, 3:4], R[:, 2:3], [f"rt{ir}g"], [f"rt{ir}w"])
                self.TS("dve", gm, Ls[il][:, 0:4], R[:, 0:1], None, ALU.is_ge, None, [f"Ls{il}", f"rt{ir}"], ["gm"])
                self.TS("dve", pen, gm, -1.0, 1e30, ALU.add, ALU.mult, ["gm"], ["pen"])
                self.TT("dve", Em, Ls[il][:, 4:36].rearrange("p (g e) -> p g e", g=4), pen.unsqueeze(2).to_broadcast([128, 4, 8]),
                        ALU.add, [f"Ls{il}", "pen"], ["Em"])
                Emf = Em.rearrange("p g e -> p (g e)")
                self.p.op("dve", lambda e, o_=top8, i_=Emf: e.max(out=o_, in_=i_), reads=["Em"], writes=["top8"])
                O_ = OH[i2]
                self.TS("dve", O_[:, 0, :], Emf, top8[:, 0:1], None, ALU.is_ge, None, ["Em", "top8"], [f"OH{i2}"])
                self.TS("dve", O_[:, 1, :], Emf, top8[:, 1:2], None, ALU.is_ge, None, ["Em", "top8", f"OH{i2}"], [f"OH{i2}"])
                self.CP("dve", OHb[i2], O_[:, 1, :], [f"OH{i2}"], [f"OHb{i2}"])
                self.TT("dve", O_[:, 1, :], O_[:, 1, :], O_[:, 0, :], ALU.subtract, [f"OH{i2}", f"OHb{i2}"], [f"OH{i2}"])
                self.TT("dve", R[:, 4:5], top8[:, 0:1], top8[:, 1:2], ALU.subtract, ["top8"], [f"rt{ir}d"])

            def s8(t, j):
                i2, ir = t % 2, t % NRT
                R = rt[ir]
                O_ = OH[i2]
                self.ACT(R[:, 5:6], R[:, 4:5], AF.Exp, [f"rt{ir}d"], [f"rt{ir}s"], scale=-1.0)
                self.MM(PS[7][:, 64:96], ustrict_b, OHb[i2], True, False, ["Cb", f"OHb{i2}"], ["ps7"])
                self.MM(PS[7][:, 64:96], ones_b, acc_b, False, True, ["Cb", "acc"], ["ps7"])
                self.TT("dve", prod, O_, PS[7][:, 64:96].unsqueeze(1).to_broadcast([128, 2, 32]), ALU.mult,
                        [f"OH{i2}", "ps7"], ["prod"])
                self.RED("dve", rk, prod, ALU.add, ["prod"], ["rk"])
                self.TT("dve", prod2, O_, eoff.unsqueeze(1).to_broadcast([128, 2, 32]), ALU.mult, [f"OH{i2}", "C"], ["prod2"])
                self.RED("dve", ek, prod2, ALU.add, ["prod2"], ["ek"])
                self.TT("pool", acc_b, acc_b, OHb[i2], ALU.add, ["acc", f"OHb{i2}"], ["acc"])
                ok_ = okk[i2]
                self.TS("dve", ok_, rk, float(CAP), None, ALU.is_lt, None, ["rk"], [f"okk{i2}"])
                self.TT("dve", slf, rk, ek, ALU.add, ["rk", "ek"], ["slf"])
                self.TS("dve", slf, slf, float(-TRASH), None, ALU.add, None, ["slf"], ["slf"])
                self.TT("dve", slf, slf, ok_, ALU.mult, ["slf", f"okk{i2}"], ["slf"])
                self.TS("dve", slf, slf, float(TRASH), None, ALU.add, None, ["slf"], ["slf"])
                self.CP("dve", slots_all[:, t, :], slf, ["slf"], ["slots"])

            def s9(t, j):
                i2, ir, ix = t % 2, t % NRT, t % NXB
                R = rt[ir]
                w1 = wts_all[:, t, 0:1]
                w2 = wts_all[:, t, 1:2]
                self.TS("dve", R[:, 6:7], R[:, 5:6], 1.0, None, ALU.add, None, [f"rt{ir}s"], [f"rt{ir}s2"])
                self.RCP(R[:, 6:7], R[:, 6:7], [f"rt{ir}s2"], [f"rt{ir}s2"])
                self.TT("dve", w1, R[:, 3:4], R[:, 6:7], ALU.mult, [f"rt{ir}w", f"rt{ir}s2"], ["wts"])
                self.TT("dve", w2, R[:, 3:4], w1, ALU.subtract, [f"rt{ir}w", "wts"], ["wts"])
                self.TT("dve", wts_all[:, t, :], wts_all[:, t, :], okk[i2], ALU.mult, ["wts", f"okk{i2}"], ["wts"])
                for k in range(2):
                    idx = slots_all[:, t, k:k + 1]
                    self.p.dma("pool", lambda e, idx_=idx, src_=xn2b[ix]: e.indirect_dma_start(
                        out=xg_d, out_offset=bass.IndirectOffsetOnAxis(ap=idx_, axis=0), in_=src_, in_offset=None),
                        reads=[f"xn2b{ix}", "slots"], writes=["xg_d"])

            sts = [s0, s1, s2, s3, s4, s5, s6, s7, s8, s9]
            K_ = len(sts)
            out_ = []
            for tau in range(TPS + K_ - 1):
                for jj in reversed(range(K_)):
                    j = tau - jj
                    if 0 <= j < TPS:
                        out_.append((lambda f=sts[jj], t=b * TPS + j, j=j: f(t, j)))
            return out_

        hc = 0
        pending = []
        for b in range(NSEQ):
            mixb = (mixTs[b % 2], f"mixT{b%2}")
            steps = []
            sidx = 0
            for h in range(16):
                i = hc % 2
                hc += 1
                for qb in range(4):
                    nk = 4 * qb + 4
                    for kt in range(nk):
                        steps.append((b, h, i, qb, kt, nk, sidx))
                        sidx += 1
            n = len(steps)
            every = max(1, n // (len(pending) + 1)) if pending else 0
            for k in range(min(LA, n)):
                emit_S(steps[k])
            for k in range(n):
                if k + LA < n:
                    emit_S(steps[k + LA])
                emit_rest(steps[k], mixb)
                if steps[k][3] == 0 and steps[k][4] == 0:
                    ensure_loaded(steps[k][0] * 16 + steps[k][1] + 1)
                if pending and (k % every == every - 1):
                    pending.pop(0)()
            while pending:
                pending.pop(0)()
            pending = outproj_stages(b, mixb)
        while pending:
            pending.pop(0)()
        if self.debug:
            dbgt = sb([128, NT * 4], F32)
            self.CP("dve", dbgt[:, 0:NT * 2], slots_all.rearrange("p t k -> p (t k)"), ["slots"], ["dbgt"])
            self.CP("dve", dbgt[:, NT * 2:NT * 4], wts_all.rearrange("p t k -> p (t k)"), ["wts", "dbgt"], ["dbgt"])
            self.DMA("sp", rt_d, dbgt, ["dbgt"], ["rt_d"])

    def phase3(self, w_gate, w_up, w_down, xg_d, y_d, ident_b):
        PS, PSb = self.PS, self.PSb
        sb = self.sb
        stg = [sb([128, 4096], F32) for _ in range(3)]
        wg = [sb([128, 8, FF], BF16) for _ in range(2)]
        wu = [sb([128, 8, FF], BF16) for _ in range(2)]
        wd = [sb([128, 4, D], BF16) for _ in range(2)]
        xrow = [sb([128, D], BF16) for _ in range(3)]
        xTe = [sb([128, 8, CAP], BF16) for _ in range(2)]
        hT = [sb([128, 4, CAP], BF16) for _ in range(2)]
        sg = [sb([128, 512], F32) for _ in range(2)]
        ysb = [sb([128, D], F32) for _ in range(6)]
        nst = CAP // 128
        cnt = {"xc": 0, "yc": 0, "gub": 0}

        def w_load(e, which):
            i = e % 2
            src, dst, dn, view = ((w_gate[e], wg[i], f"wg{i}", "(c p) f -> p c f"),
                                  (w_up[e], wu[i], f"wu{i}", "(c p) f -> p c f"),
                                  (w_down[e], wd[i], f"wd{i}", "(c p) n -> p c n"))[which]
            nch = dst.shape[1]
            s3 = stg[which].rearrange("p (c f) -> p c f", c=nch)
            self.DMA("sp", s3, src.rearrange(view, p=128), [], [f"stg{which}"])

        def w_cast(e, which):
            i = e % 2
            dst, dn = ((wg[i], f"wg{i}"), (wu[i], f"wu{i}"), (wd[i], f"wd{i}"))[which]
            nch = dst.shape[1]
            s3 = stg[which].rearrange("p (c f) -> p c f", c=nch)
            if which == 0:
                self.CP("act", dst, s3, [f"stg{which}"], [dn])
            elif which == 1:
                self.CP("dve", dst, s3, [f"stg{which}"], [dn])
            else:
                self.CP("pool", dst[:, 0:1, :], s3[:, 0:1, :], [f"stg{which}"], [dn + "c0"])
                self.CP("act", dst[:, 1:2, :], s3[:, 1:2, :], [f"stg{which}"], [dn + "c1"])
                self.CP("dve", dst[:, 2:3, :], s3[:, 2:3, :], [f"stg{which}"], [dn + "c2"])
                self.CP("act", dst[:, 3:4, :], s3[:, 3:4, :], [f"stg{which}"], [dn + "c3"])

        def x_trans(e):
            i = e % 2
            for s_ in range(nst):
                k = cnt["xc"] % 3
                cnt["xc"] += 1
                r0 = e * CAP + s_ * 128
                self.DMA("sp", xrow[k], xg_d[r0:r0 + 128, :], [], [f"xrow{k}"])
                bt = cnt["xc"] % 2
                for c in range(8):
                    self.TR(PSb[bt][:, c * 128:(c + 1) * 128], xrow[k][:, c * 128:(c + 1) * 128], ident_b,
                            [f"xrow{k}", "Cb"], [f"ps{bt}"])
                self.CP(("act", "dve")[s_ % 2], xTe[i][:, :, s_ * 128:(s_ + 1) * 128],
                        PSb[bt][:, 0:1024].rearrange("p (c n) -> p c n", c=8), [f"ps{bt}"], [f"xTe{i}"])

        def gate_up(e, ffc):
            i = e % 2
            for (n0, N) in ((0, 512), (512, CAP - 512)):
                bg = 2 + (cnt["gub"] % 2) * 2
                bu = bg + 1
                cnt["gub"] += 1
                for c in range(8):
                    self.MM(PS[bg][:, 0:N], wg[i][:, c, ffc * 128:(ffc + 1) * 128], xTe[i][:, c, n0:n0 + N],
                            c == 0, c == 7, [f"wg{i}", f"xTe{i}"], [f"ps{bg}"])
                for c in range(8):
                    self.MM(PS[bu][:, 0:N], wu[i][:, c, ffc * 128:(ffc + 1) * 128], xTe[i][:, c, n0:n0 + N],
                            c == 0, c == 7, [f"wu{i}", f"xTe{i}"], [f"ps{bu}"])
                sgi = cnt["gub"] % 2
                self.ACT(sg[sgi][:, 0:N], PS[bg][:, 0:N], AF.Silu, [f"ps{bg}"], [f"sg{sgi}"])
                self.TT("dve", hT[i][:, ffc, n0:n0 + N], sg[sgi][:, 0:N], PS[bu][:, 0:N], ALU.mult,
                        [f"sg{sgi}", f"ps{bu}"], [f"hT{i}"])

        def down(e):
            i = e % 2
            for s_ in range(nst):
                k = cnt["yc"] % 6
                cnt["yc"] += 1
                for n in range(2):
                    by = 6 + n
                    for c in range(4):
                        self.MM(PS[by][:, 0:512], hT[i][:, c, s_ * 128:(s_ + 1) * 128], wd[i][:, c, n * 512:(n + 1) * 512],
                                c == 0, c == 3, [f"hT{i}", f"wd{i}c{c}"], [f"ps{by}"])
                    self.CP(("act", "dve")[n], ysb[k][:, n * 512:(n + 1) * 512], PS[by][:, 0:512], [f"ps{by}"], [f"ysb{k}"])
                r0 = e * CAP + s_ * 128
                self.DMA("pool", y_d[r0:r0 + 128, :], ysb[k], [f"ysb{k}"], ["y_d"])

        for w in range(3):
            w_load(0, w)
            w_cast(0, w)
        x_trans(0)
        for e in range(NE):
            nxt = e + 1 < NE
            if nxt:
                for w in range(3):
                    w_load(e + 1, w)
            for ffc in range(4):
                gate_up(e, ffc)
                if nxt and ffc < 3:
                    w_cast(e + 1, ffc)
            if nxt:
                x_trans(e + 1)
            down(e)

    def phase4(self, pin, w_ple_proj, w_ple_gate, b_ple_gate, h1_d, y_d, out, ident_b, ones_b, g_ple,
               slots_all, wts_all):
        PS, PSb = self.PS, self.PSb
        sb = self.sb
        wpg = sb([128, 8, D], BF16)
        wpp = sb([128, 2, D], BF16)
        stg = [sb([128, D], F32) for _ in range(2)]
        for c in range(8):
            self.DMA("sp", stg[c % 2], w_ple_gate[c * 128:(c + 1) * 128, :], [], [f"wstg{c%2}"])
            self.CP(("pool", "dve")[c % 2], wpg[:, c, :], stg[c % 2], [f"wstg{c%2}"], ["wpg"])
        for c in range(2):
            self.DMA("sp", stg[c % 2], w_ple_proj[c * 128:(c + 1) * 128, :], [], [f"wstg{c%2}"])
            self.CP(("pool", "dve")[c % 2], wpp[:, c, :], stg[c % 2], [f"wstg{c%2}"], ["wpp"])
        bf = sb([1, D], F32)
        bhi = sb([1, D], BF16)
        blo = sb([1, D], BF16)
        bt_ = sb([1, D], F32)
        self.DMA("sp", bf[0:1, :], b_ple_gate, [], ["bf"])
        self.CP("dve", bhi[0:1, :], bf[0:1, :], ["bf"], ["bhi"])
        self.TT("dve", bt_[0:1, :], bf[0:1, :], bhi[0:1, :], ALU.subtract, ["bf", "bhi"], ["bt_"])
        self.CP("dve", blo[0:1, :], bt_[0:1, :], ["bt_"], ["blo"])
        NH = 9
        H = [sb([128, D], F32) for _ in range(NH)]
        y1 = [sb([128, D], F32) for _ in range(2)]
        y2 = [sb([128, D], F32) for _ in range(2)]
        pt = [sb([128, PLE], F32) for _ in range(2)]
        pb = [sb([128, PLE], BF16) for _ in range(2)]
        pT = [sb([128, 2, 128], BF16) for _ in range(2)]
        h2b = [sb([128, D], BF16) for _ in range(2)]
        h2T = [sb([128, 8, 128], BF16) for _ in range(2)]
        ev = [sb([128, D], F32) for _ in range(2)]
        NG = 5
        gt = [sb([128, D], F32) for _ in range(NG)]
        junk = sb([128, D], BF16)
        ssall = sb([128, NT, 2], F32)
        rsall = sb([128, NT], F32)

        def sw_pipe(ntiles, sts):
            K_ = len(sts)
            for tau in range(ntiles + K_ - 1):
                for jj in reversed(range(K_)):
                    t = tau - jj
                    if 0 <= t < ntiles:
                        sts[jj](t)

        self.MS("pool", ssall, 0.0, ["ssall"])

        def a0(t):
            i = t % 2
            self.DMA("sp", pt[i], pin[t * 128:(t + 1) * 128, :], [], [f"pt{i}"])

        def a1(t):
            i = t % 2
            self.CP("pool", pb[i], pt[i], [f"pt{i}"], [f"pb{i}"])

        def a2(t):
            i = t % 2
            for c in range(2):
                self.TR(PSb[0][:, c * 128:(c + 1) * 128], pb[i][:, c * 128:(c + 1) * 128], ident_b, [f"pb{i}", "Cb"], ["ps0"])
            self.CP("act", pT[i], PSb[0][:, 0:256].rearrange("p (c n) -> p c n", c=2), ["ps0"], [f"pT{i}"])

        def a3(t):
            i = t % 2
            for n in range(2):
                be = 2 + n
                for c in range(2):
                    self.MM(PS[be][:, 0:512], pT[i][:, c, :], wpp[:, c, n * 512:(n + 1) * 512], c == 0, c == 1,
                            [f"pT{i}", "wpp"], [f"ps{be}"])

        def a4(t):
            for n in range(2):
                be = 2 + n
                self.ACT(junk[:, 0:512], PS[be][:, 0:512], AF.Square, [f"ps{be}", "ssall"], ["ssall"],
                         accum=ssall[:, t, n:n + 1])

        sw_pipe(NT, [a0, a1, a2, a3, a4])
        self.TT("dve", rsall, ssall[:, :, 0], ssall[:, :, 1], ALU.add, ["ssall"], ["rsall"])
        self.ACT(rsall, rsall, AF.Sqrt, ["rsall", "epsc"], ["rsall"], bias=self.epsc[:, 0:1], scale=1.0 / D)
        self.RCP(rsall, rsall, ["rsall"], ["rsall"])

        def s0(t):
            i, ih = t % 2, t % NH
            self.DMA("sp", H[ih], h1_d[t * 128:(t + 1) * 128, :], [], [f"h{ih}"])
            self.DMA("sp", pt[i], pin[t * 128:(t + 1) * 128, :], [], [f"pt{i}"])
            for k, Y in ((0, y1[i]), (1, y2[i])):
                idx = slots_all[:, t, k:k + 1]
                self.p.dma("pool", lambda e, idx_=idx, dst_=Y: e.indirect_dma_start(
                    out=dst_, out_offset=None, in_=y_d, in_offset=bass.IndirectOffsetOnAxis(ap=idx_, axis=0)),
                    reads=["slots", "y_d"], writes=[f"y{k}_{i}"])

        def s1(t):
            i, ih = t % 2, t % NH
            self.CP("pool", pb[i], pt[i], [f"pt{i}"], [f"pb{i}"])
            self.STT("dve", H[ih], y1[i], wts_all[:, t, 0:1], H[ih], ALU.mult, ALU.add, [f"y0_{i}", f"h{ih}", "wts"], [f"h{ih}"])
            self.STT("dve", H[ih], y2[i], wts_all[:, t, 1:2], H[ih], ALU.mult, ALU.add, [f"y1_{i}", f"h{ih}", "wts"], [f"h{ih}"])

        def s2(t):
            i, ih = t % 2, t % NH
            for c in range(2):
                self.TR(PSb[0][:, c * 128:(c + 1) * 128], pb[i][:, c * 128:(c + 1) * 128], ident_b, [f"pb{i}", "Cb"], ["ps0"])
            self.CP("act", pT[i], PSb[0][:, 0:256].rearrange("p (c n) -> p c n", c=2), ["ps0"], [f"pT{i}"])
            self.CP("act", h2b[i], H[ih], [f"h{ih}"], [f"h2b{i}"])

        def s3(t):
            i = t % 2
            for c in range(8):
                self.TR(PSb[1][:, c * 128:(c + 1) * 128], h2b[i][:, c * 128:(c + 1) * 128], ident_b, [f"h2b{i}", "Cb"], ["ps1"])
            self.CP("act", h2T[i], PSb[1][:, 0:1024].rearrange("p (c n) -> p c n", c=8), ["ps1"], [f"h2T{i}"])
            for n in range(2):
                be = 2 + n
                for c in range(2):
                    self.MM(PS[be][:, 0:512], pT[i][:, c, :], wpp[:, c, n * 512:(n + 1) * 512], c == 0, c == 1,
                            [f"pT{i}", "wpp"], [f"ps{be}"])

        def s4(t):
            i = t % 2
            for n in range(2):
                be = 2 + n
                self.STT("dve", ev[i][:, n * 512:(n + 1) * 512], PS[be][:, 0:512], rsall[:, t:t + 1], g_ple[:, n * 512:(n + 1) * 512],
                         ALU.mult, ALU.mult, [f"ps{be}", "rsall", "G"], [f"ev{i}"])
            for n in range(2):
                bg = 4 + n
                for c in range(8):
                    self.MM(PS[bg][:, 0:512], h2T[i][:, c, :], wpg[:, c, n * 512:(n + 1) * 512], c == 0, False,
                            [f"h2T{i}", "wpg"], [f"ps{bg}"])
                self.MM(PS[bg][:, 0:512], ones_b[0:1, :], bhi[0:1, n * 512:(n + 1) * 512], False, False, ["Cb", "bhi"], [f"ps{bg}"])
                self.MM(PS[bg][:, 0:512], ones_b[0:1, :], blo[0:1, n * 512:(n + 1) * 512], False, True, ["Cb", "blo"], [f"ps{bg}"])

        def s5(t):
            ig = t % NG
            for n in range(2):
                bg = 4 + n
                self.ACT(gt[ig][:, n * 512:(n + 1) * 512], PS[bg][:, 0:512], AF.Sigmoid, [f"ps{bg}"], [f"gt{ig}"])

        def s6(t):
            i, ig = t % 2, t % NG
            Gt, E = gt[ig], ev[i]
            self.TT("pool", Gt[:, 0:384], Gt[:, 0:384], E[:, 0:384], ALU.mult, [f"gt{ig}", f"ev{i}"], [f"gt{ig}a"])
            self.TT("dve", Gt[:, 384:1024], Gt[:, 384:1024], E[:, 384:1024], ALU.mult, [f"gt{ig}", f"ev{i}"], [f"gt{ig}b"])

        def s7(t):
            ig, ih = t % NG, t % NH
            self.TT("dve", gt[ig], gt[ig], H[ih], ALU.add, [f"gt{ig}", f"gt{ig}a", f"gt{ig}b", f"h{ih}"], [f"gt{ig}"])

        def s8(t):
            ig = t % NG
            self.DMA("sp", out[t * 128:(t + 1) * 128, :], gt[ig], [f"gt{ig}"], ["out"])

        sw_pipe(NT, [s0, s1, s2, s3, s4, s5, s6, s7, s8])


def _t5_bucket(d):
    max_exact = 16
    n = np.maximum(d, 0)
    nf = np.maximum(n, 1).astype(np.float32)
    large = max_exact + (np.log(nf / np.float32(max_exact)) / np.float32(math.log(2048 / max_exact))
                         * np.float32(32 - max_exact)).astype(np.int32)
    large = np.minimum(large, 31)
    return np.where(n < max_exact, n, large)


def _constants():
    c = np.zeros((128, 1024), np.float32)
    c[:, 0:128] = np.eye(128, dtype=np.float32)
    jj = np.arange(128)[:, None]
    cc = np.arange(128)[None, :]
    c[:, 128:256] = (cc >= jj).astype(np.float32)
    c[:, 256:384] = (jj < cc).astype(np.float32)
    c[:, 384:416] = (np.arange(32) * CAP).astype(np.float32)[None, :]
    half = 16
    inv = (1.0 / (np.float32(10000.0) ** (np.arange(half, dtype=np.float32) * np.float32(2.0) / np.float32(32)))).astype(np.float32)
    pos = np.arange(S, dtype=np.float32)
    ang = (pos[:, None] * inv[None, :]).astype(np.float32)
    cos = np.cos(ang).astype(np.float32).reshape(16, 128, 16).transpose(1, 0, 2).reshape(128, 256)
    sin = np.sin(ang).astype(np.float32).reshape(16, 128, 16).transpose(1, 0, 2).reshape(128, 256)
    c[:, 512:768] = cos
    c[:, 768:1024] = sin
    dist = np.arange(MTW)[None, :] - np.arange(128)[:, None]
    valid = (dist >= 0) & (dist < S)
    d = np.clip(dist, 0, S - 1)
    mult = np.zeros(d.shape, np.float32)
    mult += (d <= 128)
    mult += ((d % 4 == 0) & (d <= 512))
    mult += (d % 16 == 0)
    mult = np.where(valid, mult, 0.0).astype(np.float32)
    bucket = _t5_bucket(d)
    return c, mult, bucket, valid


_CACHE = {}


def _get_program(debug=False, stop_after=99):
    key = (debug, stop_after)
    if key not in _CACHE:
        b = Builder(debug=debug, stop_after=stop_after)
        _CACHE[key] = b.build()
    return _CACHE[key]


def _prep_inputs(inputs):
    f = lambda k: np.asarray(inputs[k], dtype=np.float32)
    x = f("x").reshape(32 * S, D)
    pp = f("p").reshape(32 * S, PLE)
    cst, mult, bucket, valid = _constants()
    rel_bias = f("rel_bias")
    bt = rel_bias[bucket]
    bt = np.where(valid[:, :, None], bt, np.float32(0.0))
    bias_toep = np.ascontiguousarray(bt.transpose(2, 0, 1)).astype(np.float32)
    gv = np.zeros((1, 4096), np.float32)
    segs = [("norm_mix_gain", 0), ("norm_ffn_gain", 1024), ("ple_norm_gain", 2048), ("qn_a_gain", 3072),
            ("kn_a_gain", 3136), ("q_a_gain", 3200), ("kv_a_gain", 3584), ("qn_nope_gain", 3840),
            ("qn_rope_gain", 3904), ("kn_nope_gain", 3936), ("kn_rope_gain", 4000)]
    for k, o in segs:
        v = f(k).reshape(-1)
        gv[0, o:o + v.size] = v
    wr = np.concatenate([f("w_router_group")[0], f("w_router_expert")[0]], axis=1)
    br = np.concatenate([f("b_router_group")[0], f("b_router_expert")[0]], axis=0)[None, :]
    shared = {
        "w_in": f("w_in")[0], "w_q_up": f("w_q_up")[0], "w_kv_up": f("w_kv_up")[0], "w_out": f("w_out")[0],
        "wr": np.ascontiguousarray(wr), "br": np.ascontiguousarray(br),
        "w_exp_gate": f("w_exp_gate")[0], "w_exp_up": f("w_exp_up")[0], "w_exp_down": f("w_exp_down")[0],
        "w_ple_proj": f("w_ple_proj")[0], "w_ple_gate": f("w_ple_gate")[0], "b_ple_gate": f("b_ple_gate"),
        "gvec": gv, "cst": cst, "bias_toep": bias_toep, "mult_toep": mult,
    }
    in_maps = []
    for c in range(NCORES):
        m = dict(shared)
        m["x"] = x[c * NTOK:(c + 1) * NTOK]
        m["p"] = pp[c * NTOK:(c + 1) * NTOK]
        in_maps.append(m)
    return in_maps


def kernel(**inputs):
    nc = _get_program()
    in_maps = _prep_inputs(inputs)
    res = run_bass_kernel_spmd(nc, in_maps, core_ids=list(range(NCORES)))
    outs = [np.asarray(r["out"], dtype=np.float32) for r in res.results]
    return np.concatenate(outs, axis=0).reshape(32, S, D)
```

```python
import contextlib
import math
import numpy as np
import ml_dtypes
import concourse.bass as bass
import concourse.mybir as mybir
from concourse.bass_utils import run_bass_kernel_spmd

F32 = mybir.dt.float32
BF16 = mybir.dt.bfloat16
I32 = mybir.dt.int32
U8 = mybir.dt.uint8
ALU = mybir.AluOpType
AF = mybir.ActivationFunctionType
AX = mybir.AxisListType

NCORES = 8
D = 1024
S = 2048
NSEQ = 4
NTOK = NSEQ * S
NT = NTOK // 128
TPS = S // 128
INC = 2208
PLE = 256
NE = 32
FF = 512
CAP = 768
NSLOT = NE * CAP
TRASH = NSLOT
MTW = 2176
EPS = 1e-6
ENGS = ("pe", "act", "dve", "pool", "sp")


class _Op:
    __slots__ = ("eng", "fn", "deps", "is_dma", "idx", "signal", "semi", "semval", "dsem", "dval", "dprev")

    def __init__(self, eng, fn, is_dma, idx):
        self.eng = eng
        self.fn = fn
        self.deps = []
        self.is_dma = is_dma
        self.idx = idx
        self.signal = False
        self.semi = 0
        self.semval = 0
        self.dsem = None
        self.dval = 0
        self.dprev = 0


class Prog:
    EPOCH = 20000
    NDMA = 16

    def __init__(self, nc):
        self.nc = nc
        self.ops = []
        self.last_w = {}
        self.readers = {}
        self.bar_deps = None
        self.bar_seen = set()
        self.last_eng = {}
        self.dma_since = []

    def _add(self, eng, fn, reads, writes, is_dma):
        op = _Op(eng, fn, is_dma, len(self.ops))
        deps = {}
        for r in reads:
            w = self.last_w.get(r)
            if w is not None:
                deps[w.idx] = w
        for w_ in writes:
            w = self.last_w.get(w_)
            if w is not None:
                deps[w.idx] = w
            for rd in self.readers.get(w_, ()):
                deps[rd.idx] = rd
        for r in reads:
            self.readers.setdefault(r, []).append(op)
        for w_ in writes:
            self.last_w[w_] = op
            self.readers[w_] = []
        if self.bar_deps is not None and eng not in self.bar_seen:
            self.bar_seen.add(eng)
            for d in self.bar_deps:
                deps[d.idx] = d
        deps.pop(op.idx, None)
        for d in deps.values():
            if (not d.is_dma) and (not is_dma) and d.eng == "pe" and eng == "pe":
                continue
            op.deps.append(d)
        self.ops.append(op)
        if is_dma:
            self.dma_since.append(op)
        else:
            self.last_eng[eng] = op
        return op

    def op(self, eng, fn, reads=(), writes=()):
        return self._add(eng, fn, reads, writes, False)

    def dma(self, eng, fn, reads=(), writes=()):
        return self._add(eng, fn, reads, writes, True)

    def barrier(self):
        deps = list(self.last_eng.values()) + list(self.dma_since)
        self.bar_deps = deps
        self.bar_seen = set()
        self.last_w = {}
        self.readers = {}

    def emit(self, final_wait_ops=()):
        nc = self.nc
        ops = self.ops
        for o in ops:
            for d in o.deps:
                d.signal = True
        for o in final_wait_ops:
            o.signal = True
        cnt = {e: 0 for e in ENGS}
        for o in ops:
            if o.is_dma:
                continue
            if o.signal:
                c = cnt[o.eng]
                o.semi = c // self.EPOCH
                o.semval = c % self.EPOCH + 1
                cnt[o.eng] = c + 1
        nsem = {e: (cnt[e] + self.EPOCH - 1) // self.EPOCH for e in ENGS}
        n_sw = 6
        n_hw = self.NDMA - n_sw
        dcount = {"hw": 0, "sw": 0}
        dlast = [0] * self.NDMA
        for o in ops:
            if o.is_dma:
                if o.eng == "pool":
                    k = n_hw + dcount["sw"] % n_sw
                    dcount["sw"] += 1
                else:
                    k = dcount["hw"] % n_hw
                    dcount["hw"] += 1
                o.dsem = k
                o.dprev = dlast[k]
                dlast[k] += 16
                o.dval = dlast[k]
        with contextlib.ExitStack() as st:
            csem = {e: [st.enter_context(nc.semaphore(f"c_{e}_{i}")) for i in range(nsem[e])] for e in ENGS}
            dsem = [st.enter_context(nc.semaphore(f"d_{i}")) for i in range(self.NDMA)]
            block = st.enter_context(nc.Block())
            per_eng = {e: [o for o in ops if o.eng == e] for e in ENGS}
            finals = list(final_wait_ops)

            def run(e, engobj):
                known_c = {}
                known_d = {}

                def wait_for(d):
                    if d.is_dma:
                        if known_d.get(d.dsem, 0) >= d.dval:
                            return
                        engobj.wait_ge(dsem[d.dsem], d.dval)
                        known_d[d.dsem] = d.dval
                    else:
                        key = (d.eng, d.semi)
                        if known_c.get(key, 0) >= d.semval:
                            return
                        engobj.wait_ge(csem[d.eng][d.semi], d.semval)
                        known_c[key] = d.semval

                def reduce_deps(deps):
                    best = {}
                    for d in deps:
                        k = ("d", d.dsem) if d.is_dma else ("c", d.eng, d.semi)
                        v = d.dval if d.is_dma else d.semval
                        if k not in best or v > best[k][0]:
                            best[k] = (v, d)
                    return [bd[1] for bd in best.values()]

                for o in per_eng[e]:
                    for d in sorted(reduce_deps(o.deps), key=lambda z: z.idx):
                        wait_for(d)
                    if o.is_dma:
                        if o.dprev > 0 and known_d.get(o.dsem, 0) < o.dprev:
                            engobj.wait_ge(dsem[o.dsem], o.dprev)
                            known_d[o.dsem] = o.dprev
                        ins = o.fn(engobj)
                        ins.then_inc(dsem[o.dsem], 16)
                    else:
                        ins = o.fn(engobj)
                        if o.signal:
                            ins.then_inc(csem[o.eng][o.semi], 1)
                if e == "sp":
                    for o in reduce_deps(finals):
                        wait_for(o)

            @block.tensor
            def _(eng):
                run("pe", eng)

            @block.scalar
            def _(eng):
                run("act", eng)

            @block.vector
            def _(eng):
                run("dve", eng)

            @block.gpsimd
            def _(eng):
                run("pool", eng)

            @block.sync
            def _(eng):
                run("sp", eng)


class Builder:
    def __init__(self, debug=False, stop_after=99):
        self.debug = debug
        self.stop_after = stop_after
        self.nc = bass.Bass("TRN2", target_bir_lowering=False)
        self.p = Prog(self.nc)
        self.arena_off = 0
        self.rr = 0
        self.out_ops = []

    def din(self, name, shape, dt=F32):
        return self.nc.dram_tensor(name, list(shape), dt, kind="ExternalInput").ap()

    def dscr(self, name, shape, dt, out=False):
        kind = "ExternalOutput" if (out or (self.debug and name in self.debug)) else "Internal"
        return self.nc.dram_tensor(name, list(shape), dt, kind=kind).ap()

    def sb(self, shape, dt):
        esz = {F32: 4, BF16: 2, I32: 4}[dt]
        n = 1
        for s in shape[1:]:
            n *= s
        nbytes = (n * esz + 63) // 64 * 64
        off = self.arena_off
        self.arena_off += nbytes
        assert self.arena_off <= self.arena_bytes, ("SBUF arena overflow", self.arena_off)
        v = self.arena[:, off:off + n * esz].bitcast(dt)
        if len(shape) == 3:
            v = v.rearrange("p (a b) -> p a b", a=shape[1])
        elif len(shape) == 4:
            v = v.rearrange("p (a b c) -> p a b c", a=shape[1], b=shape[2])
        return v

    def DMA(self, q, out, in_, r, w):
        o = self.p.dma(q, lambda e: e.dma_start(out=out, in_=in_), reads=r, writes=w)
        if "out" in w or (self.debug and any(k in self.debug for k in w)):
            self.out_ops.append(o)
        return o

    def MM(self, out, lhsT, rhs, start, stop, r, w):
        return self.p.op("pe", lambda e: e.matmul(out, lhsT=lhsT, rhs=rhs, start=start, stop=stop,
                                                  skip_group_check=True), reads=r, writes=w)

    def TR(self, out, in_, ident, r, w):
        return self.p.op("pe", lambda e: e.transpose(out=out, in_=in_, identity=ident), reads=r, writes=w)

    def ACT(self, out, in_, func, r, w, bias=None, scale=None, accum=None):
        kw = {}
        if bias is not None:
            kw["bias"] = bias
        if scale is not None:
            kw["scale"] = scale
        if accum is not None:
            kw["accum_out"] = accum
        return self.p.op("act", lambda e: e.activation(out=out, in_=in_, func=func, **kw), reads=r, writes=w)

    def CP(self, eng, out, in_, r, w):
        if eng == "act":
            return self.p.op("act", lambda e: e.copy(out=out, in_=in_), reads=r, writes=w)
        return self.p.op(eng, lambda e: e.tensor_copy(out=out, in_=in_), reads=r, writes=w)

    def TT(self, eng, out, in0, in1, op, r, w):
        return self.p.op(eng, lambda e: e.tensor_tensor(out=out, in0=in0, in1=in1, op=op), reads=r, writes=w)

    def TS(self, eng, out, in0, s1, s2, op0, op1, r, w):
        if op1 is None:
            return self.p.op(eng, lambda e: e.tensor_scalar(out=out, in0=in0, scalar1=s1, scalar2=None, op0=op0),
                             reads=r, writes=w)
        return self.p.op(eng, lambda e: e.tensor_scalar(out=out, in0=in0, scalar1=s1, scalar2=s2, op0=op0, op1=op1),
                         reads=r, writes=w)

    def STT(self, eng, out, in0, scalar, in1, op0, op1, r, w):
        return self.p.op(eng, lambda e: e.scalar_tensor_tensor(out=out, in0=in0, scalar=scalar, in1=in1,
                                                               op0=op0, op1=op1), reads=r, writes=w)

    def RED(self, eng, out, in_, op, r, w):
        return self.p.op(eng, lambda e: e.tensor_reduce(out=out, in_=in_, axis=AX.X, op=op), reads=r, writes=w)

    def RCP(self, out, in_, r, w):
        return self.p.op("dve", lambda e: e.reciprocal(out=out, in_=in_), reads=r, writes=w)

    def MS(self, eng, out, val, w, r=()):
        return self.p.op(eng, lambda e: e.memset(out, val), reads=r, writes=w)

    def rstd(self, ss, n, nm, r_extra=()):
        self.ACT(ss, ss, AF.Ln, [nm, "epsc"] + list(r_extra), [nm], bias=self.epsc[:, 0:1], scale=1.0 / n)
        self.ACT(ss, ss, AF.Exp, [nm], [nm], scale=-0.5)

    def build(self):
        nc = self.nc
        dbg = self.debug
        x = self.din("x", [NTOK, D])
        pin = self.din("p", [NTOK, PLE])
        w_in = self.din("w_in", [D, INC])
        w_q_up = self.din("w_q_up", [384, 768])
        w_kv_up = self.din("w_kv_up", [256, 1024])
        w_out = self.din("w_out", [D, D])
        wr = self.din("wr", [D, 36])
        br = self.din("br", [1, 36])
        self.br_d = br
        w_gate = self.din("w_exp_gate", [NE, D, FF])
        w_up = self.din("w_exp_up", [NE, D, FF])
        w_down = self.din("w_exp_down", [NE, FF, D])
        w_ple_proj = self.din("w_ple_proj", [PLE, D])
        w_ple_gate = self.din("w_ple_gate", [D, D])
        b_ple_gate = self.din("b_ple_gate", [1, D])
        gvec = self.din("gvec", [1, 4096])
        cst = self.din("cst", [128, 1024])
        bias_toep = self.din("bias_toep", [8, 128, MTW])
        mult_toep = self.din("mult_toep", [128, MTW])
        out = self.nc.dram_tensor("out", [NTOK, D], F32, kind="ExternalOutput").ap()
        qTa_d = self.dscr("qTa_d", [512, NTOK], BF16)
        kTa_d = self.dscr("kTa_d", [512, NTOK], BF16)
        va_d = self.dscr("va_d", [NTOK, 512], BF16)
        qTm_d = self.dscr("qTm_d", [768, NTOK], BF16)
        kTm_d = self.dscr("kTm_d", [768, NTOK], BF16)
        vm_d = self.dscr("vm_d", [NTOK, 512], BF16)
        mt_d = self.dscr("mt_d", [8, 128, MTW], BF16)
        h1_d = self.dscr("h1_d", [NTOK, D], F32)
        xg_d = self.dscr("xg_d", [NSLOT + 128, D], BF16)
        y_d = self.dscr("y_d", [NSLOT + 128, D], F32)
        rt_d = self.dscr("rt_d", [128, NT * 4], F32, out=False)

        with contextlib.ExitStack() as st:
            self.arena_bytes = 200 * 1024
            self.arena = st.enter_context(nc.sbuf_tensor("arena", [128, self.arena_bytes], U8))
            PS = [st.enter_context(nc.psum_tensor(f"ps{i}", [128, 512], F32)) for i in range(8)]
            PSb = [t[:, :].bitcast(BF16) for t in PS]
            PS = [t[:, :] for t in PS]
            self.PS, self.PSb = PS, PSb

            C = self.sb([128, 1024], F32)
            self.C = C
            ident_f = C[:, 0:128]
            eoff = C[:, 384:416]
            cos_t = C[:, 512:768].rearrange("p (t i) -> p t i", t=16)
            sin_t = C[:, 768:1024].rearrange("p (t i) -> p t i", t=16)
            Cb = self.sb([128, 512], BF16)
            ident_b = Cb[:, 0:128]
            causal_b = Cb[:, 128:256]
            ustrict_b = Cb[:, 256:384]
            ones_b = Cb[:, 384:512]
            onesf = self.sb([128, 128], F32)
            G = self.sb([128, 4096], F32)
            self.epsc = self.sb([128, 1], F32)
            g_mix = G[:, 0:1024]
            g_ffn = G[:, 1024:2048]
            g_ple = G[:, 2048:3072]
            g_qa = G[:, 3072:3136]
            g_ka = G[:, 3136:3200]
            g_cq = G[:, 3200:3584]
            g_ckv = G[:, 3584:3840]
            g_qn = G[:, 3840:3904]
            g_qr = G[:, 3904:3936]
            g_kn = G[:, 3936:4000]
            g_kr = G[:, 4000:4032]
            slots_all = self.sb([128, NT, 2], I32)
            wts_all = self.sb([128, NT, 2], F32)
            acc_b = self.sb([128, 32], BF16)
            wr_sb = self.sb([128, 8, 36], F32)
            br_sb = self.sb([1, 36], F32)
            persist_mark = self.arena_off

            p = self.p
            self.DMA("sp", C, cst, [], ["C"])
            self.DMA("sp", G, gvec[0:1, :].partition_broadcast(128)[:, 0, :], [], ["G"])
            self.DMA("sp", wr_sb, wr.rearrange("(c p) n -> p c n", p=128), [], ["wr"])
            self.DMA("sp", br_sb[0:1, :], br, [], ["br"])
            self.MS("pool", self.epsc, EPS, ["epsc"])
            self.MS("pool", onesf, 1.0, ["onesf"])
            self.MS("pool", acc_b, 0.0, ["acc"])
            self.MS("pool", Cb[:, 384:512], 1.0, ["Cb"])
            self.CP("dve", Cb[:, 0:384], C[:, 0:384], ["C", "Cb"], ["Cb"])
            self.MS("pool", slots_all, 0, ["slots"])
            self.MS("pool", wts_all, 0.0, ["wts"])

            w_in_b = self.sb([128, 8, INC], BF16)
            wq_b = self.sb([128, 3, 768], BF16)
            wkv_b = self.sb([128, 2, 1024], BF16)
            p1_mark = self.arena_off
            stg = [self.sb([128, INC], F32) for _ in range(2)]
            for c in range(8):
                s_ = stg[c % 2]
                self.DMA("sp", s_, w_in[c * 128:(c + 1) * 128, :], [], [f"stg{c%2}"])
                self.CP(("pool", "dve")[c % 2], w_in_b[:, c, :], s_, [f"stg{c%2}"], ["w_in_b"])
            for c in range(3):
                s_ = stg[c % 2]
                self.DMA("sp", s_[:, 0:768], w_q_up[c * 128:(c + 1) * 128, :], [], [f"stg{c%2}"])
                self.CP(("pool", "dve")[c % 2], wq_b[:, c, :], s_[:, 0:768], [f"stg{c%2}"], ["wq_b"])
            for c in range(2):
                s_ = stg[c % 2]
                self.DMA("sp", s_[:, 0:1024], w_kv_up[c * 128:(c + 1) * 128, :], [], [f"stg{c%2}"])
                self.CP(("pool", "dve")[c % 2], wkv_b[:, c, :], s_[:, 0:1024], [f"stg{c%2}"], ["wkv_b"])
            mult_sb = self.sb([128, MTW], F32)
            self.DMA("sp", mult_sb, mult_toep, [], ["mult"])
            mtb = [self.sb([128, MTW], BF16) for _ in range(2)]
            for h in range(8):
                s_ = stg[h % 2]
                self.DMA("sp", s_[:, 0:MTW], bias_toep[h], [], [f"stg{h%2}"])
                self.ACT(s_[:, 0:MTW], s_[:, 0:MTW], AF.Exp, [f"stg{h%2}"], [f"stg{h%2}"])
                self.TT("dve", mtb[h % 2], s_[:, 0:MTW], mult_sb, ALU.mult, [f"stg{h%2}", "mult"], [f"mtb{h%2}"])
                self.DMA("pool", mt_d[h], mtb[h % 2], [f"mtb{h%2}"], ["mt_d"])
            zt = self.sb([128, 7, 1024], BF16)
            ztf = self.sb([128, 1024], F32)
            self.MS("pool", zt, 0.0, ["zt"])
            self.MS("pool", ztf, 0.0, ["ztf"])
            xg_v = xg_d.rearrange("(r p) n -> p r n", p=128)
            nrt = (NSLOT + 128) // 128
            for i in range(0, nrt, 7):
                n_ = min(7, nrt - i)
                self.DMA("pool", xg_v[:, i:i + n_, :], zt[:, 0:n_, :], ["zt"], ["xg_d"])
            self.DMA("pool", y_d[NSLOT:NSLOT + 128, :], ztf, ["ztf"], ["y_d"])
            p.barrier()
            self.arena_off = p1_mark

            if self.stop_after >= 1:
                self.phase1(x, w_in_b, wq_b, wkv_b, qTa_d, kTa_d, va_d, qTm_d, kTm_d, vm_d,
                            ident_b, g_mix, g_qa, g_ka, g_cq, g_ckv, g_qn, g_qr, g_kn, g_kr, cos_t, sin_t)
            p.barrier()
            self.arena_off = persist_mark
            if self.stop_after >= 2:
                self.phase2(x, w_out, qTa_d, kTa_d, va_d, qTm_d, kTm_d, vm_d, mt_d, h1_d, xg_d, rt_d,
                            ident_f, ident_b, causal_b, ustrict_b, ones_b, onesf, eoff, g_ffn,
                            wr_sb, br_sb, slots_all, wts_all, acc_b)
            p.barrier()
            self.arena_off = persist_mark
            if self.stop_after >= 3:
                self.phase3(w_gate, w_up, w_down, xg_d, y_d, ident_b)
            p.barrier()
            self.arena_off = persist_mark
            if self.stop_after >= 4:
                self.phase4(pin, w_ple_proj, w_ple_gate, b_ple_gate, h1_d, y_d, out, ident_b, ones_b, g_ple,
                            slots_all, wts_all)
            p.emit(final_wait_ops=self.out_ops)
        return nc

    def headnorm(self, src_ps, nh, hd, stride, off, gain, dst, dst_nm, ps_nm, n_real, tag, sqt, ssum, tmpt):
        v = src_ps
        sq, sqk = sqt
        tmp, tmpk = tmpt
        self.ACT(sq[:, 0:nh * stride], v, AF.Square, [ps_nm], [sqk])
        sqv = sq[:, 0:nh * stride].rearrange("p (h d) -> p h d", h=nh)[:, :, off:off + hd]
        self.RED("dve", ssum, sqv, ALU.add, [sqk], [tag + "ss"])
        self.rstd(ssum, n_real, tag + "ss")
        vv = v.rearrange("p (h d) -> p h d", h=nh)[:, :, off:off + hd]
        self.TT("dve", tmp, vv, ssum.unsqueeze(2).to_broadcast([128, nh, hd]), ALU.mult,
                [ps_nm, tag + "ss"], [tmpk])
        self.TT("pool", dst, tmp, gain.unsqueeze(1).to_broadcast([128, nh, hd]), ALU.mult,
                [tmpk, "G"], [dst_nm])

    def phase1(self, x, w_in_b, wq_b, wkv_b, qTa_d, kTa_d, va_d, qTm_d, kTm_d, vm_d,
               ident_b, g_mix, g_qa, g_ka, g_cq, g_ckv, g_qn, g_qr, g_kn, g_kr, cos_t, sin_t):
        PS, PSb = self.PS, self.PSb
        sb = self.sb
        junk = sb([128, D], BF16)
        NS = 2
        B = []
        for si in range(NS):
            d = {}
            d["xt"] = [sb([128, D], F32) for _ in range(2)]
            d["xn"] = sb([128, D], BF16)
            d["xT"] = sb([128, 8, 128], BF16)
            d["sq"] = [sb([128, 1024], F32) for _ in range(2)]
            d["tmp"] = [sb([128, 8, 64], F32) for _ in range(2)]
            d["ss"] = [sb([128, 8], F32) for _ in range(10)]
            d["qan"] = sb([128, 8, 64], BF16)
            d["kan"] = sb([128, 8, 64], BF16)
            d["cqn"] = sb([128, 384], BF16)
            d["cqT"] = sb([128, 3, 128], BF16)
            d["ckvn"] = sb([128, 256], BF16)
            d["ckvT"] = sb([128, 2, 128], BF16)
            d["qm"] = sb([128, 8, 96], BF16)
            d["km"] = sb([128, 8, 96], BF16)
            d["rp"] = sb([128, 8, 32], F32)
            d["r1"] = sb([128, 8, 16], F32)
            d["r2"] = sb([128, 8, 16], F32)
            d["krn"] = sb([128, 32], F32)
            d["kro"] = sb([128, 32], BF16)
            d["k1"] = sb([128, 16], F32)
            d["k2"] = sb([128, 16], F32)
            d["bank"] = 0
            d["sqc"] = 0
            d["tmc"] = 0
            d["xc"] = 0
            B.append(d)
        qaT_s = [sb([128, 4, 256], BF16) for _ in range(2)]
        kaT_s = [sb([128, 4, 256], BF16) for _ in range(2)]
        qmT_s = [sb([128, 8, 256], BF16) for _ in range(2)]
        kmT_s = [sb([128, 8, 256], BF16) for _ in range(2)]
        va_s = [sb([128, 2, 512], BF16) for _ in range(2)]
        vm_s = [sb([128, 2, 8, 64], BF16) for _ in range(2)]
        chunks = [(0, 512), (512, 512), (1024, 512), (1536, 384), (1920, 288)]

        def tile_gen(t, si):
            d = B[si]
            K_ = f"s{si}"
            m, ti = divmod(t, 2)
            mi = m % 2
            pj = t % TPS

            def nb():
                b_ = 4 * si + d["bank"]
                d["bank"] = (d["bank"] + 1) % 4
                return b_

            def nsq():
                k = d["sqc"] % 2
                d["sqc"] += 1
                return d["sq"][k], f"{K_}sq{k}"

            def ntmp():
                k = d["tmc"] % 2
                d["tmc"] += 1
                return d["tmp"][k], f"{K_}tmp{k}"

            ssh = d["ss"]
            xi = d["xc"] % 2
            d["xc"] += 1
            X = d["xt"][xi]
            xk = f"{K_}xt{xi}"
            self.DMA("sp", X, x[t * 128:(t + 1) * 128, :], [], [xk])
            s1 = ssh[8][:, 0:1]
            s1k = f"{K_}ss1"
            self.MS("pool", s1, 0.0, [s1k])
            yield
            self.ACT(junk, X, AF.Square, [xk, s1k], [s1k], accum=s1)
            self.rstd(s1, D, s1k)
            yield
            XN, xnk = d["xn"], f"{K_}xn"
            self.STT("dve", XN, X, s1, g_mix, ALU.mult, ALU.mult, [xk, s1k, "G"], [xnk])
            yield
            b0 = nb()
            for c in range(8):
                self.TR(PSb[b0][:, c * 128:(c + 1) * 128], XN[:, c * 128:(c + 1) * 128], ident_b, [xnk, "Cb"], [f"ps{b0}"])
            yield
            XT, xtk = d["xT"], f"{K_}xT"
            self.CP("act", XT, PSb[b0][:, 0:1024].rearrange("p (c n) -> p c n", c=8), [f"ps{b0}"], [xtk])
            yield

            def proj(ci):
                c0, cw = chunks[ci]
                b_ = nb()
                for c in range(8):
                    self.MM(PS[b_][:, 0:cw], XT[:, c, :], w_in_b[:, c, c0:c0 + cw], c == 0, c == 7,
                            [xtk, "w_in_b"], [f"ps{b_}"])
                return b_

            def headnorm_g(src_ps, ps_nm, gain, dst, dst_nm, ssum, tag):
                sq, sqk = nsq()
                tmp, tmpk = ntmp()
                self.ACT(sq[:, 0:512], src_ps, AF.Square, [ps_nm], [sqk])
                yield
                self.RED("dve", ssum, sq[:, 0:512].rearrange("p (h d) -> p h d", h=8), ALU.add, [sqk], [tag])
                yield
                self.rstd(ssum, 64, tag)
                yield
                self.TT("dve", tmp, src_ps.rearrange("p (h d) -> p h d", h=8), ssum.unsqueeze(2).to_broadcast([128, 8, 64]),
                        ALU.mult, [ps_nm, tag], [tmpk])
                yield
                self.TT("pool", dst, tmp, gain.unsqueeze(1).to_broadcast([128, 8, 64]), ALU.mult, [tmpk, "G"], [dst_nm])
                yield

            def trans_pairs(src, src_nm, stage, snm):
                bt = nb()
                for c in range(4):
                    self.TR(PSb[bt][:, c * 128:(c + 1) * 128], src[:, 2 * c:2 * c + 2, :].rearrange("p a b -> p (a b)"),
                            ident_b, [src_nm, "Cb"], [f"ps{bt}"])
                yield
                self.CP("dve", stage[:, :, ti * 128:(ti + 1) * 128],
                        PSb[bt][:, 0:512].rearrange("p (c n) -> p c n", c=4), [f"ps{bt}"], [snm])
                yield

            bc0 = proj(0)
            yield
            bc1 = proj(1)
            yield
            bc2 = proj(2)
            yield
            self.CP("act", va_s[mi][:, ti, :], PS[bc2][:, 0:512], [f"ps{bc2}"], [f"vas{mi}"])
            yield
            bc3 = proj(3)
            yield
            yield from headnorm_g(PS[bc0][:, 0:512], f"ps{bc0}", g_qa, d["qan"], f"{K_}qan", ssh[0], f"{K_}ssqa")
            bc4 = proj(4)
            yield
            yield from headnorm_g(PS[bc1][:, 0:512], f"ps{bc1}", g_ka, d["kan"], f"{K_}kan", ssh[1], f"{K_}sska")
            yield from trans_pairs(d["qan"], f"{K_}qan", qaT_s[mi], f"qaTs{mi}")
            yield from trans_pairs(d["kan"], f"{K_}kan", kaT_s[mi], f"kaTs{mi}")
            s2 = ssh[9][:, 0:1]
            cqk = f"{K_}cqss"
            self.MS("pool", s2, 0.0, [cqk])
            yield
            self.ACT(junk[:, 0:384], PS[bc3][:, 0:384], AF.Square, [f"ps{bc3}", cqk], [cqk], accum=s2)
            self.rstd(s2, 384, cqk)
            yield
            cqn, cqnk = d["cqn"], f"{K_}cqn"
            self.STT("dve", cqn, PS[bc3][:, 0:384], s2, g_cq, ALU.mult, ALU.mult, [f"ps{bc3}", cqk, "G"], [cqnk])
            yield
            bt = nb()
            for c in range(3):
                self.TR(PSb[bt][:, c * 128:(c + 1) * 128], cqn[:, c * 128:(c + 1) * 128], ident_b, [cqnk, "Cb"], [f"ps{bt}"])
            yield
            cqT, cqTk = d["cqT"], f"{K_}cqT"
            self.CP("dve", cqT, PSb[bt][:, 0:384].rearrange("p (c n) -> p c n", c=3), [f"ps{bt}"], [cqTk])
            yield
            s5 = ssh[7]
            ckk = f"{K_}ckss"
            self.MS("pool", s5[:, 0:2], 0.0, [ckk])
            yield
            self.ACT(junk[:, 0:256], PS[bc4][:, 0:256], AF.Square, [f"ps{bc4}", ckk], [ckk], accum=s5[:, 0:1])
            self.ACT(junk[:, 256:288], PS[bc4][:, 256:288], AF.Square, [f"ps{bc4}", ckk], [ckk], accum=s5[:, 1:2])
            self.rstd(s5[:, 0:1], 256, ckk)
            self.rstd(s5[:, 1:2], 32, ckk)
            yield
            ckvn, ckvnk = d["ckvn"], f"{K_}ckvn"
            krn, kro, k1, k2 = d["krn"], d["kro"], d["k1"], d["k2"]
            self.STT("dve", ckvn, PS[bc4][:, 0:256], s5[:, 0:1], g_ckv, ALU.mult, ALU.mult, [f"ps{bc4}", ckk, "G"], [ckvnk])
            self.STT("dve", krn, PS[bc4][:, 256:288], s5[:, 1:2], g_kr, ALU.mult, ALU.mult, [f"ps{bc4}", ckk, "G"], [f"{K_}krn"])
            yield
            bq0, bq1 = nb(), nb()
            for c in range(3):
                self.MM(PS[bq0][:, 0:480], cqT[:, c, :], wq_b[:, c, 0:480], c == 0, c == 2, [cqTk, "wq_b"], [f"ps{bq0}"])
            for c in range(3):
                self.MM(PS[bq1][:, 0:288], cqT[:, c, :], wq_b[:, c, 480:768], c == 0, c == 2, [cqTk, "wq_b"], [f"ps{bq1}"])
            yield
            ck, sk = cos_t[:, pj, :], sin_t[:, pj, :]
            self.TT("dve", k1, krn[:, 0:16], ck, ALU.mult, [f"{K_}krn", "C"], [f"{K_}k1"])
            self.TT("dve", k2, krn[:, 16:32], sk, ALU.mult, [f"{K_}krn", "C"], [f"{K_}k2"])
            self.TT("dve", kro[:, 0:16], k1, k2, ALU.subtract, [f"{K_}k1", f"{K_}k2"], [f"{K_}kro"])
            self.TT("dve", k1, krn[:, 16:32], ck, ALU.mult, [f"{K_}krn", "C", f"{K_}kro"], [f"{K_}k1"])
            self.TT("dve", k2, krn[:, 0:16], sk, ALU.mult, [f"{K_}krn", "C", f"{K_}kro"], [f"{K_}k2"])
            self.TT("dve", kro[:, 16:32], k1, k2, ALU.add, [f"{K_}k1", f"{K_}k2"], [f"{K_}kro"])
            km, kmk = d["km"], f"{K_}km"
            self.CP("dve", km[:, :, 64:96], kro.unsqueeze(1).to_broadcast([128, 8, 32]), [f"{K_}kro"], [kmk])
            yield
            qm, qmk = d["qm"], f"{K_}qm"
            rp, rpk = d["rp"], f"{K_}rp"
            for (bq, h0, nh, ia, ib) in ((bq0, 0, 5, 3, 5), (bq1, 5, 3, 4, 6)):
                tag = f"{K_}qm{h0}"
                src = PS[bq][:, 0:nh * 96]
                sq, sqk = nsq()
                tmpa, tmpk = ntmp()
                self.ACT(sq[:, 0:nh * 96], src, AF.Square, [f"ps{bq}"], [sqk])
                yield
                sqv = sq[:, 0:nh * 96].rearrange("p (h d) -> p h d", h=nh)
                sn = ssh[ia][:, 0:nh]
                sr = ssh[ib][:, 0:nh]
                self.RED("dve", sn, sqv[:, :, 0:64], ALU.add, [sqk], [tag + "sn"])
                self.RED("dve", sr, sqv[:, :, 64:96], ALU.add, [sqk], [tag + "sr"])
                yield
                self.rstd(sn, 64, tag + "sn")
                self.rstd(sr, 32, tag + "sr")
                yield
                sv = src.rearrange("p (h d) -> p h d", h=nh)
                self.TT("dve", tmpa[:, 0:nh, :], sv[:, :, 0:64], sn.unsqueeze(2).to_broadcast([128, nh, 64]), ALU.mult,
                        [f"ps{bq}", tag + "sn"], [tmpk])
                self.TT("dve", rp[:, h0:h0 + nh, :], sv[:, :, 64:96], sr.unsqueeze(2).to_broadcast([128, nh, 32]), ALU.mult,
                        [f"ps{bq}", tag + "sr"], [rpk])
                yield
                self.TT("pool", qm[:, h0:h0 + nh, 0:64], tmpa[:, 0:nh, :], g_qn.unsqueeze(1).to_broadcast([128, nh, 64]),
                        ALU.mult, [tmpk, "G"], [qmk])
                yield
            r1, r2 = d["r1"], d["r2"]
            cosb = cos_t[:, pj, :].unsqueeze(1).to_broadcast([128, 8, 16])
            sinb = sin_t[:, pj, :].unsqueeze(1).to_broadcast([128, 8, 16])
            self.TT("pool", rp, rp, g_qr.unsqueeze(1).to_broadcast([128, 8, 32]), ALU.mult, [rpk, "G"], [rpk])
            self.TT("pool", r1, rp[:, :, 0:16], cosb, ALU.mult, [rpk, "C"], [f"{K_}r1"])
            self.TT("pool", r2, rp[:, :, 16:32], sinb, ALU.mult, [rpk, "C"], [f"{K_}r2"])
            self.TT("pool", qm[:, :, 64:80], r1, r2, ALU.subtract, [f"{K_}r1", f"{K_}r2"], [qmk])
            self.TT("pool", r1, rp[:, :, 16:32], cosb, ALU.mult, [rpk, "C", qmk], [f"{K_}r1"])
            self.TT("pool", r2, rp[:, :, 0:16], sinb, ALU.mult, [rpk, "C", qmk], [f"{K_}r2"])
            self.TT("pool", qm[:, :, 80:96], r1, r2, ALU.add, [f"{K_}r1", f"{K_}r2"], [qmk])
            yield
            bt = nb()
            for h in range(8):
                self.TR(PSb[bt][0:96, h * 128:(h + 1) * 128], qm[:, h, :], ident_b, [qmk, "Cb"], [f"ps{bt}"])
            yield
            self.CP("act", qmT_s[mi][0:96, :, ti * 128:(ti + 1) * 128],
                    PSb[bt][0:96, 0:1024].rearrange("p (c n) -> p c n", c=8), [f"ps{bt}"], [f"qmTs{mi}"])
            yield
            bt = nb()
            for c in range(2):
                self.TR(PSb[bt][:, c * 128:(c + 1) * 128], ckvn[:, c * 128:(c + 1) * 128], ident_b, [ckvnk, "Cb"], [f"ps{bt}"])
            yield
            ckvT, ckvTk = d["ckvT"], f"{K_}ckvT"
            self.CP("dve", ckvT, PSb[bt][:, 0:256].rearrange("p (c n) -> p c n", c=2), [f"ps{bt}"], [ckvTk])
            yield
            bk0, bk1 = nb(), nb()
            for half, bk in ((0, bk0), (1, bk1)):
                for c in range(2):
                    self.MM(PS[bk][:, 0:512], ckvT[:, c, :], wkv_b[:, c, half * 512:(half + 1) * 512], c == 0, c == 1,
                            [ckvTk, "wkv_b"], [f"ps{bk}"])
            yield
            for half, bk in ((0, bk0), (1, bk1)):
                tag = f"{K_}kv{half}"
                src = PS[bk][:, 0:512]
                h0 = half * 4
                sq, sqk = nsq()
                tmpa, tmpk = ntmp()
                self.ACT(sq[:, 0:512], src, AF.Square, [f"ps{bk}"], [sqk])
                yield
                sqv = sq[:, 0:512].rearrange("p (h d) -> p h d", h=4)
                sn = ssh[2][:, half * 4:half * 4 + 4]
                self.RED("dve", sn, sqv[:, :, 0:64], ALU.add, [sqk], [tag + "sn"])
                yield
                self.rstd(sn, 64, tag + "sn")
                sv = src.rearrange("p (h d) -> p h d", h=4)
                self.CP("act", vm_s[mi][:, ti, h0:h0 + 4, :], sv[:, :, 64:128], [f"ps{bk}", tag + "sn"], [f"vms{mi}"])
                yield
                self.TT("dve", tmpa[:, 0:4, :], sv[:, :, 0:64], sn.unsqueeze(2).to_broadcast([128, 4, 64]), ALU.mult,
                        [f"ps{bk}", tag + "sn", f"vms{mi}"], [tmpk])
                yield
                self.TT("pool", km[:, h0:h0 + 4, 0:64], tmpa[:, 0:4, :], g_kn.unsqueeze(1).to_broadcast([128, 4, 64]),
                        ALU.mult, [tmpk, "G"], [kmk])
                yield
            bt = nb()
            for h in range(8):
                self.TR(PSb[bt][0:96, h * 128:(h + 1) * 128], km[:, h, :], ident_b, [kmk, "Cb"], [f"ps{bt}"])
            yield
            self.CP("act", kmT_s[mi][0:96, :, ti * 128:(ti + 1) * 128],
                    PSb[bt][0:96, 0:1024].rearrange("p (c n) -> p c n", c=8), [f"ps{bt}"], [f"kmTs{mi}"])
            yield
            if ti == 1:
                cs = slice(m * 256, (m + 1) * 256)
                self.DMA("pool", qTa_d.rearrange("(c q) n -> q c n", q=128)[:, :, cs], qaT_s[mi], [f"qaTs{mi}"], ["qTa_d"])
                self.DMA("pool", kTa_d.rearrange("(c q) n -> q c n", q=128)[:, :, cs], kaT_s[mi], [f"kaTs{mi}"], ["kTa_d"])
                self.DMA("pool", qTm_d.rearrange("(h d) n -> d h n", d=96)[:, :, cs], qmT_s[mi][0:96], [f"qmTs{mi}"], ["qTm_d"])
                self.DMA("pool", kTm_d.rearrange("(h d) n -> d h n", d=96)[:, :, cs], kmT_s[mi][0:96], [f"kmTs{mi}"], ["kTm_d"])
                self.DMA("pool", va_d[cs, :].rearrange("(t p) n -> p t n", p=128), va_s[mi], [f"vas{mi}"], ["va_d"])
                self.DMA("pool", vm_d[cs, :].rearrange("(t p) n -> p t n", p=128),
                         vm_s[mi].rearrange("p t h d -> p t (h d)"), [f"vms{mi}"], ["vm_d"])

        for m in range(NT // 2):
            gens = [tile_gen(2 * m, 0), tile_gen(2 * m + 1, 1)]
            alive = [True, True]
            while any(alive):
                for gi_, g_ in enumerate(gens):
                    if alive[gi_]:
                        try:
                            next(g_)
                        except StopIteration:
                            alive[gi_] = False

    def phase2(self, x, w_out, qTa_d, kTa_d, va_d, qTm_d, kTm_d, vm_d, mt_d, h1_d, xg_d, rt_d,
               ident_f, ident_b, causal_b, ustrict_b, ones_b, onesf, eoff, g_ffn,
               wr_sb, br_sb, slots_all, wts_all, acc_b):
        PS, PSb = self.PS, self.PSb
        sb = self.sb
        w_out_b = sb([128, 8, D], BF16)
        br_d = self.br_d
        stg = [sb([128, D], F32) for _ in range(2)]
        for c in range(8):
            self.DMA("sp", stg[c % 2], w_out[c * 128:(c + 1) * 128, :], [], [f"wstg{c%2}"])
            self.CP(("pool", "dve")[c % 2], w_out_b[:, c, :], stg[c % 2], [f"wstg{c%2}"], ["w_out_b"])
        qT = [sb([128, S], BF16) for _ in range(2)]
        kT = [sb([128, S], BF16) for _ in range(2)]
        vaug = [sb([128, TPS, 128], BF16) for _ in range(2)]
        mt = [sb([128, MTW], BF16) for _ in range(2)]
        mixTs = [sb([128, 8, S], BF16) for _ in range(2)]
        Pt = [sb([128, 512], BF16) for _ in range(4)]
        rden = [sb([128, 512], F32) for _ in range(2)]
        xr = [sb([128, D], F32) for _ in range(2)]
        h1 = [sb([128, D], F32) for _ in range(3)]
        NXB = 7
        xn2b = [sb([128, D], BF16) for _ in range(NXB)]
        xn2T = [sb([128, 8, 128], BF16) for _ in range(2)]
        junk = sb([128, D], BF16)
        junk4 = sb([128, 4], F32)
        ss2 = [sb([128, 1], F32) for _ in range(2)]
        Ls = [sb([128, 36], F32) for _ in range(3)]
        NRT = 6
        rt = [sb([128, 8], F32) for _ in range(NRT)]
        gm = sb([128, 4], F32)
        pen = sb([128, 4], F32)
        Em = sb([128, 4, 8], F32)
        top8 = sb([128, 8], F32)
        OH = [sb([128, 2, 32], F32) for _ in range(2)]
        OHb = [sb([128, 32], BF16) for _ in range(2)]
        prod = sb([128, 2, 32], F32)
        prod2 = sb([128, 2, 32], F32)
        rk = sb([128, 2], F32)
        ek = sb([128, 2], F32)
        okk = [sb([128, 2], F32) for _ in range(2)]
        slf = sb([128, 2], F32)
        wr_b = sb([128, 8, 36], BF16)
        br_bc = sb([128, 36], F32)
        self.CP("dve", wr_b, wr_sb, ["wr"], ["wr_b"])
        self.DMA("sp", br_bc, br_d[0:1, :].partition_broadcast(128)[:, 0, :], [], ["br_bc"])
        for i in range(2):
            self.MS("pool", vaug[i], 1.0, [f"vaug{i}"])

        LA = 2
        mcount = [0]

        def head_loads(b, h, i):
            tok0 = b * S
            if h < 8:
                self.DMA("sp", qT[i][0:64, :], qTa_d[h * 64:(h + 1) * 64, tok0:tok0 + S], [], [f"qT{i}"])
                self.DMA("sp", kT[i][0:64, :], kTa_d[h * 64:(h + 1) * 64, tok0:tok0 + S], [], [f"kT{i}"])
                self.DMA("sp", vaug[i][:, :, 0:64],
                         va_d[tok0:tok0 + S, h * 64:(h + 1) * 64].rearrange("(t p) d -> p t d", p=128), [], [f"vaug{i}"])
                self.DMA("sp", mt[i], mt_d[h], [], [f"mt{i}"])
            else:
                hh = h - 8
                self.DMA("sp", qT[i][0:96, :], qTm_d[hh * 96:(hh + 1) * 96, tok0:tok0 + S], [], [f"qT{i}"])
                self.DMA("sp", kT[i][0:96, :], kTm_d[hh * 96:(hh + 1) * 96, tok0:tok0 + S], [], [f"kT{i}"])
                self.DMA("sp", vaug[i][:, :, 0:64],
                         vm_d[tok0:tok0 + S, hh * 64:(hh + 1) * 64].rearrange("(t p) d -> p t d", p=128), [], [f"vaug{i}"])

        loaded = set()

        def ensure_loaded(gidx):
            if gidx in loaded or gidx >= NSEQ * 16:
                return
            loaded.add(gidx)
            head_loads(gidx // 16, gidx % 16, gidx % 2)

        def emit_S(st):
            b, h, i, qb, kt, nk, sidx = st
            if qb == 0 and kt == 0:
                ensure_loaded(b * 16 + h)
            dq = 64 if h < 8 else 96
            q0, k0 = qb * 512, kt * 128
            qlo = max(q0, k0)
            N = q0 + 512 - qlo
            sbk = 2 + sidx % 3
            self.MM(PS[sbk][:, 0:N], kT[i][0:dq, k0:k0 + 128], qT[i][0:dq, qlo:qlo + N], True, True,
                    [f"kT{i}", f"qT{i}"], [f"ps{sbk}"])

        def emit_rest(st, mixb):
            b, h, i, qb, kt, nk, sidx = st
            isA = h < 8
            scale = (64 ** -0.5) if isA else (96 ** -0.5)
            q0, k0 = qb * 512, kt * 128
            qlo = max(q0, k0)
            N = q0 + 512 - qlo
            sbk = 2 + sidx % 3
            pk = sidx % 4
            ob = qb % 2
            self.ACT(Pt[pk][:, 0:N], PS[sbk][:, 0:N], AF.Exp, [f"ps{sbk}"], [f"Pt{pk}"], scale=scale)
            if isA:
                off = qlo - k0
                eng = "dve"
                mcount[0] += 1
                self.TT(eng, Pt[pk][:, 0:N], Pt[pk][:, 0:N], mt[i][:, off:off + N], ALU.mult,
                        [f"Pt{pk}", f"mt{i}"], [f"Pt{pk}"])
            elif qlo == k0:
                self.TT("dve", Pt[pk][:, 0:128], Pt[pk][:, 0:128], causal_b, ALU.mult,
                        [f"Pt{pk}", "Cb"], [f"Pt{pk}"])
            self.MM(PS[ob][:, qlo - q0:512], vaug[i][:, kt, :], Pt[pk][:, 0:N], kt == 0, kt == nk - 1,
                    [f"vaug{i}", f"Pt{pk}"], [f"ps{ob}"])
            if kt == nk - 1:
                rd = rden[ob]
                if qb % 2 == 0:
                    self.RCP(rd[0:64, :], PS[ob][64:128, :], [f"ps{ob}"], [f"rden{ob}"])
                else:
                    self.ACT(rd[0:64, :], PS[ob][64:128, :], AF.Ln, [f"ps{ob}"], [f"rden{ob}"])
                    self.ACT(rd[0:64, :], rd[0:64, :], AF.Exp, [f"rden{ob}"], [f"rden{ob}"], scale=-1.0)
                hp = (h % 2) * 64
                self.TT("dve", mixb[0][hp:hp + 64, h // 2, q0:q0 + 512], PS[ob][0:64, :], rd[0:64, :], ALU.mult,
                        [f"ps{ob}", f"rden{ob}"], [mixb[1]])

        def outproj_stages(b, mixb):
            mixT_, mixk = mixb

            def s0(t, j):
                i2 = t % 2
                self.DMA("sp", xr[i2], x[t * 128:(t + 1) * 128, :], [], [f"xr{i2}"])
                for n in range(2):
                    bo = 5 + n
                    for c in range(8):
                        self.MM(PS[bo][:, 0:512], mixT_[:, c, j * 128:(j + 1) * 128], w_out_b[:, c, n * 512:(n + 1) * 512],
                                c == 0, c == 7, [mixk, "w_out_b"], [f"ps{bo}"])

            def s1(t, j):
                i2, i3 = t % 2, t % 3
                for n in range(2):
                    bo = 5 + n
                    self.TT("dve", h1[i3][:, n * 512:(n + 1) * 512], PS[bo][:, 0:512], xr[i2][:, n * 512:(n + 1) * 512], ALU.add,
                            [f"ps{bo}", f"xr{i2}"], [f"h1_{i3}"])

            def s2(t, j):
                i2, i3 = t % 2, t % 3
                self.DMA("pool", h1_d[t * 128:(t + 1) * 128, :], h1[i3], [f"h1_{i3}"], ["h1_d"])
                self.MS("pool", ss2[i2], 0.0, [f"ss2_{i2}"])
                self.ACT(junk, h1[i3], AF.Square, [f"h1_{i3}", f"ss2_{i2}"], [f"ss2_{i2}"], accum=ss2[i2])
                self.ACT(ss2[i2], ss2[i2], AF.Ln, [f"ss2_{i2}", "epsc"], [f"ss2_{i2}"], bias=self.epsc[:, 0:1], scale=1.0 / D)
                self.ACT(ss2[i2], ss2[i2], AF.Exp, [f"ss2_{i2}"], [f"ss2_{i2}"], scale=-0.5)

            def s3(t, j):
                i2, i3, ix = t % 2, t % 3, t % NXB
                self.STT("dve", xn2b[ix], h1[i3], ss2[i2][:, 0:1], g_ffn, ALU.mult, ALU.mult,
                         [f"h1_{i3}", f"ss2_{i2}", "G"], [f"xn2b{ix}"])

            def s4(t, j):
                i2, ix = t % 2, t % NXB
                for c in range(8):
                    self.TR(PSb[7][:, c * 128:(c + 1) * 128], xn2b[ix][:, c * 128:(c + 1) * 128], ident_b,
                            [f"xn2b{ix}", "Cb"], ["ps7"])
                self.CP("act", xn2T[i2], PSb[7][:, 0:1024].rearrange("p (c n) -> p c n", c=8), ["ps7"], [f"xn2T{i2}"])

            def s5(t, j):
                i2, il, ir = t % 2, t % 3, t % NRT
                for c in range(8):
                    self.MM(PS[7][:, 0:36], xn2T[i2][:, c, :], wr_b[:, c, :], c == 0, c == 7, [f"xn2T{i2}", "wr_b"], ["ps7"])
                self.TT("dve", Ls[il], PS[7][:, 0:36], br_bc, ALU.add, ["ps7", "br_bc"], [f"Ls{il}"])
                self.RED("dve", rt[ir][:, 0:1], Ls[il][:, 0:4], ALU.max, [f"Ls{il}"], [f"rt{ir}"])
                self.TS("dve", rt[ir][:, 1:2], rt[ir][:, 0:1], -1.0, None, ALU.mult, None, [f"rt{ir}"], [f"rt{ir}"])
                self.MS("pool", rt[ir][:, 2:3], 0.0, [f"rt{ir}g"])

            def s6(t, j):
                il, ir = t % 3, t % NRT
                self.ACT(junk4, Ls[il][:, 0:4], AF.Exp, [f"Ls{il}", f"rt{ir}", f"rt{ir}g"], [f"rt{ir}g"],
                         bias=rt[ir][:, 1:2], scale=1.0, accum=rt[ir][:, 2:3])

            def s7(t, j):
                i2, il, ir = t % 2, t % 3, t % NRT
                R = rt[ir]
                self.RCP(R[:, 3:4], R[:, 2:3], [f"rt{ir}g"], [f"rt{ir}w"])
                self.TS("dve", gm, Ls[il][:, 0:4], R[:, 0:1], None, ALU.is_ge, None, [f"Ls{il}", f"rt{ir}"], ["gm"])
                self.TS("dve", pen, gm, -1.0, 1e30, ALU.add, ALU.mult, ["gm"], ["pen"])
                self.TT("dve", Em, Ls[il][:, 4:36].rearrange("p (g e) -> p g e", g=4), pen.unsqueeze(2).to_broadcast([128, 4, 8]),
                        ALU.add, [f"Ls{il}", "pen"], ["Em"])
                Emf = Em.rearrange("p g e -> p (g e)")
                self.p.op("dve", lambda e, o_=top8, i_=Emf: e.max(out=o_, in_=i_), reads=["Em"], writes=["top8"])
                O_ = OH[i2]
                self.TS("dve", O_[:, 0, :], Emf, top8[:, 0:1], None, ALU.is_ge, None, ["Em", "top8"], [f"OH{i2}"])
                self.TS("dve", O_[:, 1, :], Emf, top8[:, 1:2], None, ALU.is_ge, None, ["Em", "top8", f"OH{i2}"], [f"OH{i2}"])
                self.CP("dve", OHb[i2], O_[:, 1, :], [f"OH{i2}"], [f"OHb{i2}"])
                self.TT("dve", O_[:, 1, :], O_[:, 1, :], O_[:, 0, :], ALU.subtract, [f"OH{i2}", f"OHb{i2}"], [f"OH{i2}"])
                self.TT("dve", R[:, 4:5], top8[:, 0:1], top8[:, 1:2], ALU.subtract, ["top8"], [f"rt{ir}d"])

            def s8(t, j):
                i2, ir = t % 2, t % NRT
                R = rt[ir]
                O_ = OH[i2]
                self.ACT(R[:, 5:6], R[:, 4:5], AF.Exp, [f"rt{ir}d"], [f"rt{ir}s"], scale=-1.0)
                self.MM(PS[7][:, 64:96], ustrict_b, OHb[i2], True, False, ["Cb", f"OHb{i2}"], ["ps7"])
                self.MM(PS[7][:, 64:96], ones_b, acc_b, False, True, ["Cb", "acc"], ["ps7"])
                self.TT("dve", prod, O_, PS[7][:, 64:96].unsqueeze(1).to_broadcast([128, 2, 32]), ALU.mult,
                        [f"OH{i2}", "ps7"], ["prod"])
                self.RED("dve", rk, prod, ALU.add, ["prod"], ["rk"])
                self.TT("dve", prod2, O_, eoff.unsqueeze(1).to_broadcast([128, 2, 32]), ALU.mult, [f"OH{i2}", "C"], ["prod2"])
                self.RED("dve", ek, prod2, ALU.add, ["prod2"], ["ek"])
                self.TT("pool", acc_b, acc_b, OHb[i2], ALU.add, ["acc", f"OHb{i2}"], ["acc"])
                ok_ = okk[i2]
                self.TS("dve", ok_, rk, float(CAP), None, ALU.is_lt, None, ["rk"], [f"okk{i2}"])
                self.TT("dve", slf, rk, ek, ALU.add, ["rk", "ek"], ["slf"])
                self.TS("dve", slf, slf, float(-TRASH), None, ALU.add, None, ["slf"], ["slf"])
                self.TT("dve", slf, slf, ok_, ALU.mult, ["slf", f"okk{i2}"], ["slf"])
                self.TS("dve", slf, slf, float(TRASH), None, ALU.add, None, ["slf"], ["slf"])
                self.CP("dve", slots_all[:, t, :], slf, ["slf"], ["slots"])

            def s9(t, j):
                i2, ir, ix = t % 2, t % NRT, t % NXB
                R = rt[ir]
                w1 = wts_all[:, t, 0:1]
                w2 = wts_all[:, t, 1:2]
                self.TS("dve", R[:, 6:7], R[:, 5:6], 1.0, None, ALU.add, None, [f"rt{ir}s"], [f"rt{ir}s2"])
                self.RCP(R[:, 6:7], R[:, 6:7], [f"rt{ir}s2"], [f"rt{ir}s2"])
                self.TT("dve", w1, R[:, 3:4], R[:, 6:7], ALU.mult, [f"rt{ir}w", f"rt{ir}s2"], ["wts"])
                self.TT("dve", w2, R[:, 3:4], w1, ALU.subtract, [f"rt{ir}w", "wts"], ["wts"])
                self.TT("dve", wts_all[:, t, :], wts_all[:, t, :], okk[i2], ALU.mult, ["wts", f"okk{i2}"], ["wts"])
                for k in range(2):
                    idx = slots_all[:, t, k:k + 1]
                    self.p.dma("pool", lambda e, idx_=idx, src_=xn2b[ix]: e.indirect_dma_start(
                        out=xg_d, out_offset=bass.IndirectOffsetOnAxis(ap=idx_, axis=0), in_=src_, in_offset=None),
                        reads=[f"xn2b{ix}", "slots"], writes=["xg_d"])

            sts = [s0, s1, s2, s3, s4, s5, s6, s7, s8, s9]
            K_ = len(sts)
            out_ = []
            for tau in range(TPS + K_ - 1):
                for jj in reversed(range(K_)):
                    j = tau - jj
                    if 0 <= j < TPS:
                        out_.append((lambda f=sts[jj], t=b * TPS + j, j=j: f(t, j)))
            return out_

        hc = 0
        pending = []
        for b in range(NSEQ):
            mixb = (mixTs[b % 2], f"mixT{b%2}")
            steps = []
            sidx = 0
            for h in range(16):
                i = hc % 2
                hc += 1
                for qb in range(4):
                    nk = 4 * qb + 4
                    for kt in range(nk):
                        steps.append((b, h, i, qb, kt, nk, sidx))
                        sidx += 1
            n = len(steps)
            every = max(1, n // (len(pending) + 1)) if pending else 0
            for k in range(min(LA, n)):
                emit_S(steps[k])
            for k in range(n):
                if k + LA < n:
                    emit_S(steps[k + LA])
                emit_rest(steps[k], mixb)
                if steps[k][3] == 0 and steps[k][4] == 0:
                    ensure_loaded(steps[k][0] * 16 + steps[k][1] + 1)
                if pending and (k % every == every - 1):
                    pending.pop(0)()
            while pending:
                pending.pop(0)()
            pending = outproj_stages(b, mixb)
        while pending:
            pending.pop(0)()
        if self.debug:
            dbgt = sb([128, NT * 4], F32)
            self.CP("dve", dbgt[:, 0:NT * 2], slots_all.rearrange("p t k -> p (t k)"), ["slots"], ["dbgt"])
            self.CP("dve", dbgt[:, NT * 2:NT * 4], wts_all.rearrange("p t k -> p (t k)"), ["wts", "dbgt"], ["dbgt"])
            self.DMA("sp", rt_d, dbgt, ["dbgt"], ["rt_d"])

    def phase3(self, w_gate, w_up, w_down, xg_d, y_d, ident_b):
        PS, PSb = self.PS, self.PSb
        sb = self.sb
        stg = [sb([128, 4096], F32) for _ in range(3)]
        wg = [sb([128, 8, FF], BF16) for _ in range(2)]
        wu = [sb([128, 8, FF], BF16) for _ in range(2)]
        wd = [sb([128, 4, D], BF16) for _ in range(2)]
        xrow = [sb([128, D], BF16) for _ in range(3)]
        xTe = [sb([128, 8, CAP], BF16) for _ in range(2)]
        hT = [sb([128, 4, CAP], BF16) for _ in range(2)]
        sg = [sb([128, 512], F32) for _ in range(2)]
        ysb = [sb([128, D], F32) for _ in range(6)]
        nst = CAP // 128
        cnt = {"xc": 0, "yc": 0, "gub": 0}

        def w_load(e, which):
            i = e % 2
            src, dst, dn, view = ((w_gate[e], wg[i], f"wg{i}", "(c p) f -> p c f"),
                                  (w_up[e], wu[i], f"wu{i}", "(c p) f -> p c f"),
                                  (w_down[e], wd[i], f"wd{i}", "(c p) n -> p c n"))[which]
            nch = dst.shape[1]
            s3 = stg[which].rearrange("p (c f) -> p c f", c=nch)
            self.DMA("sp", s3, src.rearrange(view, p=128), [], [f"stg{which}"])

        def w_cast(e, which):
            i = e % 2
            dst, dn = ((wg[i], f"wg{i}"), (wu[i], f"wu{i}"), (wd[i], f"wd{i}"))[which]
            nch = dst.shape[1]
            s3 = stg[which].rearrange("p (c f) -> p c f", c=nch)
            if which == 0:
                self.CP("act", dst, s3, [f"stg{which}"], [dn])
            elif which == 1:
                self.CP("dve", dst, s3, [f"stg{which}"], [dn])
            else:
                self.CP("pool", dst[:, 0:1, :], s3[:, 0:1, :], [f"stg{which}"], [dn + "c0"])
                self.CP("act", dst[:, 1:2, :], s3[:, 1:2, :], [f"stg{which}"], [dn + "c1"])
                self.CP("dve", dst[:, 2:3, :], s3[:, 2:3, :], [f"stg{which}"], [dn + "c2"])
                self.CP("act", dst[:, 3:4, :], s3[:, 3:4, :], [f"stg{which}"], [dn + "c3"])

        def x_trans(e):
            i = e % 2
            for s_ in range(nst):
                k = cnt["xc"] % 3
                cnt["xc"] += 1
                r0 = e * CAP + s_ * 128
                self.DMA("sp", xrow[k], xg_d[r0:r0 + 128, :], [], [f"xrow{k}"])
                bt = cnt["xc"] % 2
                for c in range(8):
                    self.TR(PSb[bt][:, c * 128:(c + 1) * 128], xrow[k][:, c * 128:(c + 1) * 128], ident_b,
                            [f"xrow{k}", "Cb"], [f"ps{bt}"])
                self.CP(("act", "dve")[s_ % 2], xTe[i][:, :, s_ * 128:(s_ + 1) * 128],
                        PSb[bt][:, 0:1024].rearrange("p (c n) -> p c n", c=8), [f"ps{bt}"], [f"xTe{i}"])

        def gate_up(e, ffc):
            i = e % 2
            for (n0, N) in ((0, 512), (512, CAP - 512)):
                bg = 2 + (cnt["gub"] % 2) * 2
                bu = bg + 1
                cnt["gub"] += 1
                for c in range(8):
                    self.MM(PS[bg][:, 0:N], wg[i][:, c, ffc * 128:(ffc + 1) * 128], xTe[i][:, c, n0:n0 + N],
                            c == 0, c == 7, [f"wg{i}", f"xTe{i}"], [f"ps{bg}"])
                for c in range(8):
                    self.MM(PS[bu][:, 0:N], wu[i][:, c, ffc * 128:(ffc + 1) * 128], xTe[i][:, c, n0:n0 + N],
                            c == 0, c == 7, [f"wu{i}", f"xTe{i}"], [f"ps{bu}"])
                sgi = cnt["gub"] % 2
                self.ACT(sg[sgi][:, 0:N], PS[bg][:, 0:N], AF.Silu, [f"ps{bg}"], [f"sg{sgi}"])
                self.TT("dve", hT[i][:, ffc, n0:n0 + N], sg[sgi][:, 0:N], PS[bu][:, 0:N], ALU.mult,
                        [f"sg{sgi}", f"ps{bu}"], [f"hT{i}"])

        def down(e):
            i = e % 2
            for s_ in range(nst):
                k = cnt["yc"] % 6
                cnt["yc"] += 1
                for n in range(2):
                    by = 6 + n
                    for c in range(4):
                        self.MM(PS[by][:, 0:512], hT[i][:, c, s_ * 128:(s_ + 1) * 128], wd[i][:, c, n * 512:(n + 1) * 512],
                                c == 0, c == 3, [f"hT{i}", f"wd{i}c{c}"], [f"ps{by}"])
                    self.CP(("act", "dve")[n], ysb[k][:, n * 512:(n + 1) * 512], PS[by][:, 0:512], [f"ps{by}"], [f"ysb{k}"])
                r0 = e * CAP + s_ * 128
                self.DMA("pool", y_d[r0:r0 + 128, :], ysb[k], [f"ysb{k}"], ["y_d"])

        for w in range(3):
            w_load(0, w)
            w_cast(0, w)
        x_trans(0)
        for e in range(NE):
            nxt = e + 1 < NE
            if nxt:
                for w in range(3):
                    w_load(e + 1, w)
            for ffc in range(4):
                gate_up(e, ffc)
                if nxt and ffc < 3:
                    w_cast(e + 1, ffc)
            if nxt:
                x_trans(e + 1)
            down(e)

    def phase4(self, pin, w_ple_proj, w_ple_gate, b_ple_gate, h1_d, y_d, out, ident_b, ones_b, g_ple,
               slots_all, wts_all):
        PS, PSb = self.PS, self.PSb
        sb = self.sb
        wpg = sb([128, 8, D], BF16)
        wpp = sb([128, 2, D], BF16)
        stg = [sb([128, D], F32) for _ in range(2)]
        for c in range(8):
            self.DMA("sp", stg[c % 2], w_ple_gate[c * 128:(c + 1) * 128, :], [], [f"wstg{c%2}"])
            self.CP(("pool", "dve")[c % 2], wpg[:, c, :], stg[c % 2], [f"wstg{c%2}"], ["wpg"])
        for c in range(2):
            self.DMA("sp", stg[c % 2], w_ple_proj[c * 128:(c + 1) * 128, :], [], [f"wstg{c%2}"])
            self.CP(("pool", "dve")[c % 2], wpp[:, c, :], stg[c % 2], [f"wstg{c%2}"], ["wpp"])
        bf = sb([1, D], F32)
        bhi = sb([1, D], BF16)
        blo = sb([1, D], BF16)
        bt_ = sb([1, D], F32)
        self.DMA("sp", bf[0:1, :], b_ple_gate, [], ["bf"])
        self.CP("dve", bhi[0:1, :], bf[0:1, :], ["bf"], ["bhi"])
        self.TT("dve", bt_[0:1, :], bf[0:1, :], bhi[0:1, :], ALU.subtract, ["bf", "bhi"], ["bt_"])
        self.CP("dve", blo[0:1, :], bt_[0:1, :], ["bt_"], ["blo"])
        NH = 9
        H = [sb([128, D], F32) for _ in range(NH)]
        y1 = [sb([128, D], F32) for _ in range(2)]
        y2 = [sb([128, D], F32) for _ in range(2)]
        pt = [sb([128, PLE], F32) for _ in range(2)]
        pb = [sb([128, PLE], BF16) for _ in range(2)]
        pT = [sb([128, 2, 128], BF16) for _ in range(2)]
        h2b = [sb([128, D], BF16) for _ in range(2)]
        h2T = [sb([128, 8, 128], BF16) for _ in range(2)]
        ev = [sb([128, D], F32) for _ in range(2)]
        NG = 5
        gt = [sb([128, D], F32) for _ in range(NG)]
        junk = sb([128, D], BF16)
        ssall = sb([128, NT, 2], F32)
        rsall = sb([128, NT], F32)

        def sw_pipe(ntiles, sts):
            K_ = len(sts)
            for tau in range(ntiles + K_ - 1):
                for jj in reversed(range(K_)):
                    t = tau - jj
                    if 0 <= t < ntiles:
                        sts[jj](t)

        self.MS("pool", ssall, 0.0, ["ssall"])

        def a0(t):
            i = t % 2
            self.DMA("sp", pt[i], pin[t * 128:(t + 1) * 128, :], [], [f"pt{i}"])

        def a1(t):
            i = t % 2
            self.CP("pool", pb[i], pt[i], [f"pt{i}"], [f"pb{i}"])

        def a2(t):
            i = t % 2
            for c in range(2):
                self.TR(PSb[0][:, c * 128:(c + 1) * 128], pb[i][:, c * 128:(c + 1) * 128], ident_b, [f"pb{i}", "Cb"], ["ps0"])
            self.CP("act", pT[i], PSb[0][:, 0:256].rearrange("p (c n) -> p c n", c=2), ["ps0"], [f"pT{i}"])

        def a3(t):
            i = t % 2
            for n in range(2):
                be = 2 + n
                for c in range(2):
                    self.MM(PS[be][:, 0:512], pT[i][:, c, :], wpp[:, c, n * 512:(n + 1) * 512], c == 0, c == 1,
                            [f"pT{i}", "wpp"], [f"ps{be}"])

        def a4(t):
            for n in range(2):
                be = 2 + n
                self.ACT(junk[:, 0:512], PS[be][:, 0:512], AF.Square, [f"ps{be}", "ssall"], ["ssall"],
                         accum=ssall[:, t, n:n + 1])

        sw_pipe(NT, [a0, a1, a2, a3, a4])
        self.TT("dve", rsall, ssall[:, :, 0], ssall[:, :, 1], ALU.add, ["ssall"], ["rsall"])
        self.ACT(rsall, rsall, AF.Sqrt, ["rsall", "epsc"], ["rsall"], bias=self.epsc[:, 0:1], scale=1.0 / D)
        self.RCP(rsall, rsall, ["rsall"], ["rsall"])

        def s0(t):
            i, ih = t % 2, t % NH
            self.DMA("sp", H[ih], h1_d[t * 128:(t + 1) * 128, :], [], [f"h{ih}"])
            self.DMA("sp", pt[i], pin[t * 128:(t + 1) * 128, :], [], [f"pt{i}"])
            for k, Y in ((0, y1[i]), (1, y2[i])):
                idx = slots_all[:, t, k:k + 1]
                self.p.dma("pool", lambda e, idx_=idx, dst_=Y: e.indirect_dma_start(
                    out=dst_, out_offset=None, in_=y_d, in_offset=bass.IndirectOffsetOnAxis(ap=idx_, axis=0)),
                    reads=["slots", "y_d"], writes=[f"y{k}_{i}"])

        def s1(t):
            i, ih = t % 2, t % NH
            self.CP("pool", pb[i], pt[i], [f"pt{i}"], [f"pb{i}"])
            self.STT("dve", H[ih], y1[i], wts_all[:, t, 0:1], H[ih], ALU.mult, ALU.add, [f"y0_{i}", f"h{ih}", "wts"], [f"h{ih}"])
            self.STT("dve", H[ih], y2[i], wts_all[:, t, 1:2], H[ih], ALU.mult, ALU.add, [f"y1_{i}", f"h{ih}", "wts"], [f"h{ih}"])

        def s2(t):
            i, ih = t % 2, t % NH
            for c in range(2):
                self.TR(PSb[0][:, c * 128:(c + 1) * 128], pb[i][:, c * 128:(c + 1) * 128], ident_b, [f"pb{i}", "Cb"], ["ps0"])
            self.CP("act", pT[i], PSb[0][:, 0:256].rearrange("p (c n) -> p c n", c=2), ["ps0"], [f"pT{i}"])
            self.CP("act", h2b[i], H[ih], [f"h{ih}"], [f"h2b{i}"])

        def s3(t):
            i = t % 2
            for c in range(8):
                self.TR(PSb[1][:, c * 128:(c + 1) * 128], h2b[i][:, c * 128:(c + 1) * 128], ident_b, [f"h2b{i}", "Cb"], ["ps1"])
            self.CP("act", h2T[i], PSb[1][:, 0:1024].rearrange("p (c n) -> p c n", c=8), ["ps1"], [f"h2T{i}"])
            for n in range(2):
                be = 2 + n
                for c in range(2):
                    self.MM(PS[be][:, 0:512], pT[i][:, c, :], wpp[:, c, n * 512:(n + 1) * 512], c == 0, c == 1,
                            [f"pT{i}", "wpp"], [f"ps{be}"])

        def s4(t):
            i = t % 2
            for n in range(2):
                be = 2 + n
                self.STT("dve", ev[i][:, n * 512:(n + 1) * 512], PS[be][:, 0:512], rsall[:, t:t + 1], g_ple[:, n * 512:(n + 1) * 512],
                         ALU.mult, ALU.mult, [f"ps{be}", "rsall", "G"], [f"ev{i}"])
            for n in range(2):
                bg = 4 + n
                for c in range(8):
                    self.MM(PS[bg][:, 0:512], h2T[i][:, c, :], wpg[:, c, n * 512:(n + 1) * 512], c == 0, False,
                            [f"h2T{i}", "wpg"], [f"ps{bg}"])
                self.MM(PS[bg][:, 0:512], ones_b[0:1, :], bhi[0:1, n * 512:(n + 1) * 512], False, False, ["Cb", "bhi"], [f"ps{bg}"])
                self.MM(PS[bg][:, 0:512], ones_b[0:1, :], blo[0:1, n * 512:(n + 1) * 512], False, True, ["Cb", "blo"], [f"ps{bg}"])

        def s5(t):
            ig = t % NG
            for n in range(2):
                bg = 4 + n
                self.ACT(gt[ig][:, n * 512:(n + 1) * 512], PS[bg][:, 0:512], AF.Sigmoid, [f"ps{bg}"], [f"gt{ig}"])

        def s6(t):
            i, ig = t % 2, t % NG
            Gt, E = gt[ig], ev[i]
            self.TT("pool", Gt[:, 0:384], Gt[:, 0:384], E[:, 0:384], ALU.mult, [f"gt{ig}", f"ev{i}"], [f"gt{ig}a"])
            self.TT("dve", Gt[:, 384:1024], Gt[:, 384:1024], E[:, 384:1024], ALU.mult, [f"gt{ig}", f"ev{i}"], [f"gt{ig}b"])

        def s7(t):
            ig, ih = t % NG, t % NH
            self.TT("dve", gt[ig], gt[ig], H[ih], ALU.add, [f"gt{ig}", f"gt{ig}a", f"gt{ig}b", f"h{ih}"], [f"gt{ig}"])

        def s8(t):
            ig = t % NG
            self.DMA("sp", out[t * 128:(t + 1) * 128, :], gt[ig], [f"gt{ig}"], ["out"])

        sw_pipe(NT, [s0, s1, s2, s3, s4, s5, s6, s7, s8])


def _t5_bucket(d):
    max_exact = 16
    n = np.maximum(d, 0)
    nf = np.maximum(n, 1).astype(np.float32)
    large = max_exact + (np.log(nf / np.float32(max_exact)) / np.float32(math.log(2048 / max_exact))
                         * np.float32(32 - max_exact)).astype(np.int32)
    large = np.minimum(large, 31)
    return np.where(n < max_exact, n, large)


def _constants():
    c = np.zeros((128, 1024), np.float32)
    c[:, 0:128] = np.eye(128, dtype=np.float32)
    jj = np.arange(128)[:, None]
    cc = np.arange(128)[None, :]
    c[:, 128:256] = (cc >= jj).astype(np.float32)
    c[:, 256:384] = (jj < cc).astype(np.float32)
    c[:, 384:416] = (np.arange(32) * CAP).astype(np.float32)[None, :]
    half = 16
    inv = (1.0 / (np.float32(10000.0) ** (np.arange(half, dtype=np.float32) * np.float32(2.0) / np.float32(32)))).astype(np.float32)
    pos = np.arange(S, dtype=np.float32)
    ang = (pos[:, None] * inv[None, :]).astype(np.float32)
    cos = np.cos(ang).astype(np.float32).reshape(16, 128, 16).transpose(1, 0, 2).reshape(128, 256)
    sin = np.sin(ang).astype(np.float32).reshape(16, 128, 16).transpose(1, 0, 2).reshape(128, 256)
    c[:, 512:768] = cos
    c[:, 768:1024] = sin
    dist = np.arange(MTW)[None, :] - np.arange(128)[:, None]
    valid = (dist >= 0) & (dist < S)
    d = np.clip(dist, 0, S - 1)
    mult = np.zeros(d.shape, np.float32)
    mult += (d <= 128)
    mult += ((d % 4 == 0) & (d <= 512))
    mult += (d % 16 == 0)
    mult = np.where(valid, mult, 0.0).astype(np.float32)
    bucket = _t5_bucket(d)
    return c, mult, bucket, valid


_CACHE = {}


def _get_program(debug=False, stop_after=99):
    key = (debug, stop_after)
    if key not in _CACHE:
        b = Builder(debug=debug, stop_after=stop_after)
        _CACHE[key] = b.build()
    return _CACHE[key]


def _prep_inputs(inputs):
    f = lambda k: np.asarray(inputs[k], dtype=np.float32)
    x = f("x").reshape(32 * S, D)
    pp = f("p").reshape(32 * S, PLE)
    cst, mult, bucket, valid = _constants()
    rel_bias = f("rel_bias")
    bt = rel_bias[bucket]
    bt = np.where(valid[:, :, None], bt, np.float32(0.0))
    bias_toep = np.ascontiguousarray(bt.transpose(2, 0, 1)).astype(np.float32)
    gv = np.zeros((1, 4096), np.float32)
    segs = [("norm_mix_gain", 0), ("norm_ffn_gain", 1024), ("ple_norm_gain", 2048), ("qn_a_gain", 3072),
            ("kn_a_gain", 3136), ("q_a_gain", 3200), ("kv_a_gain", 3584), ("qn_nope_gain", 3840),
            ("qn_rope_gain", 3904), ("kn_nope_gain", 3936), ("kn_rope_gain", 4000)]
    for k, o in segs:
        v = f(k).reshape(-1)
        gv[0, o:o + v.size] = v
    wr = np.concatenate([f("w_router_group")[0], f("w_router_expert")[0]], axis=1)
    br = np.concatenate([f("b_router_group")[0], f("b_router_expert")[0]], axis=0)[None, :]
    shared = {
        "w_in": f("w_in")[0], "w_q_up": f("w_q_up")[0], "w_kv_up": f("w_kv_up")[0], "w_out": f("w_out")[0],
        "wr": np.ascontiguousarray(wr), "br": np.ascontiguousarray(br),
        "w_exp_gate": f("w_exp_gate")[0], "w_exp_up": f("w_exp_up")[0], "w_exp_down": f("w_exp_down")[0],
        "w_ple_proj": f("w_ple_proj")[0], "w_ple_gate": f("w_ple_gate")[0], "b_ple_gate": f("b_ple_gate"),
        "gvec": gv, "cst": cst, "bias_toep": bias_toep, "mult_toep": mult,
    }
    in_maps = []
    for c in range(NCORES):
        m = dict(shared)
        m["x"] = x[c * NTOK:(c + 1) * NTOK]
        m["p"] = pp[c * NTOK:(c + 1) * NTOK]
        in_maps.append(m)
    return in_maps


def kernel(**inputs):
    nc = _get_program()
    in_maps = _prep_inputs(inputs)
    res = run_bass_kernel_spmd(nc, in_maps, core_ids=list(range(NCORES)))
    outs = [np.asarray(r["out"], dtype=np.float32) for r in res.results]
    return np.concatenate(outs, axis=0).reshape(32, S, D)
```

```python
import contextlib
import math
import numpy as np
import ml_dtypes
import concourse.bass as bass
import concourse.mybir as mybir
from concourse.bass_utils import run_bass_kernel_spmd

F32 = mybir.dt.float32
BF16 = mybir.dt.bfloat16
I32 = mybir.dt.int32
U8 = mybir.dt.uint8
ALU = mybir.AluOpType
AF = mybir.ActivationFunctionType
AX = mybir.AxisListType

NCORES = 8
D = 1024
S = 2048
NSEQ = 4
NTOK = NSEQ * S
NT = NTOK // 128
TPS = S // 128
INC = 2208
PLE = 256
NE = 32
FF = 512
CAP = 768
NSLOT = NE * CAP
TRASH = NSLOT
MTW = 2176
EPS = 1e-6
ENGS = ("pe", "act", "dve", "pool", "sp")


class _Op:
    __slots__ = ("eng", "fn", "deps", "is_dma", "idx", "signal", "semi", "semval", "dsem", "dval", "dprev")

    def __init__(self, eng, fn, is_dma, idx):
        self.eng = eng
        self.fn = fn
        self.deps = []
        self.is_dma = is_dma
        self.idx = idx
        self.signal = False
        self.semi = 0
        self.semval = 0
        self.dsem = None
        self.dval = 0
        self.dprev = 0


class Prog:
    EPOCH = 20000
    NDMA = 16

    def __init__(self, nc):
        self.nc = nc
        self.ops = []
        self.last_w = {}
        self.readers = {}
        self.bar_deps = None
        self.bar_seen = set()
        self.last_eng = {}
        self.dma_since = []

    def _add(self, eng, fn, reads, writes, is_dma):
        op = _Op(eng, fn, is_dma, len(self.ops))
        deps = {}
        for r in reads:
            w = self.last_w.get(r)
            if w is not None:
                deps[w.idx] = w
        for w_ in writes:
            w = self.last_w.get(w_)
            if w is not None:
                deps[w.idx] = w
            for rd in self.readers.get(w_, ()):
                deps[rd.idx] = rd
        for r in reads:
            self.readers.setdefault(r, []).append(op)
        for w_ in writes:
            self.last_w[w_] = op
            self.readers[w_] = []
        if self.bar_deps is not None and eng not in self.bar_seen:
            self.bar_seen.add(eng)
            for d in self.bar_deps:
                deps[d.idx] = d
        deps.pop(op.idx, None)
        for d in deps.values():
            if (not d.is_dma) and (not is_dma) and d.eng == "pe" and eng == "pe":
                continue
            op.deps.append(d)
        self.ops.append(op)
        if is_dma:
            self.dma_since.append(op)
        else:
            self.last_eng[eng] = op
        return op

    def op(self, eng, fn, reads=(), writes=()):
        return self._add(eng, fn, reads, writes, False)

    def dma(self, eng, fn, reads=(), writes=()):
        return self._add(eng, fn, reads, writes, True)

    def barrier(self):
        deps = list(self.last_eng.values()) + list(self.dma_since)
        self.bar_deps = deps
        self.bar_seen = set()
        self.last_w = {}
        self.readers = {}

    def emit(self, final_wait_ops=()):
        nc = self.nc
        ops = self.ops
        for o in ops:
            for d in o.deps:
                d.signal = True
        for o in final_wait_ops:
            o.signal = True
        cnt = {e: 0 for e in ENGS}
        for o in ops:
            if o.is_dma:
                continue
            if o.signal:
                c = cnt[o.eng]
                o.semi = c // self.EPOCH
                o.semval = c % self.EPOCH + 1
                cnt[o.eng] = c + 1
        nsem = {e: (cnt[e] + self.EPOCH - 1) // self.EPOCH for e in ENGS}
        n_sw = 6
        n_hw = self.NDMA - n_sw
        dcount = {"hw": 0, "sw": 0}
        dlast = [0] * self.NDMA
        for o in ops:
            if o.is_dma:
                if o.eng == "pool":
                    k = n_hw + dcount["sw"] % n_sw
                    dcount["sw"] += 1
                else:
                    k = dcount["hw"] % n_hw
                    dcount["hw"] += 1
                o.dsem = k
                o.dprev = dlast[k]
                dlast[k] += 16
                o.dval = dlast[k]
        with contextlib.ExitStack() as st:
            csem = {e: [st.enter_context(nc.semaphore(f"c_{e}_{i}")) for i in range(nsem[e])] for e in ENGS}
            dsem = [st.enter_context(nc.semaphore(f"d_{i}")) for i in range(self.NDMA)]
            block = st.enter_context(nc.Block())
            per_eng = {e: [o for o in ops if o.eng == e] for e in ENGS}
            finals = list(final_wait_ops)

            def run(e, engobj):
                known_c = {}
                known_d = {}

                def wait_for(d):
                    if d.is_dma:
                        if known_d.get(d.dsem, 0) >= d.dval:
                            return
                        engobj.wait_ge(dsem[d.dsem], d.dval)
                        known_d[d.dsem] = d.dval
                    else:
                        key = (d.eng, d.semi)
                        if known_c.get(key, 0) >= d.semval:
                            return
                        engobj.wait_ge(csem[d.eng][d.semi], d.semval)
                        known_c[key] = d.semval

                def reduce_deps(deps):
                    best = {}
                    for d in deps:
                        k = ("d", d.dsem) if d.is_dma else ("c", d.eng, d.semi)
                        v = d.dval if d.is_dma else d.semval
                        if k not in best or v > best[k][0]:
                            best[k] = (v, d)
                    return [bd[1] for bd in best.values()]

                for o in per_eng[e]:
                    for d in sorted(reduce_deps(o.deps), key=lambda z: z.idx):
                        wait_for(d)
                    if o.is_dma:
                        if o.dprev > 0 and known_d.get(o.dsem, 0) < o.dprev:
                            engobj.wait_ge(dsem[o.dsem], o.dprev)
                            known_d[o.dsem] = o.dprev
                        ins = o.fn(engobj)
                        ins.then_inc(dsem[o.dsem], 16)
                    else:
                        ins = o.fn(engobj)
                        if o.signal:
                            ins.then_inc(csem[o.eng][o.semi], 1)
                if e == "sp":
                    for o in reduce_deps(finals):
                        wait_for(o)

            @block.tensor
            def _(eng):
                run("pe", eng)

            @block.scalar
            def _(eng):
                run("act", eng)

            @block.vector
            def _(eng):
                run("dve", eng)

            @block.gpsimd
            def _(eng):
                run("pool", eng)

            @block.sync
            def _(eng):
                run("sp", eng)


class Builder:
    def __init__(self, debug=False, stop_after=99):
        self.debug = debug
        self.stop_after = stop_after
        self.nc = bass.Bass("TRN2", target_bir_lowering=False)
        self.p = Prog(self.nc)
        self.arena_off = 0
        self.rr = 0
        self.out_ops = []

    def din(self, name, shape, dt=F32):
        return self.nc.dram_tensor(name, list(shape), dt, kind="ExternalInput").ap()

    def dscr(self, name, shape, dt, out=False):
        kind = "ExternalOutput" if (out or (self.debug and name in self.debug)) else "Internal"
        return self.nc.dram_tensor(name, list(shape), dt, kind=kind).ap()

    def sb(self, shape, dt):
        esz = {F32: 4, BF16: 2, I32: 4}[dt]
        n = 1
        for s in shape[1:]:
            n *= s
        nbytes = (n * esz + 63) // 64 * 64
        off = self.arena_off
        self.arena_off += nbytes
        assert self.arena_off <= self.arena_bytes, ("SBUF arena overflow", self.arena_off)
        v = self.arena[:, off:off + n * esz].bitcast(dt)
        if len(shape) == 3:
            v = v.rearrange("p (a b) -> p a b", a=shape[1])
        elif len(shape) == 4:
            v = v.rearrange("p (a b c) -> p a b c", a=shape[1], b=shape[2])
        return v

    def DMA(self, q, out, in_, r, w):
        o = self.p.dma(q, lambda e: e.dma_start(out=out, in_=in_), reads=r, writes=w)
        if "out" in w or (self.debug and any(k in self.debug for k in w)):
            self.out_ops.append(o)
        return o

    def MM(self, out, lhsT, rhs, start, stop, r, w):
        return self.p.op("pe", lambda e: e.matmul(out, lhsT=lhsT, rhs=rhs, start=start, stop=stop,
                                                  skip_group_check=True), reads=r, writes=w)

    def TR(self, out, in_, ident, r, w):
        return self.p.op("pe", lambda e: e.transpose(out=out, in_=in_, identity=ident), reads=r, writes=w)

    def ACT(self, out, in_, func, r, w, bias=None, scale=None, accum=None):
        kw = {}
        if bias is not None:
            kw["bias"] = bias
        if scale is not None:
            kw["scale"] = scale
        if accum is not None:
            kw["accum_out"] = accum
        return self.p.op("act", lambda e: e.activation(out=out, in_=in_, func=func, **kw), reads=r, writes=w)

    def CP(self, eng, out, in_, r, w):
        if eng == "act":
            return self.p.op("act", lambda e: e.copy(out=out, in_=in_), reads=r, writes=w)
        return self.p.op(eng, lambda e: e.tensor_copy(out=out, in_=in_), reads=r, writes=w)

    def TT(self, eng, out, in0, in1, op, r, w):
        return self.p.op(eng, lambda e: e.tensor_tensor(out=out, in0=in0, in1=in1, op=op), reads=r, writes=w)

    def TS(self, eng, out, in0, s1, s2, op0, op1, r, w):
        if op1 is None:
            return self.p.op(eng, lambda e: e.tensor_scalar(out=out, in0=in0, scalar1=s1, scalar2=None, op0=op0),
                             reads=r, writes=w)
        return self.p.op(eng, lambda e: e.tensor_scalar(out=out, in0=in0, scalar1=s1, scalar2=s2, op0=op0, op1=op1),
                         reads=r, writes=w)

    def STT(self, eng, out, in0, scalar, in1, op0, op1, r, w):
        return self.p.op(eng, lambda e: e.scalar_tensor_tensor(out=out, in0=in0, scalar=scalar, in1=in1,
                                                               op0=op0, op1=op1), reads=r, writes=w)

    def RED(self, eng, out, in_, op, r, w):
        return self.p.op(eng, lambda e: e.tensor_reduce(out=out, in_=in_, axis=AX.X, op=op), reads=r, writes=w)

    def RCP(self, out, in_, r, w):
        return self.p.op("dve", lambda e: e.reciprocal(out=out, in_=in_), reads=r, writes=w)

    def MS(self, eng, out, val, w, r=()):
        return self.p.op(eng, lambda e: e.memset(out, val), reads=r, writes=w)

    def rstd(self, ss, n, nm, r_extra=()):
        self.ACT(ss, ss, AF.Ln, [nm, "epsc"] + list(r_extra), [nm], bias=self.epsc[:, 0:1], scale=1.0 / n)
        self.ACT(ss, ss, AF.Exp, [nm], [nm], scale=-0.5)

    def build(self):
        nc = self.nc
        dbg = self.debug
        x = self.din("x", [NTOK, D])
        pin = self.din("p", [NTOK, PLE])
        w_in = self.din("w_in", [D, INC])
        w_q_up = self.din("w_q_up", [384, 768])
        w_kv_up = self.din("w_kv_up", [256, 1024])
        w_out = self.din("w_out", [D, D])
        wr = self.din("wr", [D, 36])
        br = self.din("br", [1, 36])
        self.br_d = br
        w_gate = self.din("w_exp_gate", [NE, D, FF])
        w_up = self.din("w_exp_up", [NE, D, FF])
        w_down = self.din("w_exp_down", [NE, FF, D])
        w_ple_proj = self.din("w_ple_proj", [PLE, D])
        w_ple_gate = self.din("w_ple_gate", [D, D])
        b_ple_gate = self.din("b_ple_gate", [1, D])
        gvec = self.din("gvec", [1, 4096])
        cst = self.din("cst", [128, 1024])
        bias_toep = self.din("bias_toep", [8, 128, MTW])
        mult_toep = self.din("mult_toep", [128, MTW])
        out = self.nc.dram_tensor("out", [NTOK, D], F32, kind="ExternalOutput").ap()
        qTa_d = self.dscr("qTa_d", [512, NTOK], BF16)
        kTa_d = self.dscr("kTa_d", [512, NTOK], BF16)
        va_d = self.dscr("va_d", [NTOK, 512], BF16)
        qTm_d = self.dscr("qTm_d", [768, NTOK], BF16)
        kTm_d = self.dscr("kTm_d", [768, NTOK], BF16)
        vm_d = self.dscr("vm_d", [NTOK, 512], BF16)
        mt_d = self.dscr("mt_d", [8, 128, MTW], BF16)
        h1_d = self.dscr("h1_d", [NTOK, D], F32)
        xg_d = self.dscr("xg_d", [NSLOT + 128, D], BF16)
        y_d = self.dscr("y_d", [NSLOT + 128, D], F32)
        rt_d = self.dscr("rt_d", [128, NT * 4], F32, out=False)

        with contextlib.ExitStack() as st:
            self.arena_bytes = 200 * 1024
            self.arena = st.enter_context(nc.sbuf_tensor("arena", [128, self.arena_bytes], U8))
            PS = [st.enter_context(nc.psum_tensor(f"ps{i}", [128, 512], F32)) for i in range(8)]
            PSb = [t[:, :].bitcast(BF16) for t in PS]
            PS = [t[:, :] for t in PS]
            self.PS, self.PSb = PS, PSb

            C = self.sb([128, 1024], F32)
            self.C = C
            ident_f = C[:, 0:128]
            eoff = C[:, 384:416]
            cos_t = C[:, 512:768].rearrange("p (t i) -> p t i", t=16)
            sin_t = C[:, 768:1024].rearrange("p (t i) -> p t i", t=16)
            Cb = self.sb([128, 512], BF16)
            ident_b = Cb[:, 0:128]
            causal_b = Cb[:, 128:256]
            ustrict_b = Cb[:, 256:384]
            ones_b = Cb[:, 384:512]
            onesf = self.sb([128, 128], F32)
            G = self.sb([128, 4096], F32)
            self.epsc = self.sb([128, 1], F32)
            g_mix = G[:, 0:1024]
            g_ffn = G[:, 1024:2048]
            g_ple = G[:, 2048:3072]
            g_qa = G[:, 3072:3136]
            g_ka = G[:, 3136:3200]
            g_cq = G[:, 3200:3584]
            g_ckv = G[:, 3584:3840]
            g_qn = G[:, 3840:3904]
            g_qr = G[:, 3904:3936]
            g_kn = G[:, 3936:4000]
            g_kr = G[:, 4000:4032]
            slots_all = self.sb([128, NT, 2], I32)
            wts_all = self.sb([128, NT, 2], F32)
            acc_b = self.sb([128, 32], BF16)
            wr_sb = self.sb([128, 8, 36], F32)
            br_sb = self.sb([1, 36], F32)
            persist_mark = self.arena_off

            p = self.p
            self.DMA("sp", C, cst, [], ["C"])
            self.DMA("sp", G, gvec[0:1, :].partition_broadcast(128)[:, 0, :], [], ["G"])
            self.DMA("sp", wr_sb, wr.rearrange("(c p) n -> p c n", p=128), [], ["wr"])
            self.DMA("sp", br_sb[0:1, :], br, [], ["br"])
            self.MS("pool", self.epsc, EPS, ["epsc"])
            self.MS("pool", onesf, 1.0, ["onesf"])
            self.MS("pool", acc_b, 0.0, ["acc"])
            self.MS("pool", Cb[:, 384:512], 1.0, ["Cb"])
            self.CP("dve", Cb[:, 0:384], C[:, 0:384], ["C", "Cb"], ["Cb"])
            self.MS("pool", slots_all, 0, ["slots"])
            self.MS("pool", wts_all, 0.0, ["wts"])

            w_in_b = self.sb([128, 8, INC], BF16)
            wq_b = self.sb([128, 3, 768], BF16)
            wkv_b = self.sb([128, 2, 1024], BF16)
            zt = self.sb([128, 7, 1024], BF16)
            ztf = self.sb([128, 1024], F32)
            self.MS("pool", zt, 0.0, ["zt"])
            self.MS("pool", ztf, 0.0, ["ztf"])
            p1_mark = self.arena_off
            stg = [self.sb([128, INC], F32) for _ in range(2)]
            for c in range(8):
                s_ = stg[c % 2]
                self.DMA("sp", s_, w_in[c * 128:(c + 1) * 128, :], [], [f"stg{c%2}"])
                self.CP(("pool", "dve")[c % 2], w_in_b[:, c, :], s_, [f"stg{c%2}"], ["w_in_b"])
            for c in range(3):
                s_ = stg[c % 2]
                self.DMA("sp", s_[:, 0:768], w_q_up[c * 128:(c + 1) * 128, :], [], [f"stg{c%2}"])
                self.CP(("pool", "dve")[c % 2], wq_b[:, c, :], s_[:, 0:768], [f"stg{c%2}"], ["wq_b"])
            for c in range(2):
                s_ = stg[c % 2]
                self.DMA("sp", s_[:, 0:1024], w_kv_up[c * 128:(c + 1) * 128, :], [], [f"stg{c%2}"])
                self.CP(("pool", "dve")[c % 2], wkv_b[:, c, :], s_[:, 0:1024], [f"stg{c%2}"], ["wkv_b"])
            mult_sb = self.sb([128, MTW], F32)
            self.DMA("sp", mult_sb, mult_toep, [], ["mult"])
            mtb = [self.sb([128, MTW], BF16) for _ in range(2)]
            for h in range(8):
                s_ = stg[h % 2]
                self.DMA("sp", s_[:, 0:MTW], bias_toep[h], [], [f"stg{h%2}"])
                self.ACT(s_[:, 0:MTW], s_[:, 0:MTW], AF.Exp, [f"stg{h%2}"], [f"stg{h%2}"])
                self.TT("dve", mtb[h % 2], s_[:, 0:MTW], mult_sb, ALU.mult, [f"stg{h%2}", "mult"], [f"mtb{h%2}"])
                self.DMA("pool", mt_d[h], mtb[h % 2], [f"mtb{h%2}"], ["mt_d"])
            xg_v = xg_d.rearrange("(r p) n -> p r n", p=128)
            nrt = (NSLOT + 128) // 128
            self.zero_jobs = [(xg_v[:, i:i + min(7, nrt - i), :], zt[:, 0:min(7, nrt - i), :], "zt", "xg_d")
                              for i in range(0, nrt, 7)]
            self.zero_jobs.append((y_d[NSLOT:NSLOT + 128, :], ztf, "ztf", "y_d"))
            p.barrier()
            self.arena_off = p1_mark

            if self.stop_after >= 1:
                self.phase1(x, w_in_b, wq_b, wkv_b, qTa_d, kTa_d, va_d, qTm_d, kTm_d, vm_d,
                            ident_b, g_mix, g_qa, g_ka, g_cq, g_ckv, g_qn, g_qr, g_kn, g_kr, cos_t, sin_t)
            p.barrier()
            self.arena_off = persist_mark
            if self.stop_after >= 2:
                self.phase2(x, w_out, qTa_d, kTa_d, va_d, qTm_d, kTm_d, vm_d, mt_d, h1_d, xg_d, rt_d,
                            ident_f, ident_b, causal_b, ustrict_b, ones_b, onesf, eoff, g_ffn,
                            wr_sb, br_sb, slots_all, wts_all, acc_b)
            p.barrier()
            self.arena_off = persist_mark
            if self.stop_after >= 3:
                self.phase3(w_gate, w_up, w_down, xg_d, y_d, ident_b)
            p.barrier()
            self.arena_off = persist_mark
            if self.stop_after >= 4:
                self.phase4(pin, w_ple_proj, w_ple_gate, b_ple_gate, h1_d, y_d, out, ident_b, ones_b, g_ple,
                            slots_all, wts_all)
            p.emit(final_wait_ops=self.out_ops)
        return nc

    def headnorm(self, src_ps, nh, hd, stride, off, gain, dst, dst_nm, ps_nm, n_real, tag, sqt, ssum, tmpt):
        v = src_ps
        sq, sqk = sqt
        tmp, tmpk = tmpt
        self.ACT(sq[:, 0:nh * stride], v, AF.Square, [ps_nm], [sqk])
        sqv = sq[:, 0:nh * stride].rearrange("p (h d) -> p h d", h=nh)[:, :, off:off + hd]
        self.RED("dve", ssum, sqv, ALU.add, [sqk], [tag + "ss"])
        self.rstd(ssum, n_real, tag + "ss")
        vv = v.rearrange("p (h d) -> p h d", h=nh)[:, :, off:off + hd]
        self.TT("dve", tmp, vv, ssum.unsqueeze(2).to_broadcast([128, nh, hd]), ALU.mult,
                [ps_nm, tag + "ss"], [tmpk])
        self.TT("pool", dst, tmp, gain.unsqueeze(1).to_broadcast([128, nh, hd]), ALU.mult,
                [tmpk, "G"], [dst_nm])

    def phase1(self, x, w_in_b, wq_b, wkv_b, qTa_d, kTa_d, va_d, qTm_d, kTm_d, vm_d,
               ident_b, g_mix, g_qa, g_ka, g_cq, g_ckv, g_qn, g_qr, g_kn, g_kr, cos_t, sin_t):
        PS, PSb = self.PS, self.PSb
        sb = self.sb
        junk = sb([128, D], BF16)
        NS = 2
        B = []
        for si in range(NS):
            d = {}
            d["xt"] = [sb([128, D], F32) for _ in range(2)]
            d["xn"] = sb([128, D], BF16)
            d["xT"] = sb([128, 8, 128], BF16)
            d["sq"] = [sb([128, 1024], F32) for _ in range(2)]
            d["tmp"] = [sb([128, 8, 64], F32) for _ in range(2)]
            d["ss"] = [sb([128, 8], F32) for _ in range(10)]
            d["qan"] = sb([128, 8, 64], BF16)
            d["kan"] = sb([128, 8, 64], BF16)
            d["cqn"] = sb([128, 384], BF16)
            d["cqT"] = sb([128, 3, 128], BF16)
            d["ckvn"] = sb([128, 256], BF16)
            d["ckvT"] = sb([128, 2, 128], BF16)
            d["qm"] = sb([128, 8, 96], BF16)
            d["km"] = sb([128, 8, 96], BF16)
            d["rp"] = sb([128, 8, 32], F32)
            d["r1"] = sb([128, 8, 16], F32)
            d["r2"] = sb([128, 8, 16], F32)
            d["krn"] = sb([128, 32], F32)
            d["kro"] = sb([128, 32], BF16)
            d["k1"] = sb([128, 16], F32)
            d["k2"] = sb([128, 16], F32)
            d["bank"] = 0
            d["sqc"] = 0
            d["tmc"] = 0
            d["xc"] = 0
            B.append(d)
        qaT_s = [sb([128, 4, 256], BF16) for _ in range(2)]
        kaT_s = [sb([128, 4, 256], BF16) for _ in range(2)]
        qmT_s = [sb([128, 8, 256], BF16) for _ in range(2)]
        kmT_s = [sb([128, 8, 256], BF16) for _ in range(2)]
        va_s = [sb([128, 2, 512], BF16) for _ in range(2)]
        vm_s = [sb([128, 2, 8, 64], BF16) for _ in range(2)]
        chunks = [(0, 512), (512, 512), (1024, 512), (1536, 384), (1920, 288)]

        def tile_gen(t, si):
            d = B[si]
            K_ = f"s{si}"
            m, ti = divmod(t, 2)
            mi = m % 2
            pj = t % TPS

            def nb():
                b_ = 4 * si + d["bank"]
                d["bank"] = (d["bank"] + 1) % 4
                return b_

            def nsq():
                k = d["sqc"] % 2
                d["sqc"] += 1
                return d["sq"][k], f"{K_}sq{k}"

            def ntmp():
                k = d["tmc"] % 2
                d["tmc"] += 1
                return d["tmp"][k], f"{K_}tmp{k}"

            ssh = d["ss"]
            xi = d["xc"] % 2
            d["xc"] += 1
            X = d["xt"][xi]
            xk = f"{K_}xt{xi}"
            self.DMA("sp", X, x[t * 128:(t + 1) * 128, :], [], [xk])
            s1 = ssh[8][:, 0:1]
            s1k = f"{K_}ss1"
            self.MS("pool", s1, 0.0, [s1k])
            yield
            self.ACT(junk, X, AF.Square, [xk, s1k], [s1k], accum=s1)
            self.rstd(s1, D, s1k)
            yield
            XN, xnk = d["xn"], f"{K_}xn"
            self.STT("dve", XN, X, s1, g_mix, ALU.mult, ALU.mult, [xk, s1k, "G"], [xnk])
            yield
            b0 = nb()
            for c in range(8):
                self.TR(PSb[b0][:, c * 128:(c + 1) * 128], XN[:, c * 128:(c + 1) * 128], ident_b, [xnk, "Cb"], [f"ps{b0}"])
            yield
            XT, xtk = d["xT"], f"{K_}xT"
            self.CP("act", XT, PSb[b0][:, 0:1024].rearrange("p (c n) -> p c n", c=8), [f"ps{b0}"], [xtk])
            yield

            def proj(ci):
                c0, cw = chunks[ci]
                b_ = nb()
                for c in range(8):
                    self.MM(PS[b_][:, 0:cw], XT[:, c, :], w_in_b[:, c, c0:c0 + cw], c == 0, c == 7,
                            [xtk, "w_in_b"], [f"ps{b_}"])
                return b_

            def headnorm_g(src_ps, ps_nm, gain, dst, dst_nm, ssum, tag):
                sq, sqk = nsq()
                tmp, tmpk = ntmp()
                self.ACT(sq[:, 0:512], src_ps, AF.Square, [ps_nm], [sqk])
                yield
                self.RED("dve", ssum, sq[:, 0:512].rearrange("p (h d) -> p h d", h=8), ALU.add, [sqk], [tag])
                yield
                self.rstd(ssum, 64, tag)
                yield
                self.TT("dve", tmp, src_ps.rearrange("p (h d) -> p h d", h=8), ssum.unsqueeze(2).to_broadcast([128, 8, 64]),
                        ALU.mult, [ps_nm, tag], [tmpk])
                yield
                self.TT("pool", dst, tmp, gain.unsqueeze(1).to_broadcast([128, 8, 64]), ALU.mult, [tmpk, "G"], [dst_nm])
                yield

            def trans_pairs(src, src_nm, stage, snm):
                bt = nb()
                for c in range(4):
                    self.TR(PSb[bt][:, c * 128:(c + 1) * 128], src[:, 2 * c:2 * c + 2, :].rearrange("p a b -> p (a b)"),
                            ident_b, [src_nm, "Cb"], [f"ps{bt}"])
                yield
                self.CP("dve", stage[:, :, ti * 128:(ti + 1) * 128],
                        PSb[bt][:, 0:512].rearrange("p (c n) -> p c n", c=4), [f"ps{bt}"], [snm])
                yield

            bc0 = proj(0)
            yield
            bc1 = proj(1)
            yield
            bc2 = proj(2)
            yield
            self.CP("act", va_s[mi][:, ti, :], PS[bc2][:, 0:512], [f"ps{bc2}"], [f"vas{mi}"])
            yield
            bc3 = proj(3)
            yield
            yield from headnorm_g(PS[bc0][:, 0:512], f"ps{bc0}", g_qa, d["qan"], f"{K_}qan", ssh[0], f"{K_}ssqa")
            bc4 = proj(4)
            yield
            yield from headnorm_g(PS[bc1][:, 0:512], f"ps{bc1}", g_ka, d["kan"], f"{K_}kan", ssh[1], f"{K_}sska")
            yield from trans_pairs(d["qan"], f"{K_}qan", qaT_s[mi], f"qaTs{mi}")
            yield from trans_pairs(d["kan"], f"{K_}kan", kaT_s[mi], f"kaTs{mi}")
            s2 = ssh[9][:, 0:1]
            cqk = f"{K_}cqss"
            self.MS("pool", s2, 0.0, [cqk])
            yield
            self.ACT(junk[:, 0:384], PS[bc3][:, 0:384], AF.Square, [f"ps{bc3}", cqk], [cqk], accum=s2)
            self.rstd(s2, 384, cqk)
            yield
            cqn, cqnk = d["cqn"], f"{K_}cqn"
            self.STT("dve", cqn, PS[bc3][:, 0:384], s2, g_cq, ALU.mult, ALU.mult, [f"ps{bc3}", cqk, "G"], [cqnk])
            yield
            bt = nb()
            for c in range(3):
                self.TR(PSb[bt][:, c * 128:(c + 1) * 128], cqn[:, c * 128:(c + 1) * 128], ident_b, [cqnk, "Cb"], [f"ps{bt}"])
            yield
            cqT, cqTk = d["cqT"], f"{K_}cqT"
            self.CP("dve", cqT, PSb[bt][:, 0:384].rearrange("p (c n) -> p c n", c=3), [f"ps{bt}"], [cqTk])
            yield
            s5 = ssh[7]
            ckk = f"{K_}ckss"
            self.MS("pool", s5[:, 0:2], 0.0, [ckk])
            yield
            self.ACT(junk[:, 0:256], PS[bc4][:, 0:256], AF.Square, [f"ps{bc4}", ckk], [ckk], accum=s5[:, 0:1])
            self.ACT(junk[:, 256:288], PS[bc4][:, 256:288], AF.Square, [f"ps{bc4}", ckk], [ckk], accum=s5[:, 1:2])
            self.rstd(s5[:, 0:1], 256, ckk)
            self.rstd(s5[:, 1:2], 32, ckk)
            yield
            ckvn, ckvnk = d["ckvn"], f"{K_}ckvn"
            krn, kro, k1, k2 = d["krn"], d["kro"], d["k1"], d["k2"]
            self.STT("dve", ckvn, PS[bc4][:, 0:256], s5[:, 0:1], g_ckv, ALU.mult, ALU.mult, [f"ps{bc4}", ckk, "G"], [ckvnk])
            self.STT("dve", krn, PS[bc4][:, 256:288], s5[:, 1:2], g_kr, ALU.mult, ALU.mult, [f"ps{bc4}", ckk, "G"], [f"{K_}krn"])
            yield
            bq0, bq1 = nb(), nb()
            for c in range(3):
                self.MM(PS[bq0][:, 0:480], cqT[:, c, :], wq_b[:, c, 0:480], c == 0, c == 2, [cqTk, "wq_b"], [f"ps{bq0}"])
            for c in range(3):
                self.MM(PS[bq1][:, 0:288], cqT[:, c, :], wq_b[:, c, 480:768], c == 0, c == 2, [cqTk, "wq_b"], [f"ps{bq1}"])
            yield
            ck, sk = cos_t[:, pj, :], sin_t[:, pj, :]
            self.TT("dve", k1, krn[:, 0:16], ck, ALU.mult, [f"{K_}krn", "C"], [f"{K_}k1"])
            self.TT("dve", k2, krn[:, 16:32], sk, ALU.mult, [f"{K_}krn", "C"], [f"{K_}k2"])
            self.TT("dve", kro[:, 0:16], k1, k2, ALU.subtract, [f"{K_}k1", f"{K_}k2"], [f"{K_}kro"])
            self.TT("dve", k1, krn[:, 16:32], ck, ALU.mult, [f"{K_}krn", "C", f"{K_}kro"], [f"{K_}k1"])
            self.TT("dve", k2, krn[:, 0:16], sk, ALU.mult, [f"{K_}krn", "C", f"{K_}kro"], [f"{K_}k2"])
            self.TT("dve", kro[:, 16:32], k1, k2, ALU.add, [f"{K_}k1", f"{K_}k2"], [f"{K_}kro"])
            km, kmk = d["km"], f"{K_}km"
            self.CP("dve", km[:, :, 64:96], kro.unsqueeze(1).to_broadcast([128, 8, 32]), [f"{K_}kro"], [kmk])
            yield
            qm, qmk = d["qm"], f"{K_}qm"
            rp, rpk = d["rp"], f"{K_}rp"
            for (bq, h0, nh, ia, ib) in ((bq0, 0, 5, 3, 5), (bq1, 5, 3, 4, 6)):
                tag = f"{K_}qm{h0}"
                src = PS[bq][:, 0:nh * 96]
                sq, sqk = nsq()
                tmpa, tmpk = ntmp()
                self.ACT(sq[:, 0:nh * 96], src, AF.Square, [f"ps{bq}"], [sqk])
                yield
                sqv = sq[:, 0:nh * 96].rearrange("p (h d) -> p h d", h=nh)
                sn = ssh[ia][:, 0:nh]
                sr = ssh[ib][:, 0:nh]
                self.RED("dve", sn, sqv[:, :, 0:64], ALU.add, [sqk], [tag + "sn"])
                self.RED("dve", sr, sqv[:, :, 64:96], ALU.add, [sqk], [tag + "sr"])
                yield
                self.rstd(sn, 64, tag + "sn")
                self.rstd(sr, 32, tag + "sr")
                yield
                sv = src.rearrange("p (h d) -> p h d", h=nh)
                self.TT("dve", tmpa[:, 0:nh, :], sv[:, :, 0:64], sn.unsqueeze(2).to_broadcast([128, nh, 64]), ALU.mult,
                        [f"ps{bq}", tag + "sn"], [tmpk])
                self.TT("dve", rp[:, h0:h0 + nh, :], sv[:, :, 64:96], sr.unsqueeze(2).to_broadcast([128, nh, 32]), ALU.mult,
                        [f"ps{bq}", tag + "sr"], [rpk])
                yield
                self.TT("pool", qm[:, h0:h0 + nh, 0:64], tmpa[:, 0:nh, :], g_qn.unsqueeze(1).to_broadcast([128, nh, 64]),
                        ALU.mult, [tmpk, "G"], [qmk])
                yield
            r1, r2 = d["r1"], d["r2"]
            cosb = cos_t[:, pj, :].unsqueeze(1).to_broadcast([128, 8, 16])
            sinb = sin_t[:, pj, :].unsqueeze(1).to_broadcast([128, 8, 16])
            self.TT("pool", rp, rp, g_qr.unsqueeze(1).to_broadcast([128, 8, 32]), ALU.mult, [rpk, "G"], [rpk])
            self.TT("pool", r1, rp[:, :, 0:16], cosb, ALU.mult, [rpk, "C"], [f"{K_}r1"])
            self.TT("pool", r2, rp[:, :, 16:32], sinb, ALU.mult, [rpk, "C"], [f"{K_}r2"])
            self.TT("pool", qm[:, :, 64:80], r1, r2, ALU.subtract, [f"{K_}r1", f"{K_}r2"], [qmk])
            self.TT("pool", r1, rp[:, :, 16:32], cosb, ALU.mult, [rpk, "C", qmk], [f"{K_}r1"])
            self.TT("pool", r2, rp[:, :, 0:16], sinb, ALU.mult, [rpk, "C", qmk], [f"{K_}r2"])
            self.TT("pool", qm[:, :, 80:96], r1, r2, ALU.add, [f"{K_}r1", f"{K_}r2"], [qmk])
            yield
            bt = nb()
            for h in range(8):
                self.TR(PSb[bt][0:96, h * 128:(h + 1) * 128], qm[:, h, :], ident_b, [qmk, "Cb"], [f"ps{bt}"])
            yield
            self.CP("act", qmT_s[mi][0:96, :, ti * 128:(ti + 1) * 128],
                    PSb[bt][0:96, 0:1024].rearrange("p (c n) -> p c n", c=8), [f"ps{bt}"], [f"qmTs{mi}"])
            yield
            bt = nb()
            for c in range(2):
                self.TR(PSb[bt][:, c * 128:(c + 1) * 128], ckvn[:, c * 128:(c + 1) * 128], ident_b, [ckvnk, "Cb"], [f"ps{bt}"])
            yield
            ckvT, ckvTk = d["ckvT"], f"{K_}ckvT"
            self.CP("dve", ckvT, PSb[bt][:, 0:256].rearrange("p (c n) -> p c n", c=2), [f"ps{bt}"], [ckvTk])
            yield
            bk0, bk1 = nb(), nb()
            for half, bk in ((0, bk0), (1, bk1)):
                for c in range(2):
                    self.MM(PS[bk][:, 0:512], ckvT[:, c, :], wkv_b[:, c, half * 512:(half + 1) * 512], c == 0, c == 1,
                            [ckvTk, "wkv_b"], [f"ps{bk}"])
            yield
            for half, bk in ((0, bk0), (1, bk1)):
                tag = f"{K_}kv{half}"
                src = PS[bk][:, 0:512]
                h0 = half * 4
                sq, sqk = nsq()
                tmpa, tmpk = ntmp()
                self.ACT(sq[:, 0:512], src, AF.Square, [f"ps{bk}"], [sqk])
                yield
                sqv = sq[:, 0:512].rearrange("p (h d) -> p h d", h=4)
                sn = ssh[2][:, half * 4:half * 4 + 4]
                self.RED("dve", sn, sqv[:, :, 0:64], ALU.add, [sqk], [tag + "sn"])
                yield
                self.rstd(sn, 64, tag + "sn")
                sv = src.rearrange("p (h d) -> p h d", h=4)
                self.CP("act", vm_s[mi][:, ti, h0:h0 + 4, :], sv[:, :, 64:128], [f"ps{bk}", tag + "sn"], [f"vms{mi}"])
                yield
                self.TT("dve", tmpa[:, 0:4, :], sv[:, :, 0:64], sn.unsqueeze(2).to_broadcast([128, 4, 64]), ALU.mult,
                        [f"ps{bk}", tag + "sn", f"vms{mi}"], [tmpk])
                yield
                self.TT("pool", km[:, h0:h0 + 4, 0:64], tmpa[:, 0:4, :], g_kn.unsqueeze(1).to_broadcast([128, 4, 64]),
                        ALU.mult, [tmpk, "G"], [kmk])
                yield
            bt = nb()
            for h in range(8):
                self.TR(PSb[bt][0:96, h * 128:(h + 1) * 128], km[:, h, :], ident_b, [kmk, "Cb"], [f"ps{bt}"])
            yield
            self.CP("act", kmT_s[mi][0:96, :, ti * 128:(ti + 1) * 128],
                    PSb[bt][0:96, 0:1024].rearrange("p (c n) -> p c n", c=8), [f"ps{bt}"], [f"kmTs{mi}"])
            yield
            if ti == 1:
                cs = slice(m * 256, (m + 1) * 256)
                self.DMA("pool", qTa_d.rearrange("(c q) n -> q c n", q=128)[:, :, cs], qaT_s[mi], [f"qaTs{mi}"], ["qTa_d"])
                self.DMA("pool", kTa_d.rearrange("(c q) n -> q c n", q=128)[:, :, cs], kaT_s[mi], [f"kaTs{mi}"], ["kTa_d"])
                self.DMA("pool", qTm_d.rearrange("(h d) n -> d h n", d=96)[:, :, cs], qmT_s[mi][0:96], [f"qmTs{mi}"], ["qTm_d"])
                self.DMA("pool", kTm_d.rearrange("(h d) n -> d h n", d=96)[:, :, cs], kmT_s[mi][0:96], [f"kmTs{mi}"], ["kTm_d"])
                self.DMA("pool", va_d[cs, :].rearrange("(t p) n -> p t n", p=128), va_s[mi], [f"vas{mi}"], ["va_d"])
                self.DMA("pool", vm_d[cs, :].rearrange("(t p) n -> p t n", p=128),
                         vm_s[mi].rearrange("p t h d -> p t (h d)"), [f"vms{mi}"], ["vm_d"])

        for m in range(NT // 2):
            if m < len(self.zero_jobs):
                zo, zi, zr, zw = self.zero_jobs[m]
                self.DMA("sp", zo, zi, [zr], [zw])
            gens = [tile_gen(2 * m, 0), tile_gen(2 * m + 1, 1)]
            alive = [True, True]
            while any(alive):
                for gi_, g_ in enumerate(gens):
                    if alive[gi_]:
                        try:
                            next(g_)
                        except StopIteration:
                            alive[gi_] = False

    def phase2(self, x, w_out, qTa_d, kTa_d, va_d, qTm_d, kTm_d, vm_d, mt_d, h1_d, xg_d, rt_d,
               ident_f, ident_b, causal_b, ustrict_b, ones_b, onesf, eoff, g_ffn,
               wr_sb, br_sb, slots_all, wts_all, acc_b):
        PS, PSb = self.PS, self.PSb
        sb = self.sb
        w_out_b = sb([128, 8, D], BF16)
        br_d = self.br_d
        stg = [sb([128, D], F32) for _ in range(2)]
        for c in range(8):
            self.DMA("sp", stg[c % 2], w_out[c * 128:(c + 1) * 128, :], [], [f"wstg{c%2}"])
            self.CP(("pool", "dve")[c % 2], w_out_b[:, c, :], stg[c % 2], [f"wstg{c%2}"], ["w_out_b"])
        qT = [sb([128, S], BF16) for _ in range(2)]
        kT = [sb([128, S], BF16) for _ in range(2)]
        vaug = [sb([128, TPS, 128], BF16) for _ in range(2)]
        mt = [sb([128, MTW], BF16) for _ in range(2)]
        mixTs = [sb([128, 8, S], BF16) for _ in range(2)]
        Pt = [sb([128, 512], BF16) for _ in range(4)]
        rden = [sb([128, 512], F32) for _ in range(2)]
        xr = [sb([128, D], F32) for _ in range(2)]
        h1 = [sb([128, D], F32) for _ in range(3)]
        NXB = 7
        xn2b = [sb([128, D], BF16) for _ in range(NXB)]
        xn2T = [sb([128, 8, 128], BF16) for _ in range(2)]
        junk = sb([128, D], BF16)
        junk4 = sb([128, 4], F32)
        ss2 = [sb([128, 1], F32) for _ in range(2)]
        Ls = [sb([128, 36], F32) for _ in range(3)]
        NRT = 6
        rt = [sb([128, 8], F32) for _ in range(NRT)]
        gm = sb([128, 4], F32)
        pen = sb([128, 4], F32)
        Em = sb([128, 4, 8], F32)
        top8 = sb([128, 8], F32)
        OH = [sb([128, 2, 32], F32) for _ in range(2)]
        OHb = [sb([128, 32], BF16) for _ in range(2)]
        prod = sb([128, 2, 32], F32)
        prod2 = sb([128, 2, 32], F32)
        rk = sb([128, 2], F32)
        ek = sb([128, 2], F32)
        okk = [sb([128, 2], F32) for _ in range(2)]
        slf = sb([128, 2], F32)
        wr_b = sb([128, 8, 36], BF16)
        br_bc = sb([128, 36], F32)
        self.CP("dve", wr_b, wr_sb, ["wr"], ["wr_b"])
        self.DMA("sp", br_bc, br_d[0:1, :].partition_broadcast(128)[:, 0, :], [], ["br_bc"])
        for i in range(2):
            self.MS("pool", vaug[i], 1.0, [f"vaug{i}"])

        LA = 2
        mcount = [0]

        def head_loads(b, h, i):
            tok0 = b * S
            if h < 8:
                self.DMA("sp", qT[i][0:64, :], qTa_d[h * 64:(h + 1) * 64, tok0:tok0 + S], [], [f"qT{i}"])
                self.DMA("sp", kT[i][0:64, :], kTa_d[h * 64:(h + 1) * 64, tok0:tok0 + S], [], [f"kT{i}"])
                self.DMA("sp", vaug[i][:, :, 0:64],
                         va_d[tok0:tok0 + S, h * 64:(h + 1) * 64].rearrange("(t p) d -> p t d", p=128), [], [f"vaug{i}"])
                self.DMA("sp", mt[i], mt_d[h], [], [f"mt{i}"])
            else:
                hh = h - 8
                self.DMA("sp", qT[i][0:96, :], qTm_d[hh * 96:(hh + 1) * 96, tok0:tok0 + S], [], [f"qT{i}"])
                self.DMA("sp", kT[i][0:96, :], kTm_d[hh * 96:(hh + 1) * 96, tok0:tok0 + S], [], [f"kT{i}"])
                self.DMA("sp", vaug[i][:, :, 0:64],
                         vm_d[tok0:tok0 + S, hh * 64:(hh + 1) * 64].rearrange("(t p) d -> p t d", p=128), [], [f"vaug{i}"])

        loaded = set()

        def ensure_loaded(gidx):
            if gidx in loaded or gidx >= NSEQ * 16:
                return
            loaded.add(gidx)
            head_loads(gidx // 16, gidx % 16, gidx % 2)

        def emit_S(st):
            b, h, i, qb, kt, nk, sidx = st
            if qb == 0 and kt == 0:
                ensure_loaded(b * 16 + h)
            dq = 64 if h < 8 else 96
            q0, k0 = qb * 512, kt * 128
            qlo = max(q0, k0)
            N = q0 + 512 - qlo
            sbk = 2 + sidx % 3
            self.MM(PS[sbk][:, 0:N], kT[i][0:dq, k0:k0 + 128], qT[i][0:dq, qlo:qlo + N], True, True,
                    [f"kT{i}", f"qT{i}"], [f"ps{sbk}"])

        def emit_rest(st, mixb):
            b, h, i, qb, kt, nk, sidx = st
            isA = h < 8
            scale = (64 ** -0.5) if isA else (96 ** -0.5)
            q0, k0 = qb * 512, kt * 128
            qlo = max(q0, k0)
            N = q0 + 512 - qlo
            sbk = 2 + sidx % 3
            pk = sidx % 4
            ob = qb % 2
            self.ACT(Pt[pk][:, 0:N], PS[sbk][:, 0:N], AF.Exp, [f"ps{sbk}"], [f"Pt{pk}"], scale=scale)
            if isA:
                off = qlo - k0
                eng = "dve"
                mcount[0] += 1
                self.TT(eng, Pt[pk][:, 0:N], Pt[pk][:, 0:N], mt[i][:, off:off + N], ALU.mult,
                        [f"Pt{pk}", f"mt{i}"], [f"Pt{pk}"])
            elif qlo == k0:
                self.TT("dve", Pt[pk][:, 0:128], Pt[pk][:, 0:128], causal_b, ALU.mult,
                        [f"Pt{pk}", "Cb"], [f"Pt{pk}"])
            self.MM(PS[ob][:, qlo - q0:512], vaug[i][:, kt, :], Pt[pk][:, 0:N], kt == 0, kt == nk - 1,
                    [f"vaug{i}", f"Pt{pk}"], [f"ps{ob}"])
            if kt == nk - 1:
                rd = rden[ob]
                if qb % 2 == 0:
                    self.RCP(rd[0:64, :], PS[ob][64:128, :], [f"ps{ob}"], [f"rden{ob}"])
                else:
                    self.ACT(rd[0:64, :], PS[ob][64:128, :], AF.Ln, [f"ps{ob}"], [f"rden{ob}"])
                    self.ACT(rd[0:64, :], rd[0:64, :], AF.Exp, [f"rden{ob}"], [f"rden{ob}"], scale=-1.0)
                hp = (h % 2) * 64
                self.TT("dve", mixb[0][hp:hp + 64, h // 2, q0:q0 + 512], PS[ob][0:64, :], rd[0:64, :], ALU.mult,
                        [f"ps{ob}", f"rden{ob}"], [mixb[1]])

        def outproj_stages(b, mixb):
            mixT_, mixk = mixb

            def s0(t, j):
                i2 = t % 2
                self.DMA("sp", xr[i2], x[t * 128:(t + 1) * 128, :], [], [f"xr{i2}"])
                for n in range(2):
                    bo = 5 + n
                    for c in range(8):
                        self.MM(PS[bo][:, 0:512], mixT_[:, c, j * 128:(j + 1) * 128], w_out_b[:, c, n * 512:(n + 1) * 512],
                                c == 0, c == 7, [mixk, "w_out_b"], [f"ps{bo}"])

            def s1(t, j):
                i2, i3 = t % 2, t % 3
                for n in range(2):
                    bo = 5 + n
                    self.TT("dve", h1[i3][:, n * 512:(n + 1) * 512], PS[bo][:, 0:512], xr[i2][:, n * 512:(n + 1) * 512], ALU.add,
                            [f"ps{bo}", f"xr{i2}"], [f"h1_{i3}"])

            def s2(t, j):
                i2, i3 = t % 2, t % 3
                self.DMA("pool", h1_d[t * 128:(t + 1) * 128, :], h1[i3], [f"h1_{i3}"], ["h1_d"])
                self.MS("pool", ss2[i2], 0.0, [f"ss2_{i2}"])
                self.ACT(junk, h1[i3], AF.Square, [f"h1_{i3}", f"ss2_{i2}"], [f"ss2_{i2}"], accum=ss2[i2])
                self.ACT(ss2[i2], ss2[i2], AF.Ln, [f"ss2_{i2}", "epsc"], [f"ss2_{i2}"], bias=self.epsc[:, 0:1], scale=1.0 / D)
                self.ACT(ss2[i2], ss2[i2], AF.Exp, [f"ss2_{i2}"], [f"ss2_{i2}"], scale=-0.5)

            def s3(t, j):
                i2, i3, ix = t % 2, t % 3, t % NXB
                self.STT("dve", xn2b[ix], h1[i3], ss2[i2][:, 0:1], g_ffn, ALU.mult, ALU.mult,
                         [f"h1_{i3}", f"ss2_{i2}", "G"], [f"xn2b{ix}"])

            def s4(t, j):
                i2, ix = t % 2, t % NXB
                for c in range(8):
                    self.TR(PSb[7][:, c * 128:(c + 1) * 128], xn2b[ix][:, c * 128:(c + 1) * 128], ident_b,
                            [f"xn2b{ix}", "Cb"], ["ps7"])
                self.CP("act", xn2T[i2], PSb[7][:, 0:1024].rearrange("p (c n) -> p c n", c=8), ["ps7"], [f"xn2T{i2}"])

            def s5(t, j):
                i2, il, ir = t % 2, t % 3, t % NRT
                for c in range(8):
                    self.MM(PS[7][:, 0:36], xn2T[i2][:, c, :], wr_b[:, c, :], c == 0, c == 7, [f"xn2T{i2}", "wr_b"], ["ps7"])
                self.TT("dve", Ls[il], PS[7][:, 0:36], br_bc, ALU.add, ["ps7", "br_bc"], [f"Ls{il}"])
                self.RED("dve", rt[ir][:, 0:1], Ls[il][:, 0:4], ALU.max, [f"Ls{il}"], [f"rt{ir}"])
                self.TS("dve", rt[ir][:, 1:2], rt[ir][:, 0:1], -1.0, None, ALU.mult, None, [f"rt{ir}"], [f"rt{ir}"])
                self.MS("pool", rt[ir][:, 2:3], 0.0, [f"rt{ir}g"])

            def s6(t, j):
                il, ir = t % 3, t % NRT
                self.ACT(junk4, Ls[il][:, 0:4], AF.Exp, [f"Ls{il}", f"rt{ir}", f"rt{ir}g"], [f"rt{ir}g"],
                         bias=rt[ir][:, 1:2], scale=1.0, accum=rt[ir][:, 2:3])

            def s7(t, j):
                i2, il, ir = t % 2, t % 3, t % NRT
                R = rt[ir]
                self.RCP(R[:, 3:4], R[:, 2:3], [f"rt{ir}g"], [f"rt{ir}w"])
                self.TS("dve", gm, Ls[il][:, 0:4], R[:, 0:1], None, ALU.is_ge, None, [f"Ls{il}", f"rt{ir}"], ["gm"])
                self.TS("dve", pen, gm, -1.0, 1e30, ALU.add, ALU.mult, ["gm"], ["pen"])
                self.TT("dve", Em, Ls[il][:, 4:36].rearrange("p (g e) -> p g e", g=4), pen.unsqueeze(2).to_broadcast([128, 4, 8]),
                        ALU.add, [f"Ls{il}", "pen"], ["Em"])
                Emf = Em.rearrange("p g e -> p (g e)")
                self.p.op("dve", lambda e, o_=top8, i_=Emf: e.max(out=o_, in_=i_), reads=["Em"], writes=["top8"])
                O_ = OH[i2]
                self.TS("dve", O_[:, 0, :], Emf, top8[:, 0:1], None, ALU.is_ge, None, ["Em", "top8"], [f"OH{i2}"])
                self.TS("dve", O_[:, 1, :], Emf, top8[:, 1:2], None, ALU.is_ge, None, ["Em", "top8", f"OH{i2}"], [f"OH{i2}"])
                self.CP("dve", OHb[i2], O_[:, 1, :], [f"OH{i2}"], [f"OHb{i2}"])
                self.TT("dve", O_[:, 1, :], O_[:, 1, :], O_[:, 0, :], ALU.subtract, [f"OH{i2}", f"OHb{i2}"], [f"OH{i2}"])
                self.TT("dve", R[:, 4:5], top8[:, 0:1], top8[:, 1:2], ALU.subtract, ["top8"], [f"rt{ir}d"])

            def s8(t, j):
                i2, ir = t % 2, t % NRT
                R = rt[ir]
                O_ = OH[i2]
                self.ACT(R[:, 5:6], R[:, 4:5], AF.Exp, [f"rt{ir}d"], [f"rt{ir}s"], scale=-1.0)
                self.MM(PS[7][:, 64:96], ustrict_b, OHb[i2], True, False, ["Cb", f"OHb{i2}"], ["ps7"])
                self.MM(PS[7][:, 64:96], ones_b, acc_b, False, True, ["Cb", "acc"], ["ps7"])
                self.TT("dve", prod, O_, PS[7][:, 64:96].unsqueeze(1).to_broadcast([128, 2, 32]), ALU.mult,
                        [f"OH{i2}", "ps7"], ["prod"])
                self.RED("dve", rk, prod, ALU.add, ["prod"], ["rk"])
                self.TT("dve", prod2, O_, eoff.unsqueeze(1).to_broadcast([128, 2, 32]), ALU.mult, [f"OH{i2}", "C"], ["prod2"])
                self.RED("dve", ek, prod2, ALU.add, ["prod2"], ["ek"])
                self.TT("pool", acc_b, acc_b, OHb[i2], ALU.add, ["acc", f"OHb{i2}"], ["acc"])
                ok_ = okk[i2]
                self.TS("dve", ok_, rk, float(CAP), None, ALU.is_lt, None, ["rk"], [f"okk{i2}"])
                self.TT("dve", slf, rk, ek, ALU.add, ["rk", "ek"], ["slf"])
                self.TS("dve", slf, slf, float(-TRASH), None, ALU.add, None, ["slf"], ["slf"])
                self.TT("dve", slf, slf, ok_, ALU.mult, ["slf", f"okk{i2}"], ["slf"])
                self.TS("dve", slf, slf, float(TRASH), None, ALU.add, None, ["slf"], ["slf"])
                self.CP("dve", slots_all[:, t, :], slf, ["slf"], ["slots"])

            def s9(t, j):
                i2, ir, ix = t % 2, t % NRT, t % NXB
                R = rt[ir]
                w1 = wts_all[:, t, 0:1]
                w2 = wts_all[:, t, 1:2]
                self.TS("dve", R[:, 6:7], R[:, 5:6], 1.0, None, ALU.add, None, [f"rt{ir}s"], [f"rt{ir}s2"])
                self.RCP(R[:, 6:7], R[:, 6:7], [f"rt{ir}s2"], [f"rt{ir}s2"])
                self.TT("dve", w1, R[:, 3:4], R[:, 6:7], ALU.mult, [f"rt{ir}w", f"rt{ir}s2"], ["wts"])
                self.TT("dve", w2, R[:, 3:4], w1, ALU.subtract, [f"rt{ir}w", "wts"], ["wts"])
                self.TT("dve", wts_all[:, t, :], wts_all[:, t, :], okk[i2], ALU.mult, ["wts", f"okk{i2}"], ["wts"])
                for k in range(2):
                    idx = slots_all[:, t, k:k + 1]
                    self.p.dma("pool", lambda e, idx_=idx, src_=xn2b[ix]: e.indirect_dma_start(
                        out=xg_d, out_offset=bass.IndirectOffsetOnAxis(ap=idx_, axis=0), in_=src_, in_offset=None),
                        reads=[f"xn2b{ix}", "slots"], writes=["xg_d"])

            sts = [s0, s1, s2, s3, s4, s5, s6, s7, s8, s9]
            K_ = len(sts)
            out_ = []
            for tau in range(TPS + K_ - 1):
                for jj in reversed(range(K_)):
                    j = tau - jj
                    if 0 <= j < TPS:
                        out_.append((lambda f=sts[jj], t=b * TPS + j, j=j: f(t, j)))
            return out_

        hc = 0
        pending = []
        for b in range(NSEQ):
            mixb = (mixTs[b % 2], f"mixT{b%2}")
            steps = []
            sidx = 0
            for h in range(16):
                i = hc % 2
                hc += 1
                for qb in range(4):
                    nk = 4 * qb + 4
                    for kt in range(nk):
                        steps.append((b, h, i, qb, kt, nk, sidx))
                        sidx += 1
            n = len(steps)
            every = max(1, n // (len(pending) + 1)) if pending else 0
            for k in range(min(LA, n)):
                emit_S(steps[k])
            for k in range(n):
                if k + LA < n:
                    emit_S(steps[k + LA])
                emit_rest(steps[k], mixb)
                if steps[k][3] == 0 and steps[k][4] == 0:
                    ensure_loaded(steps[k][0] * 16 + steps[k][1] + 1)
                if pending and (k % every == every - 1):
                    pending.pop(0)()
            while pending:
                pending.pop(0)()
            pending = outproj_stages(b, mixb)
        while pending:
            pending.pop(0)()
        if self.debug:
            dbgt = sb([128, NT * 4], F32)
            self.CP("dve", dbgt[:, 0:NT * 2], slots_all.rearrange("p t k -> p (t k)"), ["slots"], ["dbgt"])
            self.CP("dve", dbgt[:, NT * 2:NT * 4], wts_all.rearrange("p t k -> p (t k)"), ["wts", "dbgt"], ["dbgt"])
            self.DMA("sp", rt_d, dbgt, ["dbgt"], ["rt_d"])

    def phase3(self, w_gate, w_up, w_down, xg_d, y_d, ident_b):
        PS, PSb = self.PS, self.PSb
        sb = self.sb
        stg = [sb([128, 4096], F32) for _ in range(3)]
        wg = [sb([128, 8, FF], BF16) for _ in range(2)]
        wu = [sb([128, 8, FF], BF16) for _ in range(2)]
        wd = [sb([128, 4, D], BF16) for _ in range(2)]
        xrow = [sb([128, D], BF16) for _ in range(3)]
        xTe = [sb([128, 8, CAP], BF16) for _ in range(2)]
        hT = [sb([128, 4, CAP], BF16) for _ in range(2)]
        sg = [sb([128, 512], F32) for _ in range(2)]
        ysb = [sb([128, D], F32) for _ in range(6)]
        nst = CAP // 128
        cnt = {"xc": 0, "yc": 0, "gub": 0}

        def w_load(e, which):
            i = e % 2
            src, dst, dn, view = ((w_gate[e], wg[i], f"wg{i}", "(c p) f -> p c f"),
                                  (w_up[e], wu[i], f"wu{i}", "(c p) f -> p c f"),
                                  (w_down[e], wd[i], f"wd{i}", "(c p) n -> p c n"))[which]
            nch = dst.shape[1]
            s3 = stg[which].rearrange("p (c f) -> p c f", c=nch)
            self.DMA("sp", s3, src.rearrange(view, p=128), [], [f"stg{which}"])

        def w_cast(e, which):
            i = e % 2
            dst, dn = ((wg[i], f"wg{i}"), (wu[i], f"wu{i}"), (wd[i], f"wd{i}"))[which]
            nch = dst.shape[1]
            s3 = stg[which].rearrange("p (c f) -> p c f", c=nch)
            if which == 0:
                self.CP("act", dst, s3, [f"stg{which}"], [dn])
            elif which == 1:
                self.CP("dve", dst, s3, [f"stg{which}"], [dn])
            else:
                self.CP("pool", dst[:, 0:1, :], s3[:, 0:1, :], [f"stg{which}"], [dn + "c0"])
                self.CP("act", dst[:, 1:2, :], s3[:, 1:2, :], [f"stg{which}"], [dn + "c1"])
                self.CP("dve", dst[:, 2:3, :], s3[:, 2:3, :], [f"stg{which}"], [dn + "c2"])
                self.CP("act", dst[:, 3:4, :], s3[:, 3:4, :], [f"stg{which}"], [dn + "c3"])

        def x_trans(e):
            i = e % 2
            for s_ in range(nst):
                k = cnt["xc"] % 3
                cnt["xc"] += 1
                r0 = e * CAP + s_ * 128
                self.DMA("sp", xrow[k], xg_d[r0:r0 + 128, :], [], [f"xrow{k}"])
                bt = cnt["xc"] % 2
                for c in range(8):
                    self.TR(PSb[bt][:, c * 128:(c + 1) * 128], xrow[k][:, c * 128:(c + 1) * 128], ident_b,
                            [f"xrow{k}", "Cb"], [f"ps{bt}"])
                self.CP(("act", "dve")[s_ % 2], xTe[i][:, :, s_ * 128:(s_ + 1) * 128],
                        PSb[bt][:, 0:1024].rearrange("p (c n) -> p c n", c=8), [f"ps{bt}"], [f"xTe{i}"])

        def gate_up(e, ffc):
            i = e % 2
            for (n0, N) in ((0, 512), (512, CAP - 512)):
                bg = 2 + (cnt["gub"] % 2) * 2
                bu = bg + 1
                cnt["gub"] += 1
                for c in range(8):
                    self.MM(PS[bg][:, 0:N], wg[i][:, c, ffc * 128:(ffc + 1) * 128], xTe[i][:, c, n0:n0 + N],
                            c == 0, c == 7, [f"wg{i}", f"xTe{i}"], [f"ps{bg}"])
                for c in range(8):
                    self.MM(PS[bu][:, 0:N], wu[i][:, c, ffc * 128:(ffc + 1) * 128], xTe[i][:, c, n0:n0 + N],
                            c == 0, c == 7, [f"wu{i}", f"xTe{i}"], [f"ps{bu}"])
                sgi = cnt["gub"] % 2
                self.ACT(sg[sgi][:, 0:N], PS[bg][:, 0:N], AF.Silu, [f"ps{bg}"], [f"sg{sgi}"])
                self.TT("dve", hT[i][:, ffc, n0:n0 + N], sg[sgi][:, 0:N], PS[bu][:, 0:N], ALU.mult,
                        [f"sg{sgi}", f"ps{bu}"], [f"hT{i}"])

        def down(e):
            i = e % 2
            for s_ in range(nst):
                k = cnt["yc"] % 6
                cnt["yc"] += 1
                for n in range(2):
                    by = 6 + n
                    for c in range(4):
                        self.MM(PS[by][:, 0:512], hT[i][:, c, s_ * 128:(s_ + 1) * 128], wd[i][:, c, n * 512:(n + 1) * 512],
                                c == 0, c == 3, [f"hT{i}", f"wd{i}c{c}"], [f"ps{by}"])
                    self.CP(("act", "dve")[n], ysb[k][:, n * 512:(n + 1) * 512], PS[by][:, 0:512], [f"ps{by}"], [f"ysb{k}"])
                r0 = e * CAP + s_ * 128
                self.DMA("pool", y_d[r0:r0 + 128, :], ysb[k], [f"ysb{k}"], ["y_d"])

        for w in range(3):
            w_load(0, w)
            w_cast(0, w)
        x_trans(0)
        for e in range(NE):
            nxt = e + 1 < NE
            if nxt:
                for w in range(3):
                    w_load(e + 1, w)
            for ffc in range(4):
                gate_up(e, ffc)
                if nxt and ffc < 3:
                    w_cast(e + 1, ffc)
            if nxt:
                x_trans(e + 1)
            down(e)

    def phase4(self, pin, w_ple_proj, w_ple_gate, b_ple_gate, h1_d, y_d, out, ident_b, ones_b, g_ple,
               slots_all, wts_all):
        PS, PSb = self.PS, self.PSb
        sb = self.sb
        wpg = sb([128, 8, D], BF16)
        wpp = sb([128, 2, D], BF16)
        stg = [sb([128, D], F32) for _ in range(2)]
        for c in range(8):
            self.DMA("sp", stg[c % 2], w_ple_gate[c * 128:(c + 1) * 128, :], [], [f"wstg{c%2}"])
            self.CP(("pool", "dve")[c % 2], wpg[:, c, :], stg[c % 2], [f"wstg{c%2}"], ["wpg"])
        for c in range(2):
            self.DMA("sp", stg[c % 2], w_ple_proj[c * 128:(c + 1) * 128, :], [], [f"wstg{c%2}"])
            self.CP(("pool", "dve")[c % 2], wpp[:, c, :], stg[c % 2], [f"wstg{c%2}"], ["wpp"])
        bf = sb([1, D], F32)
        bhi = sb([1, D], BF16)
        blo = sb([1, D], BF16)
        bt_ = sb([1, D], F32)
        self.DMA("sp", bf[0:1, :], b_ple_gate, [], ["bf"])
        self.CP("dve", bhi[0:1, :], bf[0:1, :], ["bf"], ["bhi"])
        self.TT("dve", bt_[0:1, :], bf[0:1, :], bhi[0:1, :], ALU.subtract, ["bf", "bhi"], ["bt_"])
        self.CP("dve", blo[0:1, :], bt_[0:1, :], ["bt_"], ["blo"])
        NH = 9
        H = [sb([128, D], F32) for _ in range(NH)]
        y1 = [sb([128, D], F32) for _ in range(2)]
        y2 = [sb([128, D], F32) for _ in range(2)]
        pt = [sb([128, PLE], F32) for _ in range(2)]
        pb = [sb([128, PLE], BF16) for _ in range(2)]
        pT = [sb([128, 2, 128], BF16) for _ in range(2)]
        h2b = [sb([128, D], BF16) for _ in range(2)]
        h2T = [sb([128, 8, 128], BF16) for _ in range(2)]
        ev = [sb([128, D], F32) for _ in range(2)]
        NG = 5
        gt = [sb([128, D], F32) for _ in range(NG)]
        junk = sb([128, D], BF16)
        ssall = sb([128, NT, 2], F32)
        rsall = sb([128, NT], F32)

        def sw_pipe(ntiles, sts):
            K_ = len(sts)
            for tau in range(ntiles + K_ - 1):
                for jj in reversed(range(K_)):
                    t = tau - jj
                    if 0 <= t < ntiles:
                        sts[jj](t)

        self.MS("pool", ssall, 0.0, ["ssall"])

        def a0(t):
            i = t % 2
            self.DMA("sp", pt[i], pin[t * 128:(t + 1) * 128, :], [], [f"pt{i}"])

        def a1(t):
            i = t % 2
            self.CP("pool", pb[i], pt[i], [f"pt{i}"], [f"pb{i}"])

        def a2(t):
            i = t % 2
            for c in range(2):
                self.TR(PSb[0][:, c * 128:(c + 1) * 128], pb[i][:, c * 128:(c + 1) * 128], ident_b, [f"pb{i}", "Cb"], ["ps0"])
            self.CP("act", pT[i], PSb[0][:, 0:256].rearrange("p (c n) -> p c n", c=2), ["ps0"], [f"pT{i}"])

        def a3(t):
            i = t % 2
            for n in range(2):
                be = 2 + n
                for c in range(2):
                    self.MM(PS[be][:, 0:512], pT[i][:, c, :], wpp[:, c, n * 512:(n + 1) * 512], c == 0, c == 1,
                            [f"pT{i}", "wpp"], [f"ps{be}"])

        def a4(t):
            for n in range(2):
                be = 2 + n
                self.ACT(junk[:, 0:512], PS[be][:, 0:512], AF.Square, [f"ps{be}", "ssall"], ["ssall"],
                         accum=ssall[:, t, n:n + 1])

        sw_pipe(NT, [a0, a1, a2, a3, a4])
        self.TT("dve", rsall, ssall[:, :, 0], ssall[:, :, 1], ALU.add, ["ssall"], ["rsall"])
        self.ACT(rsall, rsall, AF.Sqrt, ["rsall", "epsc"], ["rsall"], bias=self.epsc[:, 0:1], scale=1.0 / D)
        self.RCP(rsall, rsall, ["rsall"], ["rsall"])

        def s0(t):
            i, ih = t % 2, t % NH
            self.DMA("sp", H[ih], h1_d[t * 128:(t + 1) * 128, :], [], [f"h{ih}"])
            self.DMA("sp", pt[i], pin[t * 128:(t + 1) * 128, :], [], [f"pt{i}"])
            for k, Y in ((0, y1[i]), (1, y2[i])):
                idx = slots_all[:, t, k:k + 1]
                self.p.dma("pool", lambda e, idx_=idx, dst_=Y: e.indirect_dma_start(
                    out=dst_, out_offset=None, in_=y_d, in_offset=bass.IndirectOffsetOnAxis(ap=idx_, axis=0)),
                    reads=["slots", "y_d"], writes=[f"y{k}_{i}"])

        def s1(t):
            i, ih = t % 2, t % NH
            self.CP("pool", pb[i], pt[i], [f"pt{i}"], [f"pb{i}"])
            self.STT("dve", H[ih], y1[i], wts_all[:, t, 0:1], H[ih], ALU.mult, ALU.add, [f"y0_{i}", f"h{ih}", "wts"], [f"h{ih}"])
            self.STT("dve", H[ih], y2[i], wts_all[:, t, 1:2], H[ih], ALU.mult, ALU.add, [f"y1_{i}", f"h{ih}", "wts"], [f"h{ih}"])

        def s2(t):
            i, ih = t % 2, t % NH
            for c in range(2):
                self.TR(PSb[0][:, c * 128:(c + 1) * 128], pb[i][:, c * 128:(c + 1) * 128], ident_b, [f"pb{i}", "Cb"], ["ps0"])
            self.CP("act", pT[i], PSb[0][:, 0:256].rearrange("p (c n) -> p c n", c=2), ["ps0"], [f"pT{i}"])
            self.CP("act", h2b[i], H[ih], [f"h{ih}"], [f"h2b{i}"])

        def s3(t):
            i = t % 2
            for c in range(8):
                self.TR(PSb[1][:, c * 128:(c + 1) * 128], h2b[i][:, c * 128:(c + 1) * 128], ident_b, [f"h2b{i}", "Cb"], ["ps1"])
            self.CP("act", h2T[i], PSb[1][:, 0:1024].rearrange("p (c n) -> p c n", c=8), ["ps1"], [f"h2T{i}"])
            for n in range(2):
                be = 2 + n
                for c in range(2):
                    self.MM(PS[be][:, 0:512], pT[i][:, c, :], wpp[:, c, n * 512:(n + 1) * 512], c == 0, c == 1,
                            [f"pT{i}", "wpp"], [f"ps{be}"])

        def s4(t):
            i = t % 2
            for n in range(2):
                be = 2 + n
                self.STT("dve", ev[i][:, n * 512:(n + 1) * 512], PS[be][:, 0:512], rsall[:, t:t + 1], g_ple[:, n * 512:(n + 1) * 512],
                         ALU.mult, ALU.mult, [f"ps{be}", "rsall", "G"], [f"ev{i}"])
            for n in range(2):
                bg = 4 + n
                for c in range(8):
                    self.MM(PS[bg][:, 0:512], h2T[i][:, c, :], wpg[:, c, n * 512:(n + 1) * 512], c == 0, False,
                            [f"h2T{i}", "wpg"], [f"ps{bg}"])
                self.MM(PS[bg][:, 0:512], ones_b[0:1, :], bhi[0:1, n * 512:(n + 1) * 512], False, False, ["Cb", "bhi"], [f"ps{bg}"])
                self.MM(PS[bg][:, 0:512], ones_b[0:1, :], blo[0:1, n * 512:(n + 1) * 512], False, True, ["Cb", "blo"], [f"ps{bg}"])

        def s5(t):
            ig = t % NG
            for n in range(2):
                bg = 4 + n
                self.ACT(gt[ig][:, n * 512:(n + 1) * 512], PS[bg][:, 0:512], AF.Sigmoid, [f"ps{bg}"], [f"gt{ig}"])

        def s6(t):
            i, ig = t % 2, t % NG
            Gt, E = gt[ig], ev[i]
            self.TT("pool", Gt[:, 0:384], Gt[:, 0:384], E[:, 0:384], ALU.mult, [f"gt{ig}", f"ev{i}"], [f"gt{ig}a"])
            self.TT("dve", Gt[:, 384:1024], Gt[:, 384:1024], E[:, 384:1024], ALU.mult, [f"gt{ig}", f"ev{i}"], [f"gt{ig}b"])

        def s7(t):
            ig, ih = t % NG, t % NH
            self.TT("dve", gt[ig], gt[ig], H[ih], ALU.add, [f"gt{ig}", f"gt{ig}a", f"gt{ig}b", f"h{ih}"], [f"gt{ig}"])

        def s8(t):
            ig = t % NG
            self.DMA("sp", out[t * 128:(t + 1) * 128, :], gt[ig], [f"gt{ig}"], ["out"])

        sw_pipe(NT, [s0, s1, s2, s3, s4, s5, s6, s7, s8])


def _t5_bucket(d):
    max_exact = 16
    n = np.maximum(d, 0)
    nf = np.maximum(n, 1).astype(np.float32)
    large = max_exact + (np.log(nf / np.float32(max_exact)) / np.float32(math.log(2048 / max_exact))
                         * np.float32(32 - max_exact)).astype(np.int32)
    large = np.minimum(large, 31)
    return np.where(n < max_exact, n, large)


def _constants():
    c = np.zeros((128, 1024), np.float32)
    c[:, 0:128] = np.eye(128, dtype=np.float32)
    jj = np.arange(128)[:, None]
    cc = np.arange(128)[None, :]
    c[:, 128:256] = (cc >= jj).astype(np.float32)
    c[:, 256:384] = (jj < cc).astype(np.float32)
    c[:, 384:416] = (np.arange(32) * CAP).astype(np.float32)[None, :]
    half = 16
    inv = (1.0 / (np.float32(10000.0) ** (np.arange(half, dtype=np.float32) * np.float32(2.0) / np.float32(32)))).astype(np.float32)
    pos = np.arange(S, dtype=np.float32)
    ang = (pos[:, None] * inv[None, :]).astype(np.float32)
    cos = np.cos(ang).astype(np.float32).reshape(16, 128, 16).transpose(1, 0, 2).reshape(128, 256)
    sin = np.sin(ang).astype(np.float32).reshape(16, 128, 16).transpose(1, 0, 2).reshape(128, 256)
    c[:, 512:768] = cos
    c[:, 768:1024] = sin
    dist = np.arange(MTW)[None, :] - np.arange(128)[:, None]
    valid = (dist >= 0) & (dist < S)
    d = np.clip(dist, 0, S - 1)
    mult = np.zeros(d.shape, np.float32)
    mult += (d <= 128)
    mult += ((d % 4 == 0) & (d <= 512))
    mult += (d % 16 == 0)
    mult = np.where(valid, mult, 0.0).astype(np.float32)
    bucket = _t5_bucket(d)
    return c, mult, bucket, valid


_CACHE = {}


def _get_program(debug=False, stop_after=99):
    key = (debug, stop_after)
    if key not in _CACHE:
        b = Builder(debug=debug, stop_after=stop_after)
        _CACHE[key] = b.build()
    return _CACHE[key]


def _prep_inputs(inputs):
    f = lambda k: np.asarray(inputs[k], dtype=np.float32)
    x = f("x").reshape(32 * S, D)
    pp = f("p").reshape(32 * S, PLE)
    cst, mult, bucket, valid = _constants()
    rel_bias = f("rel_bias")
    bt = rel_bias[bucket]
    bt = np.where(valid[:, :, None], bt, np.float32(0.0))
    bias_toep = np.ascontiguousarray(bt.transpose(2, 0, 1)).astype(np.float32)
    gv = np.zeros((1, 4096), np.float32)
    segs = [("norm_mix_gain", 0), ("norm_ffn_gain", 1024), ("ple_norm_gain", 2048), ("qn_a_gain", 3072),
            ("kn_a_gain", 3136), ("q_a_gain", 3200), ("kv_a_gain", 3584), ("qn_nope_gain", 3840),
            ("qn_rope_gain", 3904), ("kn_nope_gain", 3936), ("kn_rope_gain", 4000)]
    for k, o in segs:
        v = f(k).reshape(-1)
        gv[0, o:o + v.size] = v
    wr = np.concatenate([f("w_router_group")[0], f("w_router_expert")[0]], axis=1)
    br = np.concatenate([f("b_router_group")[0], f("b_router_expert")[0]], axis=0)[None, :]
    shared = {
        "w_in": f("w_in")[0], "w_q_up": f("w_q_up")[0], "w_kv_up": f("w_kv_up")[0], "w_out": f("w_out")[0],
        "wr": np.ascontiguousarray(wr), "br": np.ascontiguousarray(br),
        "w_exp_gate": f("w_exp_gate")[0], "w_exp_up": f("w_exp_up")[0], "w_exp_down": f("w_exp_down")[0],
        "w_ple_proj": f("w_ple_proj")[0], "w_ple_gate": f("w_ple_gate")[0], "b_ple_gate": f("b_ple_gate"),
        "gvec": gv, "cst": cst, "bias_toep": bias_toep, "mult_toep": mult,
    }
    in_maps = []
    for c in range(NCORES):
        m = dict(shared)
        m["x"] = x[c * NTOK:(c + 1) * NTOK]
        m["p"] = pp[c * NTOK:(c + 1) * NTOK]
        in_maps.append(m)
    return in_maps


def kernel(**inputs):
    nc = _get_program()
    in_maps = _prep_inputs(inputs)
    res = run_bass_kernel_spmd(nc, in_maps, core_ids=list(range(NCORES)))
    outs = [np.asarray(r["out"], dtype=np.float32) for r in res.results]
    return np.concatenate(outs, axis=0).reshape(32, S, D)
```

```python
import contextlib
import math
import numpy as np
import ml_dtypes
import concourse.bass as bass
import concourse.mybir as mybir
from concourse.bass_utils import run_bass_kernel_spmd

F32 = mybir.dt.float32
BF16 = mybir.dt.bfloat16
I32 = mybir.dt.int32
U8 = mybir.dt.uint8
ALU = mybir.AluOpType
AF = mybir.ActivationFunctionType
AX = mybir.AxisListType

NCORES = 8
D = 1024
S = 2048
NSEQ = 4
NTOK = NSEQ * S
NT = NTOK // 128
TPS = S // 128
INC = 2208
PLE = 256
NE = 32
FF = 512
CAP = 768
NSLOT = NE * CAP
TRASH = NSLOT
MTW = 2176
EPS = 1e-6
ENGS = ("pe", "act", "dve", "pool", "sp")


class _Op:
    __slots__ = ("eng", "fn", "deps", "is_dma", "idx", "signal", "semi", "semval", "dsem", "dval", "dprev")

    def __init__(self, eng, fn, is_dma, idx):
        self.eng = eng
        self.fn = fn
        self.deps = []
        self.is_dma = is_dma
        self.idx = idx
        self.signal = False
        self.semi = 0
        self.semval = 0
        self.dsem = None
        self.dval = 0
        self.dprev = 0


class Prog:
    EPOCH = 20000
    NDMA = 16

    def __init__(self, nc):
        self.nc = nc
        self.ops = []
        self.last_w = {}
        self.readers = {}
        self.bar_deps = None
        self.bar_seen = set()
        self.last_eng = {}
        self.dma_since = []

    def _add(self, eng, fn, reads, writes, is_dma):
        op = _Op(eng, fn, is_dma, len(self.ops))
        deps = {}
        for r in reads:
            w = self.last_w.get(r)
            if w is not None:
                deps[w.idx] = w
        for w_ in writes:
            w = self.last_w.get(w_)
            if w is not None:
                deps[w.idx] = w
            for rd in self.readers.get(w_, ()):
                deps[rd.idx] = rd
        for r in reads:
            self.readers.setdefault(r, []).append(op)
        for w_ in writes:
            self.last_w[w_] = op
            self.readers[w_] = []
        if self.bar_deps is not None and eng not in self.bar_seen:
            self.bar_seen.add(eng)
            for d in self.bar_deps:
                deps[d.idx] = d
        deps.pop(op.idx, None)
        for d in deps.values():
            if (not d.is_dma) and (not is_dma) and d.eng == "pe" and eng == "pe":
                continue
            op.deps.append(d)
        self.ops.append(op)
        if is_dma:
            self.dma_since.append(op)
        else:
            self.last_eng[eng] = op
        return op

    def op(self, eng, fn, reads=(), writes=()):
        return self._add(eng, fn, reads, writes, False)

    def dma(self, eng, fn, reads=(), writes=()):
        return self._add(eng, fn, reads, writes, True)

    def barrier(self):
        deps = list(self.last_eng.values()) + list(self.dma_since)
        self.bar_deps = deps
        self.bar_seen = set()
        self.last_w = {}
        self.readers = {}

    def emit(self, final_wait_ops=()):
        nc = self.nc
        ops = self.ops
        for o in ops:
            for d in o.deps:
                d.signal = True
        for o in final_wait_ops:
            o.signal = True
        cnt = {e: 0 for e in ENGS}
        for o in ops:
            if o.is_dma:
                continue
            if o.signal:
                c = cnt[o.eng]
                o.semi = c // self.EPOCH
                o.semval = c % self.EPOCH + 1
                cnt[o.eng] = c + 1
        nsem = {e: (cnt[e] + self.EPOCH - 1) // self.EPOCH for e in ENGS}
        n_sw = 6
        n_hw = self.NDMA - n_sw
        dcount = {"hw": 0, "sw": 0}
        dlast = [0] * self.NDMA
        for o in ops:
            if o.is_dma:
                if o.eng == "pool":
                    k = n_hw + dcount["sw"] % n_sw
                    dcount["sw"] += 1
                else:
                    k = dcount["hw"] % n_hw
                    dcount["hw"] += 1
                o.dsem = k
                o.dprev = dlast[k]
                dlast[k] += 16
                o.dval = dlast[k]
        with contextlib.ExitStack() as st:
            csem = {e: [st.enter_context(nc.semaphore(f"c_{e}_{i}")) for i in range(nsem[e])] for e in ENGS}
            dsem = [st.enter_context(nc.semaphore(f"d_{i}")) for i in range(self.NDMA)]
            block = st.enter_context(nc.Block())
            per_eng = {e: [o for o in ops if o.eng == e] for e in ENGS}
            finals = list(final_wait_ops)

            def run(e, engobj):
                known_c = {}
                known_d = {}

                def wait_for(d):
                    if d.is_dma:
                        if known_d.get(d.dsem, 0) >= d.dval:
                            return
                        engobj.wait_ge(dsem[d.dsem], d.dval)
                        known_d[d.dsem] = d.dval
                    else:
                        key = (d.eng, d.semi)
                        if known_c.get(key, 0) >= d.semval:
                            return
                        engobj.wait_ge(csem[d.eng][d.semi], d.semval)
                        known_c[key] = d.semval

                def reduce_deps(deps):
                    best = {}
                    for d in deps:
                        k = ("d", d.dsem) if d.is_dma else ("c", d.eng, d.semi)
                        v = d.dval if d.is_dma else d.semval
                        if k not in best or v > best[k][0]:
                            best[k] = (v, d)
                    return [bd[1] for bd in best.values()]

                for o in per_eng[e]:
                    for d in sorted(reduce_deps(o.deps), key=lambda z: z.idx):
                        wait_for(d)
                    if o.is_dma:
                        if o.dprev > 0 and known_d.get(o.dsem, 0) < o.dprev:
                            engobj.wait_ge(dsem[o.dsem], o.dprev)
                            known_d[o.dsem] = o.dprev
                        ins = o.fn(engobj)
                        ins.then_inc(dsem[o.dsem], 16)
                    else:
                        ins = o.fn(engobj)
                        if o.signal:
                            ins.then_inc(csem[o.eng][o.semi], 1)
                if e == "sp":
                    for o in reduce_deps(finals):
                        wait_for(o)

            @block.tensor
            def _(eng):
                run("pe", eng)

            @block.scalar
            def _(eng):
                run("act", eng)

            @block.vector
            def _(eng):
                run("dve", eng)

            @block.gpsimd
            def _(eng):
                run("pool", eng)

            @block.sync
            def _(eng):
                run("sp", eng)


class Builder:
    def __init__(self, debug=False, stop_after=99):
        self.debug = debug
        self.stop_after = stop_after
        self.nc = bass.Bass("TRN2", target_bir_lowering=False)
        self.p = Prog(self.nc)
        self.arena_off = 0
        self.rr = 0
        self.out_ops = []

    def din(self, name, shape, dt=F32):
        return self.nc.dram_tensor(name, list(shape), dt, kind="ExternalInput").ap()

    def dscr(self, name, shape, dt, out=False):
        kind = "ExternalOutput" if (out or (self.debug and name in self.debug)) else "Internal"
        return self.nc.dram_tensor(name, list(shape), dt, kind=kind).ap()

    def sb(self, shape, dt):
        esz = {F32: 4, BF16: 2, I32: 4}[dt]
        n = 1
        for s in shape[1:]:
            n *= s
        nbytes = (n * esz + 63) // 64 * 64
        off = self.arena_off
        self.arena_off += nbytes
        assert self.arena_off <= self.arena_bytes, ("SBUF arena overflow", self.arena_off)
        v = self.arena[:, off:off + n * esz].bitcast(dt)
        if len(shape) == 3:
            v = v.rearrange("p (a b) -> p a b", a=shape[1])
        elif len(shape) == 4:
            v = v.rearrange("p (a b c) -> p a b c", a=shape[1], b=shape[2])
        return v

    def DMA(self, q, out, in_, r, w):
        o = self.p.dma(q, lambda e: e.dma_start(out=out, in_=in_), reads=r, writes=w)
        if "out" in w or (self.debug and any(k in self.debug for k in w)):
            self.out_ops.append(o)
        return o

    def MM(self, out, lhsT, rhs, start, stop, r, w):
        return self.p.op("pe", lambda e: e.matmul(out, lhsT=lhsT, rhs=rhs, start=start, stop=stop,
                                                  skip_group_check=True), reads=r, writes=w)

    def TR(self, out, in_, ident, r, w):
        return self.p.op("pe", lambda e: e.transpose(out=out, in_=in_, identity=ident), reads=r, writes=w)

    def ACT(self, out, in_, func, r, w, bias=None, scale=None, accum=None):
        kw = {}
        if bias is not None:
            kw["bias"] = bias
        if scale is not None:
            kw["scale"] = scale
        if accum is not None:
            kw["accum_out"] = accum
        return self.p.op("act", lambda e: e.activation(out=out, in_=in_, func=func, **kw), reads=r, writes=w)

    def CP(self, eng, out, in_, r, w):
        if eng == "act":
            return self.p.op("act", lambda e: e.copy(out=out, in_=in_), reads=r, writes=w)
        return self.p.op(eng, lambda e: e.tensor_copy(out=out, in_=in_), reads=r, writes=w)

    def TT(self, eng, out, in0, in1, op, r, w):
        return self.p.op(eng, lambda e: e.tensor_tensor(out=out, in0=in0, in1=in1, op=op), reads=r, writes=w)

    def TS(self, eng, out, in0, s1, s2, op0, op1, r, w):
        if op1 is None:
            return self.p.op(eng, lambda e: e.tensor_scalar(out=out, in0=in0, scalar1=s1, scalar2=None, op0=op0),
                             reads=r, writes=w)
        return self.p.op(eng, lambda e: e.tensor_scalar(out=out, in0=in0, scalar1=s1, scalar2=s2, op0=op0, op1=op1),
                         reads=r, writes=w)

    def STT(self, eng, out, in0, scalar, in1, op0, op1, r, w):
        return self.p.op(eng, lambda e: e.scalar_tensor_tensor(out=out, in0=in0, scalar=scalar, in1=in1,
                                                               op0=op0, op1=op1), reads=r, writes=w)

    def RED(self, eng, out, in_, op, r, w):
        return self.p.op(eng, lambda e: e.tensor_reduce(out=out, in_=in_, axis=AX.X, op=op), reads=r, writes=w)

    def RCP(self, out, in_, r, w):
        return self.p.op("dve", lambda e: e.reciprocal(out=out, in_=in_), reads=r, writes=w)

    def MS(self, eng, out, val, w, r=()):
        return self.p.op(eng, lambda e: e.memset(out, val), reads=r, writes=w)

    def rstd(self, ss, n, nm, r_extra=()):
        self.ACT(ss, ss, AF.Ln, [nm, "epsc"] + list(r_extra), [nm], bias=self.epsc[:, 0:1], scale=1.0 / n)
        self.ACT(ss, ss, AF.Exp, [nm], [nm], scale=-0.5)

    def build(self):
        nc = self.nc
        dbg = self.debug
        x = self.din("x", [NTOK, D])
        pin = self.din("p", [NTOK, PLE])
        w_in = self.din("w_in", [D, INC])
        w_q_up = self.din("w_q_up", [384, 768])
        w_kv_up = self.din("w_kv_up", [256, 1024])
        w_out = self.din("w_out", [D, D])
        wr = self.din("wr", [D, 36])
        br = self.din("br", [1, 36])
        self.br_d = br
        w_gate = self.din("w_exp_gate", [NE, D, FF])
        w_up = self.din("w_exp_up", [NE, D, FF])
        w_down = self.din("w_exp_down", [NE, FF, D])
        w_ple_proj = self.din("w_ple_proj", [PLE, D])
        w_ple_gate = self.din("w_ple_gate", [D, D])
        b_ple_gate = self.din("b_ple_gate", [1, D])
        gvec = self.din("gvec", [1, 4096])
        cst = self.din("cst", [128, 1024])
        bias_toep = self.din("bias_toep", [8, 128, MTW])
        mult_toep = self.din("mult_toep", [128, MTW])
        out = self.nc.dram_tensor("out", [NTOK, D], F32, kind="ExternalOutput").ap()
        qTa_d = self.dscr("qTa_d", [512, NTOK], BF16)
        kTa_d = self.dscr("kTa_d", [512, NTOK], BF16)
        va_d = self.dscr("va_d", [NTOK, 512], BF16)
        qTm_d = self.dscr("qTm_d", [768, NTOK], BF16)
        kTm_d = self.dscr("kTm_d", [768, NTOK], BF16)
        vm_d = self.dscr("vm_d", [NTOK, 512], BF16)
        mt_d = self.dscr("mt_d", [8, 128, MTW], BF16)
        h1_d = self.dscr("h1_d", [NTOK, D], F32)
        xg_d = self.dscr("xg_d", [NSLOT + 128, D], BF16)
        y_d = self.dscr("y_d", [NSLOT + 128, D], F32)
        rt_d = self.dscr("rt_d", [128, NT * 4], F32, out=False)

        with contextlib.ExitStack() as st:
            self.arena_bytes = 200 * 1024
            self.arena = st.enter_context(nc.sbuf_tensor("arena", [128, self.arena_bytes], U8))
            PS = [st.enter_context(nc.psum_tensor(f"ps{i}", [128, 512], F32)) for i in range(8)]
            PSb = [t[:, :].bitcast(BF16) for t in PS]
            PS = [t[:, :] for t in PS]
            self.PS, self.PSb = PS, PSb

            C = self.sb([128, 1024], F32)
            self.C = C
            ident_f = C[:, 0:128]
            eoff = C[:, 384:416]
            cos_t = C[:, 512:768].rearrange("p (t i) -> p t i", t=16)
            sin_t = C[:, 768:1024].rearrange("p (t i) -> p t i", t=16)
            Cb = self.sb([128, 512], BF16)
            ident_b = Cb[:, 0:128]
            causal_b = Cb[:, 128:256]
            ustrict_b = Cb[:, 256:384]
            ones_b = Cb[:, 384:512]
            onesf = self.sb([128, 128], F32)
            G = self.sb([128, 4096], F32)
            self.epsc = self.sb([128, 1], F32)
            g_mix = G[:, 0:1024]
            g_ffn = G[:, 1024:2048]
            g_ple = G[:, 2048:3072]
            g_qa = G[:, 3072:3136]
            g_ka = G[:, 3136:3200]
            g_cq = G[:, 3200:3584]
            g_ckv = G[:, 3584:3840]
            g_qn = G[:, 3840:3904]
            g_qr = G[:, 3904:3936]
            g_kn = G[:, 3936:4000]
            g_kr = G[:, 4000:4032]
            slots_all = self.sb([128, NT, 2], I32)
            wts_all = self.sb([128, NT, 2], F32)
            acc_b = self.sb([128, 32], BF16)
            wr_sb = self.sb([128, 8, 36], F32)
            br_sb = self.sb([1, 36], F32)
            persist_mark = self.arena_off

            p = self.p
            self.DMA("sp", C, cst, [], ["C"])
            self.DMA("sp", G, gvec[0:1, :].partition_broadcast(128)[:, 0, :], [], ["G"])
            self.DMA("sp", wr_sb, wr.rearrange("(c p) n -> p c n", p=128), [], ["wr"])
            self.DMA("sp", br_sb[0:1, :], br, [], ["br"])
            self.MS("pool", self.epsc, EPS, ["epsc"])
            self.MS("pool", onesf, 1.0, ["onesf"])
            self.MS("pool", acc_b, 0.0, ["acc"])
            self.MS("pool", Cb[:, 384:512], 1.0, ["Cb"])
            self.CP("dve", Cb[:, 0:384], C[:, 0:384], ["C", "Cb"], ["Cb"])
            self.MS("pool", slots_all, 0, ["slots"])
            self.MS("pool", wts_all, 0.0, ["wts"])

            w_in_b = self.sb([128, 8, INC], BF16)
            wq_b = self.sb([128, 3, 768], BF16)
            wkv_b = self.sb([128, 2, 1024], BF16)
            zt = self.sb([128, 7, 1024], BF16)
            ztf = self.sb([128, 1024], F32)
            self.MS("pool", zt, 0.0, ["zt"])
            self.MS("pool", ztf, 0.0, ["ztf"])
            p1_mark = self.arena_off
            stg = [self.sb([128, INC], F32) for _ in range(2)]
            for c in range(8):
                s_ = stg[c % 2]
                self.DMA("sp", s_, w_in[c * 128:(c + 1) * 128, :], [], [f"stg{c%2}"])
                self.CP(("act", "dve")[c % 2], w_in_b[:, c, :], s_, [f"stg{c%2}"], ["w_in_b"])
            for c in range(3):
                s_ = stg[c % 2]
                self.DMA("sp", s_[:, 0:768], w_q_up[c * 128:(c + 1) * 128, :], [], [f"stg{c%2}"])
                self.CP(("act", "dve")[c % 2], wq_b[:, c, :], s_[:, 0:768], [f"stg{c%2}"], ["wq_b"])
            for c in range(2):
                s_ = stg[c % 2]
                self.DMA("sp", s_[:, 0:1024], w_kv_up[c * 128:(c + 1) * 128, :], [], [f"stg{c%2}"])
                self.CP(("act", "dve")[c % 2], wkv_b[:, c, :], s_[:, 0:1024], [f"stg{c%2}"], ["wkv_b"])
            mult_sb = self.sb([128, MTW], F32)
            self.DMA("sp", mult_sb, mult_toep, [], ["mult"])
            mtb = [self.sb([128, MTW], BF16) for _ in range(2)]
            for h in range(8):
                s_ = stg[h % 2]
                self.DMA("sp", s_[:, 0:MTW], bias_toep[h], [], [f"stg{h%2}"])
                self.ACT(s_[:, 0:MTW], s_[:, 0:MTW], AF.Exp, [f"stg{h%2}"], [f"stg{h%2}"])
                self.TT("dve", mtb[h % 2], s_[:, 0:MTW], mult_sb, ALU.mult, [f"stg{h%2}", "mult"], [f"mtb{h%2}"])
                self.DMA("pool", mt_d[h], mtb[h % 2], [f"mtb{h%2}"], ["mt_d"])
            xg_v = xg_d.rearrange("(r p) n -> p r n", p=128)
            nrt = (NSLOT + 128) // 128
            self.zero_jobs = [(xg_v[:, i:i + min(7, nrt - i), :], zt[:, 0:min(7, nrt - i), :], "zt", "xg_d")
                              for i in range(0, nrt, 7)]
            self.zero_jobs.append((y_d[NSLOT:NSLOT + 128, :], ztf, "ztf", "y_d"))
            p.barrier()
            self.arena_off = p1_mark

            if self.stop_after >= 1:
                self.phase1(x, w_in_b, wq_b, wkv_b, qTa_d, kTa_d, va_d, qTm_d, kTm_d, vm_d,
                            ident_b, g_mix, g_qa, g_ka, g_cq, g_ckv, g_qn, g_qr, g_kn, g_kr, cos_t, sin_t)
            p.barrier()
            self.arena_off = persist_mark
            if self.stop_after >= 2:
                self.phase2(x, w_out, qTa_d, kTa_d, va_d, qTm_d, kTm_d, vm_d, mt_d, h1_d, xg_d, rt_d,
                            ident_f, ident_b, causal_b, ustrict_b, ones_b, onesf, eoff, g_ffn,
                            wr_sb, br_sb, slots_all, wts_all, acc_b)
            p.barrier()
            self.arena_off = persist_mark
            if self.stop_after >= 3:
                self.phase3(w_gate, w_up, w_down, xg_d, y_d, ident_b)
            p.barrier()
            self.arena_off = persist_mark
            if self.stop_after >= 4:
                self.phase4(pin, w_ple_proj, w_ple_gate, b_ple_gate, h1_d, y_d, out, ident_b, ones_b, g_ple,
                            slots_all, wts_all)
            p.emit(final_wait_ops=self.out_ops)
        return nc

    def headnorm(self, src_ps, nh, hd, stride, off, gain, dst, dst_nm, ps_nm, n_real, tag, sqt, ssum, tmpt):
        v = src_ps
        sq, sqk = sqt
        tmp, tmpk = tmpt
        self.ACT(sq[:, 0:nh * stride], v, AF.Square, [ps_nm], [sqk])
        sqv = sq[:, 0:nh * stride].rearrange("p (h d) -> p h d", h=nh)[:, :, off:off + hd]
        self.RED("dve", ssum, sqv, ALU.add, [sqk], [tag + "ss"])
        self.rstd(ssum, n_real, tag + "ss")
        vv = v.rearrange("p (h d) -> p h d", h=nh)[:, :, off:off + hd]
        self.TT("dve", tmp, vv, ssum.unsqueeze(2).to_broadcast([128, nh, hd]), ALU.mult,
                [ps_nm, tag + "ss"], [tmpk])
        self.TT("pool", dst, tmp, gain.unsqueeze(1).to_broadcast([128, nh, hd]), ALU.mult,
                [tmpk, "G"], [dst_nm])

    def phase1(self, x, w_in_b, wq_b, wkv_b, qTa_d, kTa_d, va_d, qTm_d, kTm_d, vm_d,
               ident_b, g_mix, g_qa, g_ka, g_cq, g_ckv, g_qn, g_qr, g_kn, g_kr, cos_t, sin_t):
        PS, PSb = self.PS, self.PSb
        sb = self.sb
        junk = sb([128, D], BF16)
        NS = 2
        B = []
        for si in range(NS):
            d = {}
            d["xt"] = [sb([128, D], F32) for _ in range(2)]
            d["xn"] = sb([128, D], BF16)
            d["xT"] = sb([128, 8, 128], BF16)
            d["sq"] = [sb([128, 1024], F32) for _ in range(2)]
            d["tmp"] = [sb([128, 8, 64], F32) for _ in range(2)]
            d["ss"] = [sb([128, 8], F32) for _ in range(10)]
            d["qan"] = sb([128, 8, 64], BF16)
            d["kan"] = sb([128, 8, 64], BF16)
            d["cqn"] = sb([128, 384], BF16)
            d["cqT"] = sb([128, 3, 128], BF16)
            d["ckvn"] = sb([128, 256], BF16)
            d["ckvT"] = sb([128, 2, 128], BF16)
            d["qm"] = sb([128, 8, 96], BF16)
            d["km"] = sb([128, 8, 96], BF16)
            d["rp"] = sb([128, 8, 32], F32)
            d["r1"] = sb([128, 8, 16], F32)
            d["r2"] = sb([128, 8, 16], F32)
            d["krn"] = sb([128, 32], F32)
            d["kro"] = sb([128, 32], BF16)
            d["k1"] = sb([128, 16], F32)
            d["k2"] = sb([128, 16], F32)
            d["bank"] = 0
            d["sqc"] = 0
            d["tmc"] = 0
            d["xc"] = 0
            B.append(d)
        qaT_s = [sb([128, 4, 256], BF16) for _ in range(2)]
        kaT_s = [sb([128, 4, 256], BF16) for _ in range(2)]
        qmT_s = [sb([128, 8, 256], BF16) for _ in range(2)]
        kmT_s = [sb([128, 8, 256], BF16) for _ in range(2)]
        va_s = [sb([128, 2, 512], BF16) for _ in range(2)]
        vm_s = [sb([128, 2, 8, 64], BF16) for _ in range(2)]
        chunks = [(0, 512), (512, 512), (1024, 512), (1536, 384), (1920, 288)]

        def tile_gen(t, si):
            d = B[si]
            K_ = f"s{si}"
            m, ti = divmod(t, 2)
            mi = m % 2
            pj = t % TPS

            def nb():
                b_ = 4 * si + d["bank"]
                d["bank"] = (d["bank"] + 1) % 4
                return b_

            def nsq():
                k = d["sqc"] % 2
                d["sqc"] += 1
                return d["sq"][k], f"{K_}sq{k}"

            def ntmp():
                k = d["tmc"] % 2
                d["tmc"] += 1
                return d["tmp"][k], f"{K_}tmp{k}"

            ssh = d["ss"]
            xi = d["xc"] % 2
            d["xc"] += 1
            X = d["xt"][xi]
            xk = f"{K_}xt{xi}"
            self.DMA("sp", X, x[t * 128:(t + 1) * 128, :], [], [xk])
            s1 = ssh[8][:, 0:1]
            s1k = f"{K_}ss1"
            self.MS("pool", s1, 0.0, [s1k])
            yield
            self.ACT(junk, X, AF.Square, [xk, s1k], [s1k], accum=s1)
            self.rstd(s1, D, s1k)
            yield
            XN, xnk = d["xn"], f"{K_}xn"
            self.STT("dve", XN, X, s1, g_mix, ALU.mult, ALU.mult, [xk, s1k, "G"], [xnk])
            yield
            b0 = nb()
            for c in range(8):
                self.TR(PSb[b0][:, c * 128:(c + 1) * 128], XN[:, c * 128:(c + 1) * 128], ident_b, [xnk, "Cb"], [f"ps{b0}"])
            yield
            XT, xtk = d["xT"], f"{K_}xT"
            self.CP("act", XT, PSb[b0][:, 0:1024].rearrange("p (c n) -> p c n", c=8), [f"ps{b0}"], [xtk])
            yield

            def proj(ci):
                c0, cw = chunks[ci]
                b_ = nb()
                for c in range(8):
                    self.MM(PS[b_][:, 0:cw], XT[:, c, :], w_in_b[:, c, c0:c0 + cw], c == 0, c == 7,
                            [xtk, "w_in_b"], [f"ps{b_}"])
                return b_

            def headnorm_g(src_ps, ps_nm, gain, dst, dst_nm, ssum, tag):
                sq, sqk = nsq()
                tmp, tmpk = ntmp()
                self.ACT(sq[:, 0:512], src_ps, AF.Square, [ps_nm], [sqk])
                yield
                self.RED("dve", ssum, sq[:, 0:512].rearrange("p (h d) -> p h d", h=8), ALU.add, [sqk], [tag])
                yield
                self.rstd(ssum, 64, tag)
                yield
                self.TT("dve", tmp, src_ps.rearrange("p (h d) -> p h d", h=8), ssum.unsqueeze(2).to_broadcast([128, 8, 64]),
                        ALU.mult, [ps_nm, tag], [tmpk])
                yield
                self.TT("pool", dst, tmp, gain.unsqueeze(1).to_broadcast([128, 8, 64]), ALU.mult, [tmpk, "G"], [dst_nm])
                yield

            def trans_pairs(src, src_nm, stage, snm):
                bt = nb()
                for c in range(4):
                    self.TR(PSb[bt][:, c * 128:(c + 1) * 128], src[:, 2 * c:2 * c + 2, :].rearrange("p a b -> p (a b)"),
                            ident_b, [src_nm, "Cb"], [f"ps{bt}"])
                yield
                self.CP("dve", stage[:, :, ti * 128:(ti + 1) * 128],
                        PSb[bt][:, 0:512].rearrange("p (c n) -> p c n", c=4), [f"ps{bt}"], [snm])
                yield

            bc0 = proj(0)
            yield
            bc1 = proj(1)
            yield
            bc2 = proj(2)
            yield
            self.CP("act", va_s[mi][:, ti, :], PS[bc2][:, 0:512], [f"ps{bc2}"], [f"vas{mi}"])
            yield
            bc3 = proj(3)
            yield
            yield from headnorm_g(PS[bc0][:, 0:512], f"ps{bc0}", g_qa, d["qan"], f"{K_}qan", ssh[0], f"{K_}ssqa")
            bc4 = proj(4)
            yield
            yield from headnorm_g(PS[bc1][:, 0:512], f"ps{bc1}", g_ka, d["kan"], f"{K_}kan", ssh[1], f"{K_}sska")
            yield from trans_pairs(d["qan"], f"{K_}qan", qaT_s[mi], f"qaTs{mi}")
            yield from trans_pairs(d["kan"], f"{K_}kan", kaT_s[mi], f"kaTs{mi}")
            s2 = ssh[9][:, 0:1]
            cqk = f"{K_}cqss"
            self.MS("pool", s2, 0.0, [cqk])
            yield
            self.ACT(junk[:, 0:384], PS[bc3][:, 0:384], AF.Square, [f"ps{bc3}", cqk], [cqk], accum=s2)
            self.rstd(s2, 384, cqk)
            yield
            cqn, cqnk = d["cqn"], f"{K_}cqn"
            self.STT("dve", cqn, PS[bc3][:, 0:384], s2, g_cq, ALU.mult, ALU.mult, [f"ps{bc3}", cqk, "G"], [cqnk])
            yield
            bt = nb()
            for c in range(3):
                self.TR(PSb[bt][:, c * 128:(c + 1) * 128], cqn[:, c * 128:(c + 1) * 128], ident_b, [cqnk, "Cb"], [f"ps{bt}"])
            yield
            cqT, cqTk = d["cqT"], f"{K_}cqT"
            self.CP("dve", cqT, PSb[bt][:, 0:384].rearrange("p (c n) -> p c n", c=3), [f"ps{bt}"], [cqTk])
            yield
            s5 = ssh[7]
            ckk = f"{K_}ckss"
            self.MS("pool", s5[:, 0:2], 0.0, [ckk])
            yield
            self.ACT(junk[:, 0:256], PS[bc4][:, 0:256], AF.Square, [f"ps{bc4}", ckk], [ckk], accum=s5[:, 0:1])
            self.ACT(junk[:, 256:288], PS[bc4][:, 256:288], AF.Square, [f"ps{bc4}", ckk], [ckk], accum=s5[:, 1:2])
            self.rstd(s5[:, 0:1], 256, ckk)
            self.rstd(s5[:, 1:2], 32, ckk)
            yield
            ckvn, ckvnk = d["ckvn"], f"{K_}ckvn"
            krn, kro, k1, k2 = d["krn"], d["kro"], d["k1"], d["k2"]
            self.STT("dve", ckvn, PS[bc4][:, 0:256], s5[:, 0:1], g_ckv, ALU.mult, ALU.mult, [f"ps{bc4}", ckk, "G"], [ckvnk])
            self.STT("dve", krn, PS[bc4][:, 256:288], s5[:, 1:2], g_kr, ALU.mult, ALU.mult, [f"ps{bc4}", ckk, "G"], [f"{K_}krn"])
            yield
            bq0, bq1 = nb(), nb()
            for c in range(3):
                self.MM(PS[bq0][:, 0:480], cqT[:, c, :], wq_b[:, c, 0:480], c == 0, c == 2, [cqTk, "wq_b"], [f"ps{bq0}"])
            for c in range(3):
                self.MM(PS[bq1][:, 0:288], cqT[:, c, :], wq_b[:, c, 480:768], c == 0, c == 2, [cqTk, "wq_b"], [f"ps{bq1}"])
            yield
            ck, sk = cos_t[:, pj, :], sin_t[:, pj, :]
            self.TT("dve", k1, krn[:, 0:16], ck, ALU.mult, [f"{K_}krn", "C"], [f"{K_}k1"])
            self.TT("dve", k2, krn[:, 16:32], sk, ALU.mult, [f"{K_}krn", "C"], [f"{K_}k2"])
            self.TT("dve", kro[:, 0:16], k1, k2, ALU.subtract, [f"{K_}k1", f"{K_}k2"], [f"{K_}kro"])
            self.TT("dve", k1, krn[:, 16:32], ck, ALU.mult, [f"{K_}krn", "C", f"{K_}kro"], [f"{K_}k1"])
            self.TT("dve", k2, krn[:, 0:16], sk, ALU.mult, [f"{K_}krn", "C", f"{K_}kro"], [f"{K_}k2"])
            self.TT("dve", kro[:, 16:32], k1, k2, ALU.add, [f"{K_}k1", f"{K_}k2"], [f"{K_}kro"])
            km, kmk = d["km"], f"{K_}km"
            self.CP("dve", km[:, :, 64:96], kro.unsqueeze(1).to_broadcast([128, 8, 32]), [f"{K_}kro"], [kmk])
            yield
            qm, qmk = d["qm"], f"{K_}qm"
            rp, rpk = d["rp"], f"{K_}rp"
            for (bq, h0, nh, ia, ib) in ((bq0, 0, 5, 3, 5), (bq1, 5, 3, 4, 6)):
                tag = f"{K_}qm{h0}"
                src = PS[bq][:, 0:nh * 96]
                sq, sqk = nsq()
                tmpa, tmpk = ntmp()
                self.ACT(sq[:, 0:nh * 96], src, AF.Square, [f"ps{bq}"], [sqk])
                yield
                sqv = sq[:, 0:nh * 96].rearrange("p (h d) -> p h d", h=nh)
                sn = ssh[ia][:, 0:nh]
                sr = ssh[ib][:, 0:nh]
                self.RED("dve", sn, sqv[:, :, 0:64], ALU.add, [sqk], [tag + "sn"])
                self.RED("dve", sr, sqv[:, :, 64:96], ALU.add, [sqk], [tag + "sr"])
                yield
                self.rstd(sn, 64, tag + "sn")
                self.rstd(sr, 32, tag + "sr")
                yield
                sv = src.rearrange("p (h d) -> p h d", h=nh)
                self.TT("dve", tmpa[:, 0:nh, :], sv[:, :, 0:64], sn.unsqueeze(2).to_broadcast([128, nh, 64]), ALU.mult,
                        [f"ps{bq}", tag + "sn"], [tmpk])
                self.TT("dve", rp[:, h0:h0 + nh, :], sv[:, :, 64:96], sr.unsqueeze(2).to_broadcast([128, nh, 32]), ALU.mult,
                        [f"ps{bq}", tag + "sr"], [rpk])
                yield
                self.TT("pool", qm[:, h0:h0 + nh, 0:64], tmpa[:, 0:nh, :], g_qn.unsqueeze(1).to_broadcast([128, nh, 64]),
                        ALU.mult, [tmpk, "G"], [qmk])
                yield
            r1, r2 = d["r1"], d["r2"]
            cosb = cos_t[:, pj, :].unsqueeze(1).to_broadcast([128, 8, 16])
            sinb = sin_t[:, pj, :].unsqueeze(1).to_broadcast([128, 8, 16])
            self.TT("pool", rp, rp, g_qr.unsqueeze(1).to_broadcast([128, 8, 32]), ALU.mult, [rpk, "G"], [rpk])
            self.TT("pool", r1, rp[:, :, 0:16], cosb, ALU.mult, [rpk, "C"], [f"{K_}r1"])
            self.TT("pool", r2, rp[:, :, 16:32], sinb, ALU.mult, [rpk, "C"], [f"{K_}r2"])
            self.TT("pool", qm[:, :, 64:80], r1, r2, ALU.subtract, [f"{K_}r1", f"{K_}r2"], [qmk])
            self.TT("pool", r1, rp[:, :, 16:32], cosb, ALU.mult, [rpk, "C", qmk], [f"{K_}r1"])
            self.TT("pool", r2, rp[:, :, 0:16], sinb, ALU.mult, [rpk, "C", qmk], [f"{K_}r2"])
            self.TT("pool", qm[:, :, 80:96], r1, r2, ALU.add, [f"{K_}r1", f"{K_}r2"], [qmk])
            yield
            bt = nb()
            for h in range(8):
                self.TR(PSb[bt][0:96, h * 128:(h + 1) * 128], qm[:, h, :], ident_b, [qmk, "Cb"], [f"ps{bt}"])
            yield
            self.CP("act", qmT_s[mi][0:96, :, ti * 128:(ti + 1) * 128],
                    PSb[bt][0:96, 0:1024].rearrange("p (c n) -> p c n", c=8), [f"ps{bt}"], [f"qmTs{mi}"])
            yield
            bt = nb()
            for c in range(2):
                self.TR(PSb[bt][:, c * 128:(c + 1) * 128], ckvn[:, c * 128:(c + 1) * 128], ident_b, [ckvnk, "Cb"], [f"ps{bt}"])
            yield
            ckvT, ckvTk = d["ckvT"], f"{K_}ckvT"
            self.CP("dve", ckvT, PSb[bt][:, 0:256].rearrange("p (c n) -> p c n", c=2), [f"ps{bt}"], [ckvTk])
            yield
            bk0, bk1 = nb(), nb()
            for half, bk in ((0, bk0), (1, bk1)):
                for c in range(2):
                    self.MM(PS[bk][:, 0:512], ckvT[:, c, :], wkv_b[:, c, half * 512:(half + 1) * 512], c == 0, c == 1,
                            [ckvTk, "wkv_b"], [f"ps{bk}"])
            yield
            for half, bk in ((0, bk0), (1, bk1)):
                tag = f"{K_}kv{half}"
                src = PS[bk][:, 0:512]
                h0 = half * 4
                sq, sqk = nsq()
                tmpa, tmpk = ntmp()
                self.ACT(sq[:, 0:512], src, AF.Square, [f"ps{bk}"], [sqk])
                yield
                sqv = sq[:, 0:512].rearrange("p (h d) -> p h d", h=4)
                sn = ssh[2][:, half * 4:half * 4 + 4]
                self.RED("dve", sn, sqv[:, :, 0:64], ALU.add, [sqk], [tag + "sn"])
                yield
                self.rstd(sn, 64, tag + "sn")
                sv = src.rearrange("p (h d) -> p h d", h=4)
                self.CP("act", vm_s[mi][:, ti, h0:h0 + 4, :], sv[:, :, 64:128], [f"ps{bk}", tag + "sn"], [f"vms{mi}"])
                yield
                self.TT("dve", tmpa[:, 0:4, :], sv[:, :, 0:64], sn.unsqueeze(2).to_broadcast([128, 4, 64]), ALU.mult,
                        [f"ps{bk}", tag + "sn", f"vms{mi}"], [tmpk])
                yield
                self.TT("pool", km[:, h0:h0 + 4, 0:64], tmpa[:, 0:4, :], g_kn.unsqueeze(1).to_broadcast([128, 4, 64]),
                        ALU.mult, [tmpk, "G"], [kmk])
                yield
            bt = nb()
            for h in range(8):
                self.TR(PSb[bt][0:96, h * 128:(h + 1) * 128], km[:, h, :], ident_b, [kmk, "Cb"], [f"ps{bt}"])
            yield
            self.CP("act", kmT_s[mi][0:96, :, ti * 128:(ti + 1) * 128],
                    PSb[bt][0:96, 0:1024].rearrange("p (c n) -> p c n", c=8), [f"ps{bt}"], [f"kmTs{mi}"])
            yield
            if ti == 1:
                cs = slice(m * 256, (m + 1) * 256)
                self.DMA("pool", qTa_d.rearrange("(c q) n -> q c n", q=128)[:, :, cs], qaT_s[mi], [f"qaTs{mi}"], ["qTa_d"])
                self.DMA("pool", kTa_d.rearrange("(c q) n -> q c n", q=128)[:, :, cs], kaT_s[mi], [f"kaTs{mi}"], ["kTa_d"])
                self.DMA("pool", qTm_d.rearrange("(h d) n -> d h n", d=96)[:, :, cs], qmT_s[mi][0:96], [f"qmTs{mi}"], ["qTm_d"])
                self.DMA("pool", kTm_d.rearrange("(h d) n -> d h n", d=96)[:, :, cs], kmT_s[mi][0:96], [f"kmTs{mi}"], ["kTm_d"])
                self.DMA("pool", va_d[cs, :].rearrange("(t p) n -> p t n", p=128), va_s[mi], [f"vas{mi}"], ["va_d"])
                self.DMA("pool", vm_d[cs, :].rearrange("(t p) n -> p t n", p=128),
                         vm_s[mi].rearrange("p t h d -> p t (h d)"), [f"vms{mi}"], ["vm_d"])

        for m in range(NT // 2):
            if m < len(self.zero_jobs):
                zo, zi, zr, zw = self.zero_jobs[m]
                self.DMA("sp", zo, zi, [zr], [zw])
            gens = [tile_gen(2 * m, 0), tile_gen(2 * m + 1, 1)]
            alive = [True, True]
            while any(alive):
                for gi_, g_ in enumerate(gens):
                    if alive[gi_]:
                        try:
                            next(g_)
                        except StopIteration:
                            alive[gi_] = False

    def phase2(self, x, w_out, qTa_d, kTa_d, va_d, qTm_d, kTm_d, vm_d, mt_d, h1_d, xg_d, rt_d,
               ident_f, ident_b, causal_b, ustrict_b, ones_b, onesf, eoff, g_ffn,
               wr_sb, br_sb, slots_all, wts_all, acc_b):
        PS, PSb = self.PS, self.PSb
        sb = self.sb
        w_out_b = sb([128, 8, D], BF16)
        br_d = self.br_d
        stg = [sb([128, D], F32) for _ in range(2)]
        for c in range(8):
            self.DMA("sp", stg[c % 2], w_out[c * 128:(c + 1) * 128, :], [], [f"wstg{c%2}"])
            self.CP(("act", "dve")[c % 2], w_out_b[:, c, :], stg[c % 2], [f"wstg{c%2}"], ["w_out_b"])
        qT = [sb([128, S], BF16) for _ in range(2)]
        kT = [sb([128, S], BF16) for _ in range(2)]
        vaug = [sb([128, TPS, 128], BF16) for _ in range(2)]
        mt = [sb([128, MTW], BF16) for _ in range(2)]
        mixTs = [sb([128, 8, S], BF16) for _ in range(2)]
        Pt = [sb([128, 512], BF16) for _ in range(4)]
        rden = [sb([128, 512], F32) for _ in range(2)]
        xr = [sb([128, D], F32) for _ in range(2)]
        h1 = [sb([128, D], F32) for _ in range(3)]
        NXB = 7
        xn2b = [sb([128, D], BF16) for _ in range(NXB)]
        xn2T = [sb([128, 8, 128], BF16) for _ in range(2)]
        junk = sb([128, D], BF16)
        junk4 = sb([128, 4], F32)
        ss2 = [sb([128, 1], F32) for _ in range(2)]
        Ls = [sb([128, 36], F32) for _ in range(3)]
        NRT = 6
        rt = [sb([128, 8], F32) for _ in range(NRT)]
        gm = sb([128, 4], F32)
        pen = sb([128, 4], F32)
        Em = sb([128, 4, 8], F32)
        top8 = sb([128, 8], F32)
        OH = [sb([128, 2, 32], F32) for _ in range(2)]
        OHb = [sb([128, 32], BF16) for _ in range(2)]
        prod = sb([128, 2, 32], F32)
        prod2 = sb([128, 2, 32], F32)
        rk = sb([128, 2], F32)
        ek = sb([128, 2], F32)
        okk = [sb([128, 2], F32) for _ in range(2)]
        slf = sb([128, 2], F32)
        wr_b = sb([128, 8, 36], BF16)
        br_bc = sb([128, 36], F32)
        self.CP("dve", wr_b, wr_sb, ["wr"], ["wr_b"])
        self.DMA("sp", br_bc, br_d[0:1, :].partition_broadcast(128)[:, 0, :], [], ["br_bc"])
        for i in range(2):
            self.MS("pool", vaug[i], 1.0, [f"vaug{i}"])

        LA = 2
        mcount = [0]

        def head_loads(b, h, i):
            tok0 = b * S
            if h < 8:
                self.DMA("sp", qT[i][0:64, :], qTa_d[h * 64:(h + 1) * 64, tok0:tok0 + S], [], [f"qT{i}"])
                self.DMA("sp", kT[i][0:64, :], kTa_d[h * 64:(h + 1) * 64, tok0:tok0 + S], [], [f"kT{i}"])
                self.DMA("sp", vaug[i][:, :, 0:64],
                         va_d[tok0:tok0 + S, h * 64:(h + 1) * 64].rearrange("(t p) d -> p t d", p=128), [], [f"vaug{i}"])
                self.DMA("sp", mt[i], mt_d[h], [], [f"mt{i}"])
            else:
                hh = h - 8
                self.DMA("sp", qT[i][0:96, :], qTm_d[hh * 96:(hh + 1) * 96, tok0:tok0 + S], [], [f"qT{i}"])
                self.DMA("sp", kT[i][0:96, :], kTm_d[hh * 96:(hh + 1) * 96, tok0:tok0 + S], [], [f"kT{i}"])
                self.DMA("sp", vaug[i][:, :, 0:64],
                         vm_d[tok0:tok0 + S, hh * 64:(hh + 1) * 64].rearrange("(t p) d -> p t d", p=128), [], [f"vaug{i}"])

        loaded = set()

        def ensure_loaded(gidx):
            if gidx in loaded or gidx >= NSEQ * 16:
                return
            loaded.add(gidx)
            head_loads(gidx // 16, gidx % 16, gidx % 2)

        def emit_S(st):
            b, h, i, qb, kt, nk, sidx = st
            if qb == 0 and kt == 0:
                ensure_loaded(b * 16 + h)
            dq = 64 if h < 8 else 96
            q0, k0 = qb * 512, kt * 128
            qlo = max(q0, k0)
            N = q0 + 512 - qlo
            sbk = 2 + sidx % 3
            self.MM(PS[sbk][:, 0:N], kT[i][0:dq, k0:k0 + 128], qT[i][0:dq, qlo:qlo + N], True, True,
                    [f"kT{i}", f"qT{i}"], [f"ps{sbk}"])

        def emit_rest(st, mixb):
            b, h, i, qb, kt, nk, sidx = st
            isA = h < 8
            scale = (64 ** -0.5) if isA else (96 ** -0.5)
            q0, k0 = qb * 512, kt * 128
            qlo = max(q0, k0)
            N = q0 + 512 - qlo
            sbk = 2 + sidx % 3
            pk = sidx % 4
            ob = qb % 2
            self.ACT(Pt[pk][:, 0:N], PS[sbk][:, 0:N], AF.Exp, [f"ps{sbk}"], [f"Pt{pk}"], scale=scale)
            if isA:
                off = qlo - k0
                eng = "dve"
                mcount[0] += 1
                self.TT(eng, Pt[pk][:, 0:N], Pt[pk][:, 0:N], mt[i][:, off:off + N], ALU.mult,
                        [f"Pt{pk}", f"mt{i}"], [f"Pt{pk}"])
            elif qlo == k0:
                self.TT("dve", Pt[pk][:, 0:128], Pt[pk][:, 0:128], causal_b, ALU.mult,
                        [f"Pt{pk}", "Cb"], [f"Pt{pk}"])
            self.MM(PS[ob][:, qlo - q0:512], vaug[i][:, kt, :], Pt[pk][:, 0:N], kt == 0, kt == nk - 1,
                    [f"vaug{i}", f"Pt{pk}"], [f"ps{ob}"])
            if kt == nk - 1:
                rd = rden[ob]
                if qb % 2 == 0:
                    self.RCP(rd[0:64, :], PS[ob][64:128, :], [f"ps{ob}"], [f"rden{ob}"])
                else:
                    self.ACT(rd[0:64, :], PS[ob][64:128, :], AF.Ln, [f"ps{ob}"], [f"rden{ob}"])
                    self.ACT(rd[0:64, :], rd[0:64, :], AF.Exp, [f"rden{ob}"], [f"rden{ob}"], scale=-1.0)
                hp = (h % 2) * 64
                self.TT("dve", mixb[0][hp:hp + 64, h // 2, q0:q0 + 512], PS[ob][0:64, :], rd[0:64, :], ALU.mult,
                        [f"ps{ob}", f"rden{ob}"], [mixb[1]])

        def outproj_stages(b, mixb):
            mixT_, mixk = mixb

            def s0(t, j):
                i2 = t % 2
                self.DMA("sp", xr[i2], x[t * 128:(t + 1) * 128, :], [], [f"xr{i2}"])
                for n in range(2):
                    bo = 5 + n
                    for c in range(8):
                        self.MM(PS[bo][:, 0:512], mixT_[:, c, j * 128:(j + 1) * 128], w_out_b[:, c, n * 512:(n + 1) * 512],
                                c == 0, c == 7, [mixk, "w_out_b"], [f"ps{bo}"])

            def s1(t, j):
                i2, i3 = t % 2, t % 3
                for n in range(2):
                    bo = 5 + n
                    self.TT("dve", h1[i3][:, n * 512:(n + 1) * 512], PS[bo][:, 0:512], xr[i2][:, n * 512:(n + 1) * 512], ALU.add,
                            [f"ps{bo}", f"xr{i2}"], [f"h1_{i3}"])

            def s2(t, j):
                i2, i3 = t % 2, t % 3
                self.DMA("pool", h1_d[t * 128:(t + 1) * 128, :], h1[i3], [f"h1_{i3}"], ["h1_d"])
                self.MS("pool", ss2[i2], 0.0, [f"ss2_{i2}"])
                self.ACT(junk, h1[i3], AF.Square, [f"h1_{i3}", f"ss2_{i2}"], [f"ss2_{i2}"], accum=ss2[i2])
                self.ACT(ss2[i2], ss2[i2], AF.Ln, [f"ss2_{i2}", "epsc"], [f"ss2_{i2}"], bias=self.epsc[:, 0:1], scale=1.0 / D)
                self.ACT(ss2[i2], ss2[i2], AF.Exp, [f"ss2_{i2}"], [f"ss2_{i2}"], scale=-0.5)

            def s3(t, j):
                i2, i3, ix = t % 2, t % 3, t % NXB
                self.STT("dve", xn2b[ix], h1[i3], ss2[i2][:, 0:1], g_ffn, ALU.mult, ALU.mult,
                         [f"h1_{i3}", f"ss2_{i2}", "G"], [f"xn2b{ix}"])

            def s4(t, j):
                i2, ix = t % 2, t % NXB
                for c in range(8):
                    self.TR(PSb[7][:, c * 128:(c + 1) * 128], xn2b[ix][:, c * 128:(c + 1) * 128], ident_b,
                            [f"xn2b{ix}", "Cb"], ["ps7"])
                self.CP("act", xn2T[i2], PSb[7][:, 0:1024].rearrange("p (c n) -> p c n", c=8), ["ps7"], [f"xn2T{i2}"])

            def s5(t, j):
                i2, il, ir = t % 2, t % 3, t % NRT
                for c in range(8):
                    self.MM(PS[7][:, 0:36], xn2T[i2][:, c, :], wr_b[:, c, :], c == 0, c == 7, [f"xn2T{i2}", "wr_b"], ["ps7"])
                self.TT("dve", Ls[il], PS[7][:, 0:36], br_bc, ALU.add, ["ps7", "br_bc"], [f"Ls{il}"])
                self.RED("dve", rt[ir][:, 0:1], Ls[il][:, 0:4], ALU.max, [f"Ls{il}"], [f"rt{ir}"])
                self.TS("dve", rt[ir][:, 1:2], rt[ir][:, 0:1], -1.0, None, ALU.mult, None, [f"rt{ir}"], [f"rt{ir}"])
                self.MS("pool", rt[ir][:, 2:3], 0.0, [f"rt{ir}g"])

            def s6(t, j):
                il, ir = t % 3, t % NRT
                self.ACT(junk4, Ls[il][:, 0:4], AF.Exp, [f"Ls{il}", f"rt{ir}", f"rt{ir}g"], [f"rt{ir}g"],
                         bias=rt[ir][:, 1:2], scale=1.0, accum=rt[ir][:, 2:3])

            def s7(t, j):
                i2, il, ir = t % 2, t % 3, t % NRT
                R = rt[ir]
                self.RCP(R[:, 3:4], R[:, 2:3], [f"rt{ir}g"], [f"rt{ir}w"])
                self.TS("dve", gm, Ls[il][:, 0:4], R[:, 0:1], None, ALU.is_ge, None, [f"Ls{il}", f"rt{ir}"], ["gm"])
                self.TS("dve", pen, gm, -1.0, 1e30, ALU.add, ALU.mult, ["gm"], ["pen"])
                self.TT("dve", Em, Ls[il][:, 4:36].rearrange("p (g e) -> p g e", g=4), pen.unsqueeze(2).to_broadcast([128, 4, 8]),
                        ALU.add, [f"Ls{il}", "pen"], ["Em"])
                Emf = Em.rearrange("p g e -> p (g e)")
                self.p.op("dve", lambda e, o_=top8, i_=Emf: e.max(out=o_, in_=i_), reads=["Em"], writes=["top8"])
                O_ = OH[i2]
                self.TS("dve", O_[:, 0, :], Emf, top8[:, 0:1], None, ALU.is_ge, None, ["Em", "top8"], [f"OH{i2}"])
                self.TS("dve", O_[:, 1, :], Emf, top8[:, 1:2], None, ALU.is_ge, None, ["Em", "top8", f"OH{i2}"], [f"OH{i2}"])
                self.CP("dve", OHb[i2], O_[:, 1, :], [f"OH{i2}"], [f"OHb{i2}"])
                self.TT("dve", O_[:, 1, :], O_[:, 1, :], O_[:, 0, :], ALU.subtract, [f"OH{i2}", f"OHb{i2}"], [f"OH{i2}"])
                self.TT("dve", R[:, 4:5], top8[:, 0:1], top8[:, 1:2], ALU.subtract, ["top8"], [f"rt{ir}d"])

            def s8(t, j):
                i2, ir = t % 2, t % NRT
                R = rt[ir]
                O_ = OH[i2]
                self.ACT(R[:, 5:6], R[:, 4:5], AF.Exp, [f"rt{ir}d"], [f"rt{ir}s"], scale=-1.0)
                self.MM(PS[7][:, 64:96], ustrict_b, OHb[i2], True, False, ["Cb", f"OHb{i2}"], ["ps7"])
                self.MM(PS[7][:, 64:96], ones_b, acc_b, False, True, ["Cb", "acc"], ["ps7"])
                self.TT("dve", prod, O_, PS[7][:, 64:96].unsqueeze(1).to_broadcast([128, 2, 32]), ALU.mult,
                        [f"OH{i2}", "ps7"], ["prod"])
                self.RED("dve", rk, prod, ALU.add, ["prod"], ["rk"])
                self.TT("dve", prod2, O_, eoff.unsqueeze(1).to_broadcast([128, 2, 32]), ALU.mult, [f"OH{i2}", "C"], ["prod2"])
                self.RED("dve", ek, prod2, ALU.add, ["prod2"], ["ek"])
                self.TT("pool", acc_b, acc_b, OHb[i2], ALU.add, ["acc", f"OHb{i2}"], ["acc"])
                ok_ = okk[i2]
                self.TS("dve", ok_, rk, float(CAP), None, ALU.is_lt, None, ["rk"], [f"okk{i2}"])
                self.TT("dve", slf, rk, ek, ALU.add, ["rk", "ek"], ["slf"])
                self.TS("dve", slf, slf, float(-TRASH), None, ALU.add, None, ["slf"], ["slf"])
                self.TT("dve", slf, slf, ok_, ALU.mult, ["slf", f"okk{i2}"], ["slf"])
                self.TS("dve", slf, slf, float(TRASH), None, ALU.add, None, ["slf"], ["slf"])
                self.CP("dve", slots_all[:, t, :], slf, ["slf"], ["slots"])

            def s9(t, j):
                i2, ir, ix = t % 2, t % NRT, t % NXB
                R = rt[ir]
                w1 = wts_all[:, t, 0:1]
                w2 = wts_all[:, t, 1:2]
                self.TS("dve", R[:, 6:7], R[:, 5:6], 1.0, None, ALU.add, None, [f"rt{ir}s"], [f"rt{ir}s2"])
                self.RCP(R[:, 6:7], R[:, 6:7], [f"rt{ir}s2"], [f"rt{ir}s2"])
                self.TT("dve", w1, R[:, 3:4], R[:, 6:7], ALU.mult, [f"rt{ir}w", f"rt{ir}s2"], ["wts"])
                self.TT("dve", w2, R[:, 3:4], w1, ALU.subtract, [f"rt{ir}w", "wts"], ["wts"])
                self.TT("dve", wts_all[:, t, :], wts_all[:, t, :], okk[i2], ALU.mult, ["wts", f"okk{i2}"], ["wts"])
                for k in range(2):
                    idx = slots_all[:, t, k:k + 1]
                    self.p.dma("pool", lambda e, idx_=idx, src_=xn2b[ix]: e.indirect_dma_start(
                        out=xg_d, out_offset=bass.IndirectOffsetOnAxis(ap=idx_, axis=0), in_=src_, in_offset=None),
                        reads=[f"xn2b{ix}", "slots"], writes=["xg_d"])

            sts = [s0, s1, s2, s3, s4, s5, s6, s7, s8, s9]
            K_ = len(sts)
            out_ = []
            for tau in range(TPS + K_ - 1):
                for jj in reversed(range(K_)):
                    j = tau - jj
                    if 0 <= j < TPS:
                        out_.append((lambda f=sts[jj], t=b * TPS + j, j=j: f(t, j)))
            return out_

        hc = 0
        pending = []
        for b in range(NSEQ):
            mixb = (mixTs[b % 2], f"mixT{b%2}")
            steps = []
            sidx = 0
            for h in range(16):
                i = hc % 2
                hc += 1
                for qb in range(4):
                    nk = 4 * qb + 4
                    for kt in range(nk):
                        steps.append((b, h, i, qb, kt, nk, sidx))
                        sidx += 1
            n = len(steps)
            every = max(1, n // (len(pending) + 1)) if pending else 0
            for k in range(min(LA, n)):
                emit_S(steps[k])
            for k in range(n):
                if k + LA < n:
                    emit_S(steps[k + LA])
                emit_rest(steps[k], mixb)
                if steps[k][3] == 0 and steps[k][4] == 0:
                    ensure_loaded(steps[k][0] * 16 + steps[k][1] + 1)
                if pending and (k % every == every - 1):
                    pending.pop(0)()
            while pending:
                pending.pop(0)()
            pending = outproj_stages(b, mixb)
        while pending:
            pending.pop(0)()
        if self.debug:
            dbgt = sb([128, NT * 4], F32)
            self.CP("dve", dbgt[:, 0:NT * 2], slots_all.rearrange("p t k -> p (t k)"), ["slots"], ["dbgt"])
            self.CP("dve", dbgt[:, NT * 2:NT * 4], wts_all.rearrange("p t k -> p (t k)"), ["wts", "dbgt"], ["dbgt"])
            self.DMA("sp", rt_d, dbgt, ["dbgt"], ["rt_d"])

    def phase3(self, w_gate, w_up, w_down, xg_d, y_d, ident_b):
        PS, PSb = self.PS, self.PSb
        sb = self.sb
        stg = [sb([128, 4096], F32) for _ in range(3)]
        wg = [sb([128, 8, FF], BF16) for _ in range(2)]
        wu = [sb([128, 8, FF], BF16) for _ in range(2)]
        wd = [sb([128, 4, D], BF16) for _ in range(2)]
        xrow = [sb([128, D], BF16) for _ in range(3)]
        xTe = [sb([128, 8, CAP], BF16) for _ in range(2)]
        hT = [sb([128, 4, CAP], BF16) for _ in range(2)]
        sg = [sb([128, 512], F32) for _ in range(2)]
        ysb = [sb([128, D], F32) for _ in range(6)]
        nst = CAP // 128
        cnt = {"xc": 0, "yc": 0, "gub": 0}

        def w_load(e, which):
            i = e % 2
            src, dst, dn, view = ((w_gate[e], wg[i], f"wg{i}", "(c p) f -> p c f"),
                                  (w_up[e], wu[i], f"wu{i}", "(c p) f -> p c f"),
                                  (w_down[e], wd[i], f"wd{i}", "(c p) n -> p c n"))[which]
            nch = dst.shape[1]
            s3 = stg[which].rearrange("p (c f) -> p c f", c=nch)
            self.DMA("sp", s3, src.rearrange(view, p=128), [], [f"stg{which}"])

        def w_cast(e, which):
            i = e % 2
            dst, dn = ((wg[i], f"wg{i}"), (wu[i], f"wu{i}"), (wd[i], f"wd{i}"))[which]
            nch = dst.shape[1]
            s3 = stg[which].rearrange("p (c f) -> p c f", c=nch)
            if which == 0:
                self.CP("act", dst, s3, [f"stg{which}"], [dn])
            elif which == 1:
                self.CP("dve", dst, s3, [f"stg{which}"], [dn])
            else:
                self.CP("pool", dst[:, 0:1, :], s3[:, 0:1, :], [f"stg{which}"], [dn + "c0"])
                self.CP("act", dst[:, 1:2, :], s3[:, 1:2, :], [f"stg{which}"], [dn + "c1"])
                self.CP("dve", dst[:, 2:3, :], s3[:, 2:3, :], [f"stg{which}"], [dn + "c2"])
                self.CP("act", dst[:, 3:4, :], s3[:, 3:4, :], [f"stg{which}"], [dn + "c3"])

        def x_trans(e):
            i = e % 2
            for s_ in range(nst):
                k = cnt["xc"] % 3
                cnt["xc"] += 1
                r0 = e * CAP + s_ * 128
                self.DMA("sp", xrow[k], xg_d[r0:r0 + 128, :], [], [f"xrow{k}"])
                bt = cnt["xc"] % 2
                for c in range(8):
                    self.TR(PSb[bt][:, c * 128:(c + 1) * 128], xrow[k][:, c * 128:(c + 1) * 128], ident_b,
                            [f"xrow{k}", "Cb"], [f"ps{bt}"])
                self.CP(("act", "dve")[s_ % 2], xTe[i][:, :, s_ * 128:(s_ + 1) * 128],
                        PSb[bt][:, 0:1024].rearrange("p (c n) -> p c n", c=8), [f"ps{bt}"], [f"xTe{i}"])

        def gate_up(e, ffc):
            i = e % 2
            for (n0, N) in ((0, 512), (512, CAP - 512)):
                bg = 2 + (cnt["gub"] % 2) * 2
                bu = bg + 1
                cnt["gub"] += 1
                for c in range(8):
                    self.MM(PS[bg][:, 0:N], wg[i][:, c, ffc * 128:(ffc + 1) * 128], xTe[i][:, c, n0:n0 + N],
                            c == 0, c == 7, [f"wg{i}", f"xTe{i}"], [f"ps{bg}"])
                for c in range(8):
                    self.MM(PS[bu][:, 0:N], wu[i][:, c, ffc * 128:(ffc + 1) * 128], xTe[i][:, c, n0:n0 + N],
                            c == 0, c == 7, [f"wu{i}", f"xTe{i}"], [f"ps{bu}"])
                sgi = cnt["gub"] % 2
                self.ACT(sg[sgi][:, 0:N], PS[bg][:, 0:N], AF.Silu, [f"ps{bg}"], [f"sg{sgi}"])
                self.TT("dve", hT[i][:, ffc, n0:n0 + N], sg[sgi][:, 0:N], PS[bu][:, 0:N], ALU.mult,
                        [f"sg{sgi}", f"ps{bu}"], [f"hT{i}"])

        def down(e):
            i = e % 2
            for s_ in range(nst):
                k = cnt["yc"] % 6
                cnt["yc"] += 1
                for n in range(2):
                    by = 6 + n
                    for c in range(4):
                        self.MM(PS[by][:, 0:512], hT[i][:, c, s_ * 128:(s_ + 1) * 128], wd[i][:, c, n * 512:(n + 1) * 512],
                                c == 0, c == 3, [f"hT{i}", f"wd{i}c{c}"], [f"ps{by}"])
                    self.CP(("act", "dve")[n], ysb[k][:, n * 512:(n + 1) * 512], PS[by][:, 0:512], [f"ps{by}"], [f"ysb{k}"])
                r0 = e * CAP + s_ * 128
                self.DMA("pool", y_d[r0:r0 + 128, :], ysb[k], [f"ysb{k}"], ["y_d"])

        for w in range(3):
            w_load(0, w)
            w_cast(0, w)
        x_trans(0)
        for e in range(NE):
            nxt = e + 1 < NE
            if nxt:
                for w in range(3):
                    w_load(e + 1, w)
            for ffc in range(4):
                gate_up(e, ffc)
                if nxt and ffc < 3:
                    w_cast(e + 1, ffc)
            if nxt:
                x_trans(e + 1)
            down(e)

    def phase4(self, pin, w_ple_proj, w_ple_gate, b_ple_gate, h1_d, y_d, out, ident_b, ones_b, g_ple,
               slots_all, wts_all):
        PS, PSb = self.PS, self.PSb
        sb = self.sb
        wpg = sb([128, 8, D], BF16)
        wpp = sb([128, 2, D], BF16)
        stg = [sb([128, D], F32) for _ in range(2)]
        for c in range(8):
            self.DMA("sp", stg[c % 2], w_ple_gate[c * 128:(c + 1) * 128, :], [], [f"wstg{c%2}"])
            self.CP(("act", "dve")[c % 2], wpg[:, c, :], stg[c % 2], [f"wstg{c%2}"], ["wpg"])
        for c in range(2):
            self.DMA("sp", stg[c % 2], w_ple_proj[c * 128:(c + 1) * 128, :], [], [f"wstg{c%2}"])
            self.CP(("act", "dve")[c % 2], wpp[:, c, :], stg[c % 2], [f"wstg{c%2}"], ["wpp"])
        bf = sb([1, D], F32)
        bhi = sb([1, D], BF16)
        blo = sb([1, D], BF16)
        bt_ = sb([1, D], F32)
        self.DMA("sp", bf[0:1, :], b_ple_gate, [], ["bf"])
        self.CP("dve", bhi[0:1, :], bf[0:1, :], ["bf"], ["bhi"])
        self.TT("dve", bt_[0:1, :], bf[0:1, :], bhi[0:1, :], ALU.subtract, ["bf", "bhi"], ["bt_"])
        self.CP("dve", blo[0:1, :], bt_[0:1, :], ["bt_"], ["blo"])
        NH = 9
        H = [sb([128, D], F32) for _ in range(NH)]
        y1 = [sb([128, D], F32) for _ in range(2)]
        y2 = [sb([128, D], F32) for _ in range(2)]
        pt = [sb([128, PLE], F32) for _ in range(2)]
        pb = [sb([128, PLE], BF16) for _ in range(2)]
        pT = [sb([128, 2, 128], BF16) for _ in range(2)]
        h2b = [sb([128, D], BF16) for _ in range(2)]
        h2T = [sb([128, 8, 128], BF16) for _ in range(2)]
        ev = [sb([128, D], F32) for _ in range(2)]
        NG = 5
        gt = [sb([128, D], F32) for _ in range(NG)]
        junk = sb([128, D], BF16)
        ssall = sb([128, NT, 2], F32)
        rsall = sb([128, NT], F32)

        def sw_pipe(ntiles, sts):
            K_ = len(sts)
            for tau in range(ntiles + K_ - 1):
                for jj in reversed(range(K_)):
                    t = tau - jj
                    if 0 <= t < ntiles:
                        sts[jj](t)

        self.MS("pool", ssall, 0.0, ["ssall"])

        def a0(t):
            i = t % 2
            self.DMA("sp", pt[i], pin[t * 128:(t + 1) * 128, :], [], [f"pt{i}"])

        def a1(t):
            i = t % 2
            self.CP("pool", pb[i], pt[i], [f"pt{i}"], [f"pb{i}"])

        def a2(t):
            i = t % 2
            for c in range(2):
                self.TR(PSb[0][:, c * 128:(c + 1) * 128], pb[i][:, c * 128:(c + 1) * 128], ident_b, [f"pb{i}", "Cb"], ["ps0"])
            self.CP("act", pT[i], PSb[0][:, 0:256].rearrange("p (c n) -> p c n", c=2), ["ps0"], [f"pT{i}"])

        def a3(t):
            i = t % 2
            for n in range(2):
                be = 2 + n
                for c in range(2):
                    self.MM(PS[be][:, 0:512], pT[i][:, c, :], wpp[:, c, n * 512:(n + 1) * 512], c == 0, c == 1,
                            [f"pT{i}", "wpp"], [f"ps{be}"])

        def a4(t):
            for n in range(2):
                be = 2 + n
                self.ACT(junk[:, 0:512], PS[be][:, 0:512], AF.Square, [f"ps{be}", "ssall"], ["ssall"],
                         accum=ssall[:, t, n:n + 1])

        sw_pipe(NT, [a0, a1, a2, a3, a4])
        self.TT("dve", rsall, ssall[:, :, 0], ssall[:, :, 1], ALU.add, ["ssall"], ["rsall"])
        self.ACT(rsall, rsall, AF.Sqrt, ["rsall", "epsc"], ["rsall"], bias=self.epsc[:, 0:1], scale=1.0 / D)
        self.RCP(rsall, rsall, ["rsall"], ["rsall"])

        def s0(t):
            i, ih = t % 2, t % NH
            self.DMA("sp", H[ih], h1_d[t * 128:(t + 1) * 128, :], [], [f"h{ih}"])
            self.DMA("sp", pt[i], pin[t * 128:(t + 1) * 128, :], [], [f"pt{i}"])
            for k, Y in ((0, y1[i]), (1, y2[i])):
                idx = slots_all[:, t, k:k + 1]
                self.p.dma("pool", lambda e, idx_=idx, dst_=Y: e.indirect_dma_start(
                    out=dst_, out_offset=None, in_=y_d, in_offset=bass.IndirectOffsetOnAxis(ap=idx_, axis=0)),
                    reads=["slots", "y_d"], writes=[f"y{k}_{i}"])

        def s1(t):
            i, ih = t % 2, t % NH
            self.CP("pool", pb[i], pt[i], [f"pt{i}"], [f"pb{i}"])
            self.STT("dve", H[ih], y1[i], wts_all[:, t, 0:1], H[ih], ALU.mult, ALU.add, [f"y0_{i}", f"h{ih}", "wts"], [f"h{ih}"])
            self.STT("dve", H[ih], y2[i], wts_all[:, t, 1:2], H[ih], ALU.mult, ALU.add, [f"y1_{i}", f"h{ih}", "wts"], [f"h{ih}"])

        def s2(t):
            i, ih = t % 2, t % NH
            for c in range(2):
                self.TR(PSb[0][:, c * 128:(c + 1) * 128], pb[i][:, c * 128:(c + 1) * 128], ident_b, [f"pb{i}", "Cb"], ["ps0"])
            self.CP("act", pT[i], PSb[0][:, 0:256].rearrange("p (c n) -> p c n", c=2), ["ps0"], [f"pT{i}"])
            self.CP("act", h2b[i], H[ih], [f"h{ih}"], [f"h2b{i}"])

        def s3(t):
            i = t % 2
            for c in range(8):
                self.TR(PSb[1][:, c * 128:(c + 1) * 128], h2b[i][:, c * 128:(c + 1) * 128], ident_b, [f"h2b{i}", "Cb"], ["ps1"])
            self.CP("act", h2T[i], PSb[1][:, 0:1024].rearrange("p (c n) -> p c n", c=8), ["ps1"], [f"h2T{i}"])
            for n in range(2):
                be = 2 + n
                for c in range(2):
                    self.MM(PS[be][:, 0:512], pT[i][:, c, :], wpp[:, c, n * 512:(n + 1) * 512], c == 0, c == 1,
                            [f"pT{i}", "wpp"], [f"ps{be}"])

        def s4(t):
            i = t % 2
            for n in range(2):
                be = 2 + n
                self.STT("dve", ev[i][:, n * 512:(n + 1) * 512], PS[be][:, 0:512], rsall[:, t:t + 1], g_ple[:, n * 512:(n + 1) * 512],
                         ALU.mult, ALU.mult, [f"ps{be}", "rsall", "G"], [f"ev{i}"])
            for n in range(2):
                bg = 4 + n
                for c in range(8):
                    self.MM(PS[bg][:, 0:512], h2T[i][:, c, :], wpg[:, c, n * 512:(n + 1) * 512], c == 0, False,
                            [f"h2T{i}", "wpg"], [f"ps{bg}"])
                self.MM(PS[bg][:, 0:512], ones_b[0:1, :], bhi[0:1, n * 512:(n + 1) * 512], False, False, ["Cb", "bhi"], [f"ps{bg}"])
                self.MM(PS[bg][:, 0:512], ones_b[0:1, :], blo[0:1, n * 512:(n + 1) * 512], False, True, ["Cb", "blo"], [f"ps{bg}"])

        def s5(t):
            ig = t % NG
            for n in range(2):
                bg = 4 + n
                self.ACT(gt[ig][:, n * 512:(n + 1) * 512], PS[bg][:, 0:512], AF.Sigmoid, [f"ps{bg}"], [f"gt{ig}"])

        def s6(t):
            i, ig = t % 2, t % NG
            Gt, E = gt[ig], ev[i]
            self.TT("pool", Gt[:, 0:384], Gt[:, 0:384], E[:, 0:384], ALU.mult, [f"gt{ig}", f"ev{i}"], [f"gt{ig}a"])
            self.TT("dve", Gt[:, 384:1024], Gt[:, 384:1024], E[:, 384:1024], ALU.mult, [f"gt{ig}", f"ev{i}"], [f"gt{ig}b"])

        def s7(t):
            ig, ih = t % NG, t % NH
            self.TT("dve", gt[ig], gt[ig], H[ih], ALU.add, [f"gt{ig}", f"gt{ig}a", f"gt{ig}b", f"h{ih}"], [f"gt{ig}"])

        def s8(t):
            ig = t % NG
            self.DMA("sp", out[t * 128:(t + 1) * 128, :], gt[ig], [f"gt{ig}"], ["out"])

        sw_pipe(NT, [s0, s1, s2, s3, s4, s5, s6, s7, s8])


def _t5_bucket(d):
    max_exact = 16
    n = np.maximum(d, 0)
    nf = np.maximum(n, 1).astype(np.float32)
    large = max_exact + (np.log(nf / np.float32(max_exact)) / np.float32(math.log(2048 / max_exact))
                         * np.float32(32 - max_exact)).astype(np.int32)
    large = np.minimum(large, 31)
    return np.where(n < max_exact, n, large)


def _constants():
    c = np.zeros((128, 1024), np.float32)
    c[:, 0:128] = np.eye(128, dtype=np.float32)
    jj = np.arange(128)[:, None]
    cc = np.arange(128)[None, :]
    c[:, 128:256] = (cc >= jj).astype(np.float32)
    c[:, 256:384] = (jj < cc).astype(np.float32)
    c[:, 384:416] = (np.arange(32) * CAP).astype(np.float32)[None, :]
    half = 16
    inv = (1.0 / (np.float32(10000.0) ** (np.arange(half, dtype=np.float32) * np.float32(2.0) / np.float32(32)))).astype(np.float32)
    pos = np.arange(S, dtype=np.float32)
    ang = (pos[:, None] * inv[None, :]).astype(np.float32)
    cos = np.cos(ang).astype(np.float32).reshape(16, 128, 16).transpose(1, 0, 2).reshape(128, 256)
    sin = np.sin(ang).astype(np.float32).reshape(16, 128, 16).transpose(1, 0, 2).reshape(128, 256)
    c[:, 512:768] = cos
    c[:, 768:1024] = sin
    dist = np.arange(MTW)[None, :] - np.arange(128)[:, None]
    valid = (dist >= 0) & (dist < S)
    d = np.clip(dist, 0, S - 1)
    mult = np.zeros(d.shape, np.float32)
    mult += (d <= 128)
    mult += ((d % 4 == 0) & (d <= 512))
    mult += (d % 16 == 0)
    mult = np.where(valid, mult, 0.0).astype(np.float32)
    bucket = _t5_bucket(d)
    return c, mult, bucket, valid


_CACHE = {}


def _get_program(debug=False, stop_after=99):
    key = (debug, stop_after)
    if key not in _CACHE:
        b = Builder(debug=debug, stop_after=stop_after)
        _CACHE[key] = b.build()
    return _CACHE[key]


def _prep_inputs(inputs):
    f = lambda k: np.asarray(inputs[k], dtype=np.float32)
    x = f("x").reshape(32 * S, D)
    pp = f("p").reshape(32 * S, PLE)
    cst, mult, bucket, valid = _constants()
    rel_bias = f("rel_bias")
    bt = rel_bias[bucket]
    bt = np.where(valid[:, :, None], bt, np.float32(0.0))
    bias_toep = np.ascontiguousarray(bt.transpose(2, 0, 1)).astype(np.float32)
    gv = np.zeros((1, 4096), np.float32)
    segs = [("norm_mix_gain", 0), ("norm_ffn_gain", 1024), ("ple_norm_gain", 2048), ("qn_a_gain", 3072),
            ("kn_a_gain", 3136), ("q_a_gain", 3200), ("kv_a_gain", 3584), ("qn_nope_gain", 3840),
            ("qn_rope_gain", 3904), ("kn_nope_gain", 3936), ("kn_rope_gain", 4000)]
    for k, o in segs:
        v = f(k).reshape(-1)
        gv[0, o:o + v.size] = v
    wr = np.concatenate([f("w_router_group")[0], f("w_router_expert")[0]], axis=1)
    br = np.concatenate([f("b_router_group")[0], f("b_router_expert")[0]], axis=0)[None, :]
    shared = {
        "w_in": f("w_in")[0], "w_q_up": f("w_q_up")[0], "w_kv_up": f("w_kv_up")[0], "w_out": f("w_out")[0],
        "wr": np.ascontiguousarray(wr), "br": np.ascontiguousarray(br),
        "w_exp_gate": f("w_exp_gate")[0], "w_exp_up": f("w_exp_up")[0], "w_exp_down": f("w_exp_down")[0],
        "w_ple_proj": f("w_ple_proj")[0], "w_ple_gate": f("w_ple_gate")[0], "b_ple_gate": f("b_ple_gate"),
        "gvec": gv, "cst": cst, "bias_toep": bias_toep, "mult_toep": mult,
    }
    in_maps = []
    for c in range(NCORES):
        m = dict(shared)
        m["x"] = x[c * NTOK:(c + 1) * NTOK]
        m["p"] = pp[c * NTOK:(c + 1) * NTOK]
        in_maps.append(m)
    return in_maps


def kernel(**inputs):
    nc = _get_program()
    in_maps = _prep_inputs(inputs)
    res = run_bass_kernel_spmd(nc, in_maps, core_ids=list(range(NCORES)))
    outs = [np.asarray(r["out"], dtype=np.float32) for r in res.results]
    return np.concatenate(outs, axis=0).reshape(32, S, D)
```
